# Optimizing a Trainium2 kernel written in Bass

```python
import math
import numpy as np
import jax
import jax.numpy as jnp
from jax import lax

D_MODEL = 1024
BATCH = 4
SEQ = 8192
DEPTH = 2

CHUNK = 64
Q_BLOCK = 128
HEAD_DIM = 128
N_MIX_HEADS = D_MODEL // HEAD_DIM
MIX_WIDTH = N_MIX_HEADS * HEAD_DIM
A_HEADS = N_MIX_HEADS // 2
B_HEADS = N_MIX_HEADS - A_HEADS
C_HEADS = N_MIX_HEADS // 2
D_HEADS = N_MIX_HEADS - C_HEADS
A_HALF = HEAD_DIM // 2
A_W = A_HEADS * HEAD_DIM
B_W = B_HEADS * HEAD_DIM
C_W = C_HEADS * HEAD_DIM
D_W = D_HEADS * HEAD_DIM
CONV_W = 4
ROPE_THETA = 10000.0
RMS_EPS = 1e-6
PLE_DIM = 256
N_GROUPS = 4
EXPERTS_PER_GROUP = 4
N_EXPERTS = N_GROUPS * EXPERTS_PER_GROUP
TOP_K_IN_GROUP = 2
EXPERT_FF = D_MODEL // 2

EVEN_SPLITS = (A_W, A_W, A_W, B_W, B_W, B_W, B_W, B_HEADS, B_HEADS)
ODD_SPLITS = (C_W, C_W, C_W, C_W, C_HEADS, C_HEADS, D_W, D_W, D_W, D_HEADS)
EVEN_IN = sum(EVEN_SPLITS)
ODD_IN = sum(ODD_SPLITS)

kernel_name = 'hybrid_chunk_causal_trunk'


def rmsnorm(x, gain):
    xf = x.astype(jnp.float32)
    y = xf * lax.rsqrt(jnp.mean(xf * xf, axis=-1, keepdims=True) + RMS_EPS)
    return (y * gain.astype(jnp.float32)).astype(x.dtype)


def l2norm(x):
    xf = x.astype(jnp.float32)
    return xf * lax.rsqrt(jnp.sum(xf * xf, axis=-1, keepdims=True) + RMS_EPS)


def _split(z, sizes):
    offs = np.cumsum(sizes)[:-1].tolist()
    return jnp.split(z, offs, axis=-1)


def _heads(t, n, d):
    return t.reshape(t.shape[0], t.shape[1], n, d).transpose(0, 2, 1, 3)


def _merge(t):
    b, h, s, d = t.shape
    return t.transpose(0, 2, 1, 3).reshape(b, s, h * d)


def rope(x):
    d = x.shape[-1]
    inv = ROPE_THETA ** (-jnp.arange(0, d, 2, dtype=jnp.float32) / d)
    ang = jnp.arange(x.shape[2], dtype=jnp.float32)[:, None] * inv[None, :]
    cos, sin = jnp.cos(ang), jnp.sin(ang)
    xf = x.astype(jnp.float32)
    x1, x2 = xf[..., : d // 2], xf[..., d // 2:]
    return jnp.concatenate([x1 * cos - x2 * sin, x2 * cos + x1 * sin], axis=-1).astype(x.dtype)


def causal_conv(x, w):
    return lax.conv_general_dilated(
        x, w[:, None, :].astype(x.dtype), window_strides=(1,), padding=[(CONV_W - 1, 0)],
        dimension_numbers=('NWC', 'WIO', 'NWC'), feature_group_count=x.shape[-1])


def diff_attention(q, k, v, lam, gain, lam_init):
    b, h2, s, d = q.shape
    nh = h2 // 2
    nb = s // Q_BLOCK
    key_chunk = jnp.arange(s) // CHUNK
    q_blocks = jnp.moveaxis(q.reshape(b, h2, nb, Q_BLOCK, d), 2, 0)

    def one_block(args):
        qi, bi = args
        logits = jnp.einsum('bhqd,bhkd->bhqk', qi, k).astype(jnp.float32) * (d ** -0.5)
        q_chunk = (bi * Q_BLOCK + jnp.arange(Q_BLOCK)) // CHUNK
        logits = jnp.where(key_chunk[None, :] <= q_chunk[:, None], logits, -jnp.inf)
        probs = jax.nn.softmax(logits, axis=-1).reshape(b, nh, 2, Q_BLOCK, s)
        weights = probs[:, :, 0] - lam * probs[:, :, 1]
        return jnp.einsum('bhqk,bhkd->bhqd', weights.astype(v.dtype), v)

    out = lax.map(one_block, (q_blocks, jnp.arange(nb)))
    out = jnp.moveaxis(out, 0, 2).reshape(b, nh, s, v.shape[-1])
    return rmsnorm(out, gain) * (1.0 - lam_init)


def mlstm(q, k, v, i_pre, f_pre):
    q, k, v = (t.astype(jnp.float32) for t in (q, k, v))
    b, nh, s, d = q.shape
    nc = s // CHUNK
    k = k * (d ** -0.5)
    log_f = jax.nn.log_sigmoid(f_pre.astype(jnp.float32))
    i_pre = i_pre.astype(jnp.float32)
    to_c = lambda t: jnp.moveaxis(t.reshape(b, nh, nc, CHUNK, *t.shape[3:]), 2, 0)
    qc, kc, vc, ic = to_c(q), to_c(k), to_c(v), to_c(i_pre)
    bc = jnp.cumsum(to_c(log_f), axis=-1)
    causal = jnp.tril(jnp.ones((CHUNK, CHUNK), bool))

    def step(carry, xs):
        c_st, n_st, m_st = carry
        qi, ki, vi, bi, ii = xs
        d_log = jnp.where(causal, bi[..., :, None] - bi[..., None, :] + ii[..., None, :], -jnp.inf)
        inter_log = bi + m_st[..., None]
        m_t = jnp.maximum(inter_log, jnp.max(d_log, axis=-1))
        d_w = jnp.exp(d_log - m_t[..., None])
        inter_w = jnp.exp(inter_log - m_t)
        a = jnp.einsum('bhtd,bhsd->bhts', qi, ki) * d_w
        num = jnp.einsum('bhts,bhsv->bhtv', a, vi) + inter_w[..., None] * jnp.einsum('bhvk,bhtk->bhtv', c_st, qi)
        den = jnp.sum(a, axis=-1) + inter_w * jnp.einsum('bhk,bhtk->bht', n_st, qi)
        h = num / jnp.maximum(jnp.abs(den), jnp.exp(-m_t))[..., None]
        b_last = bi[..., -1]
        w_log = b_last[..., None] - bi + ii
        m_new = jnp.maximum(b_last + m_st, jnp.max(w_log, axis=-1))
        sw = jnp.exp(w_log - m_new[..., None])
        decay = jnp.exp(b_last + m_st - m_new)
        c_new = decay[..., None, None] * c_st + jnp.einsum('bhs,bhsv,bhsk->bhvk', sw, vi, ki)
        n_new = decay[..., None] * n_st + jnp.einsum('bhs,bhsk->bhk', sw, ki)
        return (c_new, n_new, m_new), h

    init = (jnp.zeros((b, nh, d, d), jnp.float32), jnp.zeros((b, nh, d), jnp.float32),
            jnp.zeros((b, nh), jnp.float32))
    _, hs = lax.scan(step, init, (qc, kc, vc, bc, ic))
    return jnp.moveaxis(hs, 0, 2).reshape(b, nh, s, d)


def gated_deltanet(q, k, v, g, beta):
    q, k, v, g, beta = (t.astype(jnp.float32) for t in (q, k, v, g, beta))
    b, nh, s, dk = q.shape
    dv = v.shape[-1]
    nc = s // CHUNK
    q = q * (dk ** -0.5)
    to_c = lambda t: t.reshape(b, nh, nc, CHUNK, *t.shape[3:])
    qc, kc, vc, betac = to_c(q), to_c(k), to_c(v), to_c(beta)
    bc = jnp.cumsum(to_c(g), axis=-1)
    incl = jnp.tril(jnp.ones((CHUNK, CHUNK), bool))
    strict = jnp.tril(jnp.ones((CHUNK, CHUNK), bool), -1)
    gam = jnp.exp(jnp.where(incl, bc[..., :, None] - bc[..., None, :], -jnp.inf))
    kb = kc * betac[..., None]
    a = jnp.where(strict, jnp.einsum('bhcid,bhcjd->bhcij', kb, kc) * gam, 0.0) + jnp.eye(CHUNK, dtype=jnp.float32)
    rhs = jnp.concatenate([vc * betac[..., None], kb * jnp.exp(bc)[..., None]], axis=-1)
    sol = lax.linalg.triangular_solve(a, rhs, left_side=True, lower=True, unit_diagonal=True)
    u, w = sol[..., :dv], sol[..., dv:]
    qk = jnp.einsum('bhcid,bhcjd->bhcij', qc, kc) * gam
    q_dec = qc * jnp.exp(bc)[..., None]
    k_dec = kc * jnp.exp(bc[..., -1:] - bc)[..., None]
    chunk_decay = jnp.exp(bc[..., -1])
    mv = lambda t: jnp.moveaxis(t, 2, 0)

    def step(st, xs):
        ui, wi, qki, qdi, kdi, cdi = xs
        v_new = ui - jnp.einsum('bhlk,bhkv->bhlv', wi, st)
        o = jnp.einsum('bhlk,bhkv->bhlv', qdi, st) + jnp.einsum('bhls,bhsv->bhlv', qki, v_new)
        st = cdi[..., None, None] * st + jnp.einsum('bhsk,bhsv->bhkv', kdi, v_new)
        return st, o

    _, os = lax.scan(step, jnp.zeros((b, nh, dk, dv), jnp.float32),
                     (mv(u), mv(w), mv(qk), mv(q_dec), mv(k_dec), mv(chunk_decay)))
    return jnp.moveaxis(os, 0, 2).reshape(b, nh, s, dv)


def forgetting_attention(q, k, v, log_f):
    b, nh, s, d = q.shape
    nb = s // Q_BLOCK
    cum = jnp.cumsum(log_f, axis=-1)
    key_pos = jnp.arange(s)
    q_blocks = jnp.moveaxis(q.reshape(b, nh, nb, Q_BLOCK, d), 2, 0)
    c_blocks = jnp.moveaxis(cum.reshape(b, nh, nb, Q_BLOCK), 2, 0)

    def one_block(args):
        qi, ci, bi = args
        logits = (jnp.einsum('bhqd,bhkd->bhqk', qi, k).astype(jnp.float32) * (d ** -0.5)
                  + ci[..., :, None] - cum[..., None, :])
        q_pos = bi * Q_BLOCK + jnp.arange(Q_BLOCK)
        logits = jnp.where(key_pos[None, :] <= q_pos[:, None], logits, -jnp.inf)
        probs = jax.nn.softmax(logits, axis=-1)
        return jnp.einsum('bhqk,bhkd->bhqd', probs.astype(v.dtype), v)

    out = lax.map(one_block, (q_blocks, c_blocks, jnp.arange(nb)))
    return jnp.moveaxis(out, 0, 2).reshape(b, nh, s, d)


def even_mixer(hn, w_in, w_out, lam_q1, lam_k1, lam_q2, lam_k2, subln, conv_b, ig_bias, fg_bias, norm_b, layer_idx):
    qa, ka, va, qb, kb, vb, ob, ib, fb = _split(hn @ w_in, EVEN_SPLITS)
    qa = rope(_heads(qa, 2 * A_HEADS, A_HALF))
    ka = rope(_heads(ka, 2 * A_HEADS, A_HALF))
    va = _heads(va, A_HEADS, HEAD_DIM)
    lam_init = 0.8 - 0.6 * math.exp(-0.3 * layer_idx)
    lam = (jnp.exp(jnp.sum(lam_q1 * lam_k1).astype(jnp.float32))
           - jnp.exp(jnp.sum(lam_q2 * lam_k2).astype(jnp.float32)) + lam_init)
    out_a = diff_attention(qa, ka, va, lam, subln, lam_init)
    qk = jax.nn.silu(causal_conv(jnp.concatenate([qb, kb], axis=-1), conv_b))
    qb, kb = jnp.split(qk, 2, axis=-1)
    i_pre = jnp.swapaxes(ib + ig_bias, 1, 2)
    f_pre = jnp.swapaxes(fb + fg_bias, 1, 2)
    hb = mlstm(_heads(qb, B_HEADS, HEAD_DIM), _heads(kb, B_HEADS, HEAD_DIM), _heads(vb, B_HEADS, HEAD_DIM),
               i_pre, f_pre).astype(hn.dtype)
    out_b = rmsnorm(hb, norm_b) * jax.nn.sigmoid(_heads(ob, B_HEADS, HEAD_DIM))
    return jnp.concatenate([_merge(out_a), _merge(out_b)], axis=-1) @ w_out


def odd_mixer(hn, w_in, w_out, conv_c, a_log, dt_bias, norm_c, fd_bias):
    qc, kc, vc, gc, ac, bc, qd, kd, vd, fd = _split(hn @ w_in, ODD_SPLITS)
    qkv = jax.nn.silu(causal_conv(jnp.concatenate([qc, kc, vc], axis=-1), conv_c))
    qc, kc, vc = jnp.split(qkv, 3, axis=-1)
    qc = l2norm(_heads(qc, C_HEADS, HEAD_DIM))
    kc = l2norm(_heads(kc, C_HEADS, HEAD_DIM))
    vc = _heads(vc, C_HEADS, HEAD_DIM)
    g = -jnp.exp(a_log.astype(jnp.float32)) * jax.nn.softplus((ac + dt_bias).astype(jnp.float32))
    beta = jax.nn.sigmoid(bc.astype(jnp.float32))
    oc = gated_deltanet(qc, kc, vc, jnp.swapaxes(g, 1, 2), jnp.swapaxes(beta, 1, 2)).astype(hn.dtype)
    out_c = rmsnorm(oc, norm_c) * jax.nn.silu(_heads(gc, C_HEADS, HEAD_DIM))
    log_f = jax.nn.log_sigmoid(jnp.swapaxes(fd + fd_bias, 1, 2).astype(jnp.float32))
    out_d = forgetting_attention(_heads(qd, D_HEADS, HEAD_DIM), _heads(kd, D_HEADS, HEAD_DIM),
                                 _heads(vd, D_HEADS, HEAD_DIM), log_f)
    return jnp.concatenate([_merge(out_c), _merge(out_d)], axis=-1) @ w_out


def hier_moe(x, w_group, b_group, w_router, b_router, w_gate, w_up, w_down):
    b, s, d = x.shape
    xt = x.reshape(-1, d)
    n = xt.shape[0]
    p_group = jax.nn.softmax((xt @ w_group + b_group).astype(jnp.float32), axis=-1)
    g_sel = jnp.argmax(p_group, axis=-1)
    p_top = jnp.take_along_axis(p_group, g_sel[:, None], axis=-1)
    e_logits = (xt @ w_router + b_router).astype(jnp.float32).reshape(n, N_GROUPS, EXPERTS_PER_GROUP)
    e_sel = jnp.take_along_axis(e_logits, g_sel[:, None, None], axis=1)[:, 0]
    top_v, top_i = lax.top_k(e_sel, TOP_K_IN_GROUP)
    w_k = jax.nn.softmax(top_v, axis=-1) * p_top
    e_idx = g_sel[:, None] * EXPERTS_PER_GROUP + top_i
    combine = jnp.sum(jax.nn.one_hot(e_idx, N_EXPERTS, dtype=jnp.float32) * w_k[..., None], axis=1).astype(x.dtype)
    y = jnp.zeros_like(xt)
    for e in range(N_EXPERTS):
        he = jax.nn.silu(xt @ w_gate[e]) * (xt @ w_up[e])
        y = y + combine[:, e:e + 1] * (he @ w_down[e])
    return y.reshape(b, s, d)


def setup_inputs(seed: int = 0) -> dict:
    key = jax.random.key(seed)
    keys = iter(jax.random.split(key, 48))

    def nrm(shape, scale):
        return jax.random.normal(next(keys), shape, jnp.float32) * scale

    def unif(shape, lo, hi):
        return jax.random.uniform(next(keys), shape, jnp.float32, lo, hi)

    ne = (DEPTH + 1) // 2
    no = DEPTH // 2
    dt = jnp.exp(unif((no, C_HEADS), math.log(1e-3), math.log(1e-1)))
    return {
        'x': nrm((BATCH, SEQ, D_MODEL), 1.0),
        'p': nrm((DEPTH, BATCH, SEQ, PLE_DIM), 1.0),
        'norm_mix': 1.0 + nrm((DEPTH, D_MODEL), 0.02),
        'norm_ffn': 1.0 + nrm((DEPTH, D_MODEL), 0.02),
        'norm_final': 1.0 + nrm((D_MODEL,), 0.02),
        'ab_w_in': nrm((ne, D_MODEL, EVEN_IN), D_MODEL ** -0.5),
        'ab_w_out': nrm((ne, MIX_WIDTH, D_MODEL), MIX_WIDTH ** -0.5),
        'a_lam_q1': nrm((ne, A_HALF), 0.1),
        'a_lam_k1': nrm((ne, A_HALF), 0.1),
        'a_lam_q2': nrm((ne, A_HALF), 0.1),
        'a_lam_k2': nrm((ne, A_HALF), 0.1),
        'a_subln': 1.0 + nrm((ne, HEAD_DIM), 0.02),
        'b_conv': nrm((ne, CONV_W, 2 * B_W), CONV_W ** -0.5),
        'b_igate_bias': nrm((ne, B_HEADS), 0.1),
        'b_fgate_bias': jnp.linspace(3.0, 6.0, B_HEADS)[None, :] + nrm((ne, B_HEADS), 0.1),
        'b_norm': 1.0 + nrm((ne, HEAD_DIM), 0.02),
        'cd_w_in': nrm((no, D_MODEL, ODD_IN), D_MODEL ** -0.5),
        'cd_w_out': nrm((no, MIX_WIDTH, D_MODEL), MIX_WIDTH ** -0.5),
        'c_conv': nrm((no, CONV_W, 3 * C_W), CONV_W ** -0.5),
        'c_a_log': jnp.log(unif((no, C_HEADS), 1.0, 16.0)),
        'c_dt_bias': dt + jnp.log(-jnp.expm1(-dt)),
        'c_norm': 1.0 + nrm((no, HEAD_DIM), 0.02),
        'd_fgate_bias': unif((no, D_HEADS), 2.0, 5.0),
        'moe_w_group': nrm((DEPTH, D_MODEL, N_GROUPS), D_MODEL ** -0.5),
        'moe_b_group': nrm((DEPTH, N_GROUPS), 0.01),
        'moe_w_router': nrm((DEPTH, D_MODEL, N_EXPERTS), D_MODEL ** -0.5),
        'moe_b_router': nrm((DEPTH, N_EXPERTS), 0.01),
        'moe_w_gate': nrm((DEPTH, N_EXPERTS, D_MODEL, EXPERT_FF), D_MODEL ** -0.5),
        'moe_w_up': nrm((DEPTH, N_EXPERTS, D_MODEL, EXPERT_FF), D_MODEL ** -0.5),
        'moe_w_down': nrm((DEPTH, N_EXPERTS, EXPERT_FF, D_MODEL), EXPERT_FF ** -0.5),
        'ple_w_gate': nrm((DEPTH, D_MODEL, D_MODEL), D_MODEL ** -0.5),
        'ple_w_proj': nrm((DEPTH, PLE_DIM, D_MODEL), PLE_DIM ** -0.5),
    }


def reference(x, p, norm_mix, norm_ffn, norm_final,
              ab_w_in, ab_w_out, a_lam_q1, a_lam_k1, a_lam_q2, a_lam_k2, a_subln,
              b_conv, b_igate_bias, b_fgate_bias, b_norm,
              cd_w_in, cd_w_out, c_conv, c_a_log, c_dt_bias, c_norm, d_fgate_bias,
              moe_w_group, moe_b_group, moe_w_router, moe_b_router, moe_w_gate, moe_w_up, moe_w_down,
              ple_w_gate, ple_w_proj):
    h = x
    for i in range(DEPTH):
        j = i // 2
        hn = rmsnorm(h, norm_mix[i])
        if i % 2 == 0:
            mix = even_mixer(hn, ab_w_in[j], ab_w_out[j], a_lam_q1[j], a_lam_k1[j], a_lam_q2[j], a_lam_k2[j],
                             a_subln[j], b_conv[j], b_igate_bias[j], b_fgate_bias[j], b_norm[j], i)
        else:
            mix = odd_mixer(hn, cd_w_in[j], cd_w_out[j], c_conv[j], c_a_log[j], c_dt_bias[j], c_norm[j],
                            d_fgate_bias[j])
        h = h + mix
        h = h + hier_moe(rmsnorm(h, norm_ffn[i]), moe_w_group[i], moe_b_group[i], moe_w_router[i],
                         moe_b_router[i], moe_w_gate[i], moe_w_up[i], moe_w_down[i])
        h = h + jax.nn.sigmoid(h @ ple_w_gate[i]) * (p[i] @ ple_w_proj[i])
    return rmsnorm(h, norm_final)
```

```python
import numpy as np
from contextlib import ExitStack
import concourse.bass as bass
import concourse.mybir as mybir
from concourse.bass_utils import run_bass_kernel_spmd

F32 = mybir.dt.float32
BF16 = mybir.dt.bfloat16
AF = mybir.ActivationFunctionType
ALU = mybir.AluOpType
AX = mybir.AxisListType

D = 1024
NCORES = 8
RMS_EPS = 1e-6


class Res:
    __slots__ = ("name", "lw", "rd", "dsem", "dcnt")

    def __init__(self, name):
        self.name = name
        self.lw = None
        self.rd = {}
        self.dsem = None
        self.dcnt = 0


class Sched:
    ENGS = ("pe", "act", "dve", "pool", "sp")

    def __init__(self, nc, es):
        self.nc = nc
        self.es = es
        self.prog = {e: [] for e in self.ENGS}
        self.cnt = {e: 0 for e in self.ENGS}
        self.sems = {}
        for e in self.ENGS:
            self.sems[e] = es.enter_context(nc.semaphore("s_" + e))
        self.waited = {e: {} for e in self.ENGS}
        self.nsem = 0
        self.n_inst = 0
        self.n_wait = 0
        self.dcount = {}

    def new_sem(self, name):
        k = "d_" + name
        if k not in self.sems:
            self.nsem += 1
            self.sems[k] = self.es.enter_context(self.nc.semaphore(k))
            self.dcount[k] = 0
        return k

    def barrier(self):
        for e in self.ENGS:
            for f in self.ENGS:
                if f != e:
                    self._wait(e, f, self.cnt[f])
            for key, c in self.dcount.items():
                self._wait(e, key, c)
        for e in self.ENGS:
            if e != "pe":
                self._wait(e, e, self.cnt[e])

    def _wait(self, eng, key, val):
        if val <= 0:
            return
        if key == eng and eng == "pe":
            return
        w = self.waited[eng]
        if w.get(key, 0) >= val:
            return
        w[key] = val
        self.prog[eng].append(("w", key, val))
        self.n_wait += 1

    def _deps(self, eng, reads, writes):
        for r in reads:
            if r.lw is not None:
                self._wait(eng, r.lw[0], r.lw[1])
        for w in writes:
            if w.lw is not None:
                self._wait(eng, w.lw[0], w.lw[1])
            for k, v in w.rd.items():
                self._wait(eng, k, v)

    def op(self, eng, fn, reads=(), writes=()):
        self._deps(eng, reads, writes)
        self.cnt[eng] += 1
        c = self.cnt[eng]
        self.prog[eng].append(("i", fn, eng, 1))
        for r in reads:
            if r.rd.get(eng, 0) < c:
                r.rd[eng] = c
        for w in writes:
            w.lw = (eng, c)
            w.rd = {}
        self.n_inst += 1

    def dma(self, q, fn, reads=(), writes=(), sem_res=None, inc=16):
        self._deps(q, reads, writes)
        sr = sem_res if sem_res is not None else (writes[0] if writes else reads[0])
        if sr.dsem is None:
            sr.dsem = self.new_sem(sr.name)
        key = sr.dsem
        self.dcount[key] += inc
        c = self.dcount[key]
        sr.dcnt = c
        self.prog[q].append(("i", fn, key, inc))
        for r in reads:
            if r.rd.get(key, 0) < c:
                r.rd[key] = c
        for w in writes:
            w.lw = (key, c)
            w.rd = {}
        self.n_inst += 1

    def wait_all(self, eng, ress):
        for r in ress:
            if r.lw is not None:
                self._wait(eng, r.lw[0], r.lw[1])
            for k, v in r.rd.items():
                self._wait(eng, k, v)

    def emit(self):
        nc = self.nc
        sems = self.sems
        prog = self.prog

        def run(engh, lst):
            for it in lst:
                if it[0] == "w":
                    engh.wait_ge(sems[it[1]], it[2])
                else:
                    it[1](engh).then_inc(sems[it[2]], it[3])

        with nc.Block() as block:
            @block.tensor
            def _(e):
                run(e, prog["pe"])

            @block.scalar
            def _(e):
                run(e, prog["act"])

            @block.vector
            def _(e):
                run(e, prog["dve"])

            @block.gpsimd
            def _(e):
                run(e, prog["pool"])

            @block.sync
            def _(e):
                run(e, prog["sp"])


class Tile:
    def __init__(self, t, name):
        self.t = t
        self.r = Res(name)

    def __getitem__(self, k):
        return self.t[k]


class KB:
    def __init__(self):
        self.nc = bass.Bass("TRN2", target_bir_lowering=False)
        self.es = ExitStack()
        self.S = Sched(self.nc, self.es)
        self.tes = ExitStack()
        self.pfx = ""
        self.in_names = []

    def new_section(self, pfx):
        self.S.barrier()
        self.tes.close()
        self.tes = ExitStack()
        self.pfx = pfx

    def din(self, name, shape, dt=F32):
        self.in_names.append(self.pfx + name)
        return self.nc.dram_tensor(self.pfx + name, list(shape), dt, kind="ExternalInput").ap()

    def dout(self, name, shape, dt=F32):
        return self.nc.dram_tensor(self.pfx + name, list(shape), dt, kind="ExternalOutput").ap()

    def dint(self, name, shape, dt=F32):
        return self.nc.dram_tensor(name, list(shape), dt).ap()

    def sb(self, name, shape, dt=F32):
        return Tile(self.tes.enter_context(self.nc.sbuf_tensor(self.pfx + name, list(shape), dt)), self.pfx + name)

    def ps(self, name, shape, dt=F32):
        return Tile(self.tes.enter_context(self.nc.psum_tensor(self.pfx + name, list(shape), dt)), self.pfx + name)

    def op(self, eng, fn, reads, writes):
        self.S.op(eng, fn, [x.r for x in reads], [x.r for x in writes])

    def load(self, out_ap, in_ap, wt, q="sp", reads=()):
        self.S.dma(q, lambda e: e.dma_start(out=out_ap, in_=in_ap), [x if isinstance(x, Res) else x.r for x in reads], [wt.r])

    def store(self, out_ap, in_ap, rt, ores, q="sp"):
        for kk, vv in ores.rd.items():
            self.S._wait(q, kk, vv)
        self.S.dma(q, lambda e: e.dma_start(out=out_ap, in_=in_ap), [rt.r], [], sem_res=ores)
        ores.lw = (ores.dsem, self.S.dcount[ores.dsem])

    def collective(self, kind, op, in_ap, out_ap, in_res, out_res, groups):
        import os
        if os.environ.get("MK_NOCC"):
            return
        self.S.dma("pool", lambda e: e.collective_compute(kind, op, replica_groups=groups, ins=[in_ap], outs=[out_ap]),
                   [in_res], [out_res], inc=1)

    def mm(self, out_ap, lhsT, rhs, start, stop, reads, wt):
        self.op("pe", lambda e: e.matmul(out_ap, lhsT, rhs, start=start, stop=stop), reads, [wt])

    def tr(self, out_ap, in_ap, ident_ap, reads, wt):
        self.op("pe", lambda e: e.transpose(out_ap, in_ap, ident_ap), reads, [wt])

    def finish(self, out_res_list):
        self.S.wait_all("sp", out_res_list)
        self.S.emit()
        self.tes.close()
        self.es.close()
        return self.nc


def build_ffn(T, final, SBT=1024, k=None, io=None):
    NB = T // 512
    NSB = max(1, T // SBT)
    BPS = NB // NSB
    standalone = k is None
    c8 = lambda ap, tok: ap[:, tok].rearrange("(c p) t -> p c t", p=128)
    if standalone:
        k = KB()
        hT = k.din("hT", [D, T])
        p0T = k.din("p0T", [D, T])
        p1T = k.din("p1T", [D, T])
        io = {"h": lambda tok: c8(hT, tok), "h_res": [], "pins": [lambda tok: c8(p0T, tok), lambda tok: c8(p1T, tok)], "pin_res": []}
    pT = k.din("pT", [256, T])
    gain_d = k.din("gain", [128, 8])
    gfin_d = k.din("gfin", [128, 8])
    wr_d = k.din("wr", [128, 8 * 20])
    br_d = k.din("br", [1, 20])
    wg_d = k.din("wg", [16, 128, 4096])
    wu_d = k.din("wu", [16, 128, 4096])
    wd_d = k.din("wd", [16, 128, 4096])
    plg_d = k.din("plg", [D, D])
    plp_d = k.din("plp", [256, D])
    ident_d = k.din("ident", [128, 128])
    sel_d = k.din("sel", [16, 16 * 128])
    if standalone:
        outT = k.dout("outT", [D, T])
        ores = Res("out")
        io["out"] = lambda tok: c8(outT, tok)
        io["out_res"] = ores
    ores = io["out_res"]

    acc = k.sb("acc", [128, BPS, 8, 512])
    hn16 = k.sb("hn16", [128, BPS, 8, 512], BF16)
    big32 = k.sb("big32", [128, 8, 512])
    combT = k.sb("combT", [16, BPS * 512])
    wbuf = [k.sb("wbuf%d" % i, [128, 12288], BF16) for i in range(2)]
    stage = [k.sb("stage%d" % i, [128, 2048]) for i in range(3)]
    p16 = k.sb("p16", [128, 2, 512], BF16)
    h2b = k.sb("h2b", [128, 8, 512], BF16)
    sg = [k.sb("sg%d" % i, [128, 512]) for i in range(2)]
    tt = [k.sb("tt%d" % i, [128, 512]) for i in range(2)]
    he = [k.sb("he%d" % i, [128, 4, 512], BF16) for i in range(2)]
    sq = [k.sb("sq%d" % i, [128, 512], BF16) for i in range(2)]
    rstd = k.sb("rstd", [128, 512])
    gain = k.sb("gain_s", [128, 8])
    gfin = k.sb("gfin_s", [128, 8])
    wr = k.sb("wr_s", [128, 8 * 20])
    br = k.sb("br_s", [128, 20])
    ident = k.sb("ident_s", [128, 128])
    sel = k.sb("sel_s", [16, 16 * 128])
    ones16 = k.sb("ones16", [128, 128], BF16)
    rt = {n: k.sb("rt_" + n, [128, w]) for n, w in
          [("L", 20), ("gmax", 1), ("ngmax", 1), ("gm", 4), ("ex", 4), ("se", 1), ("ptop", 1), ("t44", 16), ("esel", 4),
           ("m1", 1), ("k1", 4), ("e2", 4), ("m2", 1), ("k2", 4), ("d", 1), ("ed", 1), ("w1", 1), ("w2", 1), ("t1", 4),
           ("cl", 4), ("comb", 16)]}
    ps_gu = [k.ps("ps_gu%d" % i, [128, 512]) for i in range(4)]
    ps_y = [k.ps("ps_y%d" % i, [128, 512]) for i in range(2)]
    ps_c = k.ps("ps_c", [128, 512])
    ps_m = k.ps("ps_m", [128, 512])

    k.load(gain[:], gain_d[:, :], gain)
    k.load(gfin[:], gfin_d[:, :], gfin)
    k.load(wr[:], wr_d[:, :], wr)
    k.load(br[:], br_d.partition_broadcast(128), br)
    k.load(ident[:], ident_d[:, :], ident)
    k.load(sel[:], sel_d[:, :], sel)
    k.op("dve", lambda e: e.memset(ones16[:], 1.0), [], [ones16])
    epsb = k.sb("epsb", [128, 1])
    k.op("dve", lambda e: e.memset(epsb[:], float(D * RMS_EPS)), [], [epsb])
    k.op("dve", lambda e: e.tensor_scalar_mul(out=gain[:], in0=gain[:], scalar1=32.0), [gain], [gain])
    k.op("dve", lambda e: e.tensor_scalar_mul(out=gfin[:], in0=gfin[:], scalar1=32.0), [gfin], [gfin])

    stage_i = [0]

    def load_cast(dst_ap, src_ap, dst_tile, shape3=None):
        st = stage[stage_i[0] % 3]
        stage_i[0] += 1
        if shape3 is None:
            k.load(st[:], src_ap, st)
            k.op("act", lambda e: e.copy(out=dst_ap, in_=st[:]), [st], [dst_tile])
        else:
            a, b = shape3
            k.load(st[:].rearrange("p (a b) -> p a b", a=a), src_ap, st)
            k.op("act", lambda e: e.copy(out=dst_ap, in_=st[:]), [st], [dst_tile])

    def rmsnorm_stats(src_chunks, src_tile):
        for c in range(8):
            s = sq[c % 2]
            k.op("act", (lambda s, c: lambda e: e.activation(out=s[:], in_=src_chunks(c), func=AF.Square))(s, c),
                 [src_tile], [s])
            k.mm(ps_m[:], ones16[:], s[:], c == 0, c == 7, [ones16, s], ps_m)
        k.op("act", lambda e: e.activation(out=rstd[:], in_=ps_m[:], func=AF.Sqrt, bias=epsb[:, 0:1], scale=1.0),
             [ps_m, epsb], [rstd])
        k.op("dve", lambda e: e.reciprocal(out=rstd[:], in_=rstd[:]), [rstd], [rstd])

    for sb_i in range(NSB):
        for j in range(BPS):
            tok = slice((sb_i * BPS + j) * 512, (sb_i * BPS + j + 1) * 512)
            k.load(acc[:, j], io["h"](tok), acc, reads=io["h_res"])
            for src in io["pins"]:
                k.load(big32[:], src(tok), big32, reads=io["pin_res"])
                k.op("dve", (lambda j: lambda e: e.tensor_tensor(out=acc[:, j], in0=acc[:, j], in1=big32[:], op=ALU.add))(j),
                     [acc, big32], [acc])
            rmsnorm_stats(lambda c, j=j: acc[:, j, c], acc)
            for c in range(8):
                k.op("dve", (lambda j, c: lambda e: e.scalar_tensor_tensor(
                    out=big32[:, c], in0=acc[:, j, c], scalar=gain[:, c:c + 1], in1=rstd[:], op0=ALU.mult, op1=ALU.mult))(j, c),
                    [acc, gain, rstd], [big32])
            k.op("act", (lambda j: lambda e: e.copy(out=hn16[:, j], in_=big32[:]))(j), [big32], [hn16])
            for t4 in range(4):
                for c in range(8):
                    k.mm(ps_m[:, 0:20], big32[:, c, t4 * 128:(t4 + 1) * 128], wr[:, c * 20:(c + 1) * 20], c == 0, c == 7,
                         [big32, wr], ps_m)
                R = rt
                V = "dve"
                k.op(V, lambda e: e.tensor_tensor(out=R["L"][:], in0=ps_m[:, 0:20], in1=br[:], op=ALU.add), [ps_m, br], [R["L"]])
                k.op(V, lambda e: e.reduce_max(out=R["gmax"][:], in_=R["L"][:, 0:4], axis=AX.X), [R["L"]], [R["gmax"]])
                k.op(V, lambda e: e.tensor_tensor(out=R["gm"][:], in0=R["L"][:, 0:4], in1=R["gmax"][:, 0:1].to_broadcast([128, 4]),
                                                  op=ALU.is_ge), [R["L"], R["gmax"]], [R["gm"]])
                k.op(V, lambda e: e.tensor_scalar_mul(out=R["ngmax"][:], in0=R["gmax"][:], scalar1=-1.0), [R["gmax"]], [R["ngmax"]])
                k.op("act", lambda e: e.activation(out=R["ex"][:], in_=R["L"][:, 0:4], func=AF.Exp, bias=R["ngmax"][:, 0:1],
                                                   scale=1.0, accum_out=R["se"][:]), [R["L"], R["ngmax"]], [R["ex"], R["se"]])
                k.op(V, lambda e: e.reciprocal(out=R["ptop"][:], in_=R["se"][:]), [R["se"]], [R["ptop"]])
                k.op(V, lambda e: e.tensor_tensor(
                    out=R["t44"][:].rearrange("p (g x) -> p g x", g=4),
                    in0=R["L"][:, 4:20].rearrange("p (g x) -> p g x", g=4),
                    in1=R["gm"][:].unsqueeze(2).to_broadcast([128, 4, 4]), op=ALU.mult), [R["L"], R["gm"]], [R["t44"]])
                k.op(V, lambda e: e.tensor_reduce(out=R["esel"][:], in_=R["t44"][:].rearrange("p (g x) -> p x g", g=4),
                                                  axis=AX.X, op=ALU.add), [R["t44"]], [R["esel"]])
                k.op(V, lambda e: e.reduce_max(out=R["m1"][:], in_=R["esel"][:], axis=AX.X), [R["esel"]], [R["m1"]])
                k.op(V, lambda e: e.tensor_tensor(out=R["k1"][:], in0=R["esel"][:], in1=R["m1"][:, 0:1].to_broadcast([128, 4]),
                                                  op=ALU.is_ge), [R["esel"], R["m1"]], [R["k1"]])
                k.op(V, lambda e: e.scalar_tensor_tensor(out=R["e2"][:], in0=R["k1"][:], scalar=-1e30, in1=R["esel"][:],
                                                         op0=ALU.mult, op1=ALU.add), [R["k1"], R["esel"]], [R["e2"]])
                k.op(V, lambda e: e.reduce_max(out=R["m2"][:], in_=R["e2"][:], axis=AX.X), [R["e2"]], [R["m2"]])
                k.op(V, lambda e: e.tensor_tensor(out=R["k2"][:], in0=R["e2"][:], in1=R["m2"][:, 0:1].to_broadcast([128, 4]),
                                                  op=ALU.is_ge), [R["e2"], R["m2"]], [R["k2"]])
                k.op(V, lambda e: e.tensor_tensor(out=R["d"][:], in0=R["m2"][:], in1=R["m1"][:], op=ALU.subtract),
                     [R["m1"], R["m2"]], [R["d"]])
                k.op("act", lambda e: e.activation(out=R["ed"][:], in_=R["d"][:], func=AF.Exp), [R["d"]], [R["ed"]])
                k.op(V, lambda e: e.tensor_scalar_add(out=R["w1"][:], in0=R["ed"][:], scalar1=1.0), [R["ed"]], [R["w1"]])
                k.op(V, lambda e: e.reciprocal(out=R["w1"][:], in_=R["w1"][:]), [R["w1"]], [R["w1"]])
                k.op(V, lambda e: e.tensor_tensor(out=R["w1"][:], in0=R["w1"][:], in1=R["ptop"][:], op=ALU.mult),
                     [R["w1"], R["ptop"]], [R["w1"]])
                k.op(V, lambda e: e.tensor_tensor(out=R["w2"][:], in0=R["w1"][:], in1=R["ed"][:], op=ALU.mult),
                     [R["w1"], R["ed"]], [R["w2"]])
                k.op(V, lambda e: e.tensor_scalar(out=R["t1"][:], in0=R["k1"][:], scalar1=R["w1"][:, 0:1], scalar2=None,
                                                  op0=ALU.mult), [R["k1"], R["w1"]], [R["t1"]])
                k.op(V, lambda e: e.scalar_tensor_tensor(out=R["cl"][:], in0=R["k2"][:], scalar=R["w2"][:, 0:1], in1=R["t1"][:],
                                                         op0=ALU.mult, op1=ALU.add), [R["k2"], R["w2"], R["t1"]], [R["cl"]])
                k.op(V, lambda e: e.tensor_tensor(
                    out=R["comb"][:].rearrange("p (g x) -> p g x", g=4),
                    in0=R["gm"][:].unsqueeze(2).to_broadcast([128, 4, 4]),
                    in1=R["cl"][:].unsqueeze(1).to_broadcast([128, 4, 4]), op=ALU.mult), [R["gm"], R["cl"]], [R["comb"]])
                k.tr(ps_m[0:16, 128:256], R["comb"][:], ident[:], [R["comb"], ident], ps_m)
                k.op(V, (lambda j, t4: lambda e: e.tensor_copy(out=combT[:, j * 512 + t4 * 128: j * 512 + (t4 + 1) * 128],
                                                               in_=ps_m[0:16, 128:256]))(j, t4), [ps_m], [combT])

        def load_expert(e):
            wb = wbuf[e % 2]
            for mi, src in enumerate((wg_d, wu_d, wd_d)):
                for half in range(2):
                    load_cast(wb[:, mi * 4096 + half * 2048: mi * 4096 + (half + 1) * 2048], src[e, :, half * 2048:(half + 1) * 2048], wb)

        load_expert(0)
        gi = 0
        yi = 0
        for e in range(16):
            if e + 1 < 16:
                load_expert(e + 1)
            wb = wbuf[e % 2]
            for j in range(BPS):
                k.mm(ps_c[:], sel[:, e * 128:(e + 1) * 128], combT[:, j * 512:(j + 1) * 512], True, True, [sel, combT], ps_c)
                hb = he[(e * BPS + j) % 2]
                for f in range(4):
                    pg = ps_gu[gi % 4]
                    pu = ps_gu[(gi + 1) % 4]
                    gi += 2
                    for c in range(8):
                        k.mm(pg[:], wb[:, c * 512 + f * 128: c * 512 + (f + 1) * 128], hn16[:, j, c], c == 0, c == 7, [wb, hn16], pg)
                    for c in range(8):
                        k.mm(pu[:], wb[:, 4096 + c * 512 + f * 128: 4096 + c * 512 + (f + 1) * 128], hn16[:, j, c], c == 0, c == 7,
                             [wb, hn16], pu)
                    s_ = sg[f % 2]
                    t_ = tt[f % 2]
                    k.op("act", (lambda s_, pg: lambda e: e.activation(out=s_[:], in_=pg[:], func=AF.Silu))(s_, pg), [pg], [s_])
                    k.op("dve", (lambda t_, s_, pu: lambda e: e.tensor_tensor(out=t_[:], in0=s_[:], in1=pu[:], op=ALU.mult))(t_, s_, pu),
                         [s_, pu], [t_])
                    k.op("dve", (lambda hb, f, t_: lambda e: e.tensor_tensor(out=hb[:, f], in0=t_[:], in1=ps_c[:], op=ALU.mult))(hb, f, t_),
                         [t_, ps_c], [hb])
                for c in range(8):
                    py = ps_y[yi % 2]
                    yi += 1
                    for f in range(4):
                        k.mm(py[:], wb[:, 8192 + f * 1024 + c * 128: 8192 + f * 1024 + (c + 1) * 128], hb[:, f], f == 0, f == 3,
                             [wb, hb], py)
                    k.op("dve", (lambda j, c, py: lambda e: e.tensor_tensor(out=acc[:, j, c], in0=acc[:, j, c], in1=py[:], op=ALU.add))(j, c, py),
                         [acc, py], [acc])

        wp = wbuf[0]
        for q4 in range(4):
            load_cast(wp[:, q4 * 2048:(q4 + 1) * 2048],
                      plg_d[q4 * 256:(q4 + 1) * 256, :].rearrange("(k p) n -> p k n", p=128), wp, (2, 1024))
        load_cast(wp[:, 8192:10240], plp_d[:, :].rearrange("(k p) n -> p k n", p=128), wp, (2, 1024))
        for j in range(BPS):
            tok = slice((sb_i * BPS + j) * 512, (sb_i * BPS + j + 1) * 512)
            st = stage[stage_i[0] % 3]
            stage_i[0] += 1
            k.load(st[:, 0:1024].rearrange("p (a b) -> p a b", a=2), pT[:, tok].rearrange("(c p) t -> p c t", p=128), st)
            k.op("act", (lambda st: lambda e: e.copy(out=p16[:], in_=st[:, 0:1024]))(st), [st], [p16])
            k.op("act", (lambda j: lambda e: e.copy(out=h2b[:], in_=acc[:, j]))(j), [acc], [h2b])
            for c in range(8):
                pg = ps_gu[gi % 4]
                pu = ps_gu[(gi + 1) % 4]
                gi += 2
                for kk in range(8):
                    k.mm(pg[:], wp[:, kk * 1024 + c * 128: kk * 1024 + (c + 1) * 128], h2b[:, kk], kk == 0, kk == 7, [wp, h2b], pg)
                for kk in range(2):
                    k.mm(pu[:], wp[:, 8192 + kk * 1024 + c * 128: 8192 + kk * 1024 + (c + 1) * 128], p16[:, kk], kk == 0, kk == 1,
                         [wp, p16], pu)
                s_ = sg[c % 2]
                t_ = tt[c % 2]
                k.op("act", (lambda s_, pg: lambda e: e.activation(out=s_[:], in_=pg[:], func=AF.Sigmoid))(s_, pg), [pg], [s_])
                k.op("dve", (lambda t_, s_, pu: lambda e: e.tensor_tensor(out=t_[:], in0=s_[:], in1=pu[:], op=ALU.mult))(t_, s_, pu),
                     [s_, pu], [t_])
                k.op("dve", (lambda j, c, t_: lambda e: e.tensor_tensor(out=acc[:, j, c], in0=acc[:, j, c], in1=t_[:], op=ALU.add))(j, c, t_),
                     [acc, t_], [acc])
            if final:
                rmsnorm_stats(lambda c, j=j: acc[:, j, c], acc)
                for c in range(8):
                    k.op("dve", (lambda j, c: lambda e: e.scalar_tensor_tensor(
                        out=big32[:, c], in0=acc[:, j, c], scalar=gfin[:, c:c + 1], in1=rstd[:], op0=ALU.mult, op1=ALU.mult))(j, c),
                        [acc, gfin, rstd], [big32])
                k.store(io["out"](tok), big32[:], big32, ores)
            else:
                k.store(io["out"](tok), acc[:, j], acc, ores)
    if not standalone:
        return None, k
    nc = k.finish([ores])
    return nc, k


def ffn_consts():
    ident = np.eye(128, dtype=np.float32)
    sel = np.zeros((16, 16 * 128), np.float32)
    for e in range(16):
        sel[e, e * 128:(e + 1) * 128] = 1.0
    return ident, sel


def tile_w(w):
    w = np.asarray(w, np.float32)
    E, K_, N_ = w.shape
    return np.ascontiguousarray(w.reshape(E, K_ // 128, 128, N_).transpose(0, 2, 1, 3).reshape(E, 128, (K_ // 128) * N_))


def chunk_cols(v):
    return np.ascontiguousarray(np.asarray(v, np.float32).reshape(8, 128).T)


class Sub:
    def __init__(self, ap, name, res=None):
        self.ap = ap
        self.r = res if res is not None else Res(name)

    def __getitem__(self, k):
        return self.ap[k]


def _v(k, eng, meth, reads, writes, **kw):
    k.S.op(eng, lambda e: getattr(e, meth)(**kw), [x.r for x in reads], [x.r for x in writes])


def build_mix(S, odd, lam_init=0.2, skip_attn=False, skip_rec=False, stop=99, k=None, io=None):
    NB = S // 512
    NKB = S // 128
    standalone = k is None
    if standalone:
        k = KB()
    V = lambda *a, **kw: _v(k, *a, **kw)
    NFG = 10 if odd else 12
    NTG = 4 if odd else 6
    NG = 6 if odd else 4
    NCV = 6 if odd else 4
    if standalone:
        hT = k.din("hT", [D, S])
        io = {"h": lambda blk: hT[:, blk * 512:(blk + 1) * 512].rearrange("(c p) t -> p c t", p=128), "h_res": []}
    gain_d = k.din("gain", [128, 8])
    wf_d = k.din("wf", [D, NFG * 128])
    wt_d = k.din("wt", [D, NTG * 128])
    wgt_d = k.din("wgt", [128, 8 * NG])
    gb_d = k.din("gbias", [1, NG])
    wout_d = k.din("wout", [512, D])
    convw_d = k.din("convw", [128, NCV * 4])
    nrm_d = k.din("nrm", [1, 128])
    ident_d = k.din("ident", [128, 128])
    U_d = k.din("U", [128, 128])
    BD_d = k.din("BD", [128, 128])
    MBu_d = k.din("MBu", [128, 128])
    MBl_d = k.din("MBl", [128, 128])
    mask_d = k.din("masks", [4, 128, 512])
    if odd:
        alog_d = k.din("alog", [1, 2])
        Uf_d = k.din("Uf", [128, 128])
    else:
        lamv_d = k.din("lamv", [1, 256])
        subln_d = k.din("subln", [128, 1])
        cos_d = k.din("cosT", [128, S])
        sin_d = k.din("sinT", [128, S])
    if standalone:
        partT = k.dout("partT", [D, S])
        io["out"] = lambda blk: partT[:, blk * 512:(blk + 1) * 512].rearrange("(c p) t -> p c t", p=128)
        io["out_res"] = Res("out")
    ores = io["out_res"]

    x32 = k.sb("x32", [128, 8, 512])
    hn16 = k.sb("hn16", [128, 8, 512], BF16)
    wf16 = k.sb("wf16", [128, 8, NFG * 128], BF16)
    wt16 = k.sb("wt16", [128, 8, NTG * 128], BF16)
    wout16 = k.sb("wout16", [128, 4, D], BF16)
    wg32 = k.sb("wg32", [128, 8 * NG])
    gbias = k.sb("gbias_s", [128, NG])
    gain = k.sb("gain_s", [128, 8])
    convw = k.sb("convw_s", [128, NCV * 4])
    nrmrep = k.sb("nrmrep", [128, 128])
    ident = k.sb("ident_s", [128, 128])
    Um = k.sb("U_s", [128, 128])
    BDm = k.sb("BD_s", [128, 128])
    MBu = k.sb("MBu_s", [128, 128])
    MBl = k.sb("MBl_s", [128, 128])
    masks = k.sb("masks_s", [128, 4, 512], F32 if odd else BF16)
    ones16 = k.sb("ones16", [128, 128], BF16)
    ones32 = k.sb("ones32", [128, 128])
    epsb = k.sb("epsb", [128, 1])
    eps1 = k.sb("eps1", [128, 1])
    onec = k.sb("onec", [128, 1])
    sq = [k.sb("sq%d" % i, [128, 512], BF16) for i in range(2)]
    rstd = k.sb("rstd", [128, 512])
    Xt = rstd
    kcache = [k.sb("kc%d" % i, [128, S], BF16) for i in range(2)]
    vcache = [k.sb("vc%d" % i, [128, NKB, 128], BF16) for i in range(2)]
    qa = [k.sb("qa%d" % i, [128, 512], BF16) for i in range(2)]
    oT = k.sb("oT", [128, 4, 512], BF16)
    Et = [k.sb("E%d" % i, [128, 512], BF16) for i in range(3)]
    Rt = [k.sb("R%d" % i, [128, 512]) for i in range(2)]
    rz = k.sb("rz", [128, 512])
    cvin = [k.sb("cvin%d" % i, [128, 515]) for i in range(NCV)]
    cvo = [k.sb("cvo%d" % i, [128, 512]) for i in range(NCV)]
    vaug = [k.sb("vaug%d" % i, [128, 4, 130] if not odd else [128, 2]) for i in range(2)]
    gtok = [k.sb("gtok%d" % i, [128, 4, 128]) for i in range(2)]
    gts = k.sb("gts", [128, 4, NG])
    lf = k.sb("lf", [128, 4, NG])
    gt2 = k.sb("gt2", [128, 4, NG])
    Sst = [[k.sb("S%d_%d" % (i, j), [128, 130]) for j in range(2)] for i in range(2)]
    qhat = [k.sb("qhat%d" % i, [128, 2, 128]) for i in range(2)]
    sm = {n: k.sb("sm_" + n, [128, w]) for n, w in
          [("lfb", 128), ("crep", 128), ("bc", 8), ("rcol", 1), ("e1", 1), ("Dm", 128), ("G", 128), ("ecr", 128), ("AT", 128),
           ("k2", 128), ("dm", 1), ("hh", 128), ("junk", 128), ("ssq", 1), ("rs", 1), ("hn", 128), ("sgo", 128), ("ob", 128)] +
          ([("Dl", 128), ("Gl", 128), ("N", 128), ("M", 128), ("P", 128), ("Mk", 128), ("Y", 128), ("vb", 128), ("kp", 128),
            ("u", 128), ("wT0", 128), ("wT1", 128), ("vn", 128), ("ktok", 128), ("bcol", 1), ("bebc", 1), ("ebd", 1), ("kd", 128),
            ("tmpc", 1)] if odd else [])}
    if odd:
        alog = k.sb("alog_s", [128, 2])
        Ufm = k.sb("Uf_s", [128, 128])
        carry = k.sb("carry", [128, 2])
        ncum = [k.sb("ncum%d" % i, [128, NKB]) for i in range(2)]
        Rq = [k.sb("Rq%d" % i, [1, 512]) for i in range(2)]
    else:
        lamv = k.sb("lamv_s", [128, 256])
        lamt = k.sb("lamt", [128, 8])
        subc = k.sb("subc", [128, 1])
        cost = k.sb("cost", [128, 512])
        sint = k.sb("sint", [128, 512])
        rt1 = Rt[0]
        rt2 = Rt[1]
    pj = [k.ps("pj%d" % i, [128, 512]) for i in range(2)]
    pst = [k.ps("pst%d" % i, [128, 512]) for i in range(2)]
    po = k.ps("po", [128, 512])
    pz = k.ps("pz", [128, 512])
    pr0 = k.ps("pr0", [128, 512])
    pr1 = k.ps("pr1", [128, 512])
    pA = Sub(pr0.t[:, 0:128], "pA", pr0.r)
    pB = Sub(pr0.t[:, 128:136], "pB", pr0.r)
    pC = Sub(pr1.t[:, 0:128], "pC", pr1.r)
    pH = Sub(pr1.t[:, 128:256], "pH", pr1.r)
    pD = Sub(pst[0].t[:, 0:128], "pD", pst[0].r)
    pG = Sub(pst[0].t[:, 128:256], "pG", pst[0].r)
    pF = Sub(pst[1].t[:, 0:130], "pF", pst[1].r)
    pE = Sub(po.t[:, 0:130], "pE", po.r)

    for t_, d_ in ((gain, gain_d), (wg32, wgt_d), (convw, convw_d), (ident, ident_d), (Um, U_d), (BDm, BD_d), (MBu, MBu_d), (MBl, MBl_d)):
        k.load(t_[:], d_[:, :], t_)
    k.load(gbias[:], gb_d.partition_broadcast(128), gbias)
    k.load(nrmrep[:], nrm_d.partition_broadcast(128), nrmrep)
    V("dve", "memset", [], [ones16], ap=ones16[:], constant=1.0)
    V("dve", "memset", [], [ones32], ap=ones32[:], constant=1.0)
    V("dve", "memset", [], [epsb], ap=epsb[:], constant=float(D * RMS_EPS))
    V("dve", "memset", [], [eps1], ap=eps1[:], constant=float(RMS_EPS))
    V("dve", "memset", [], [onec], ap=onec[:], constant=1.0)
    V("dve", "tensor_scalar_mul", [gain], [gain], out=gain[:], in0=gain[:], scalar1=32.0)
    for i in range(2):
        V("dve", "memset", [], [qhat[i]], ap=qhat[i][:], constant=0.0)
        V("dve", "memset", [], [vaug[i]], ap=vaug[i][:], constant=1.0)
        for j in range(2):
            V("dve", "memset", [], [Sst[i][j]], ap=Sst[i][j][:], constant=0.0)
    for c_ in cvin:
        V("dve", "memset", [], [c_], ap=c_[:], constant=0.0)
    V("dve", "memset", [], [lf], ap=lf[:], constant=0.0)
    if odd:
        k.load(alog[:], alog_d.partition_broadcast(128), alog)
        k.load(Ufm[:], Uf_d[:, :], Ufm)
        V("act", "activation", [alog], [alog], out=alog[:], in_=alog[:], func=AF.Exp)
        V("dve", "tensor_scalar_mul", [alog], [alog], out=alog[:], in0=alog[:], scalar1=-1.0)
        V("dve", "memset", [], [carry], ap=carry[:], constant=0.0)
    else:
        k.load(lamv[:], lamv_d.partition_broadcast(128), lamv)
        k.load(subc[:], subln_d[:, :], subc)
        V("dve", "tensor_scalar_mul", [subc], [subc], out=subc[:], in0=subc[:], scalar1=float(1.0 - lam_init))
        V("dve", "tensor_tensor", [lamv], [lamv], out=lamv[:, 0:64], in0=lamv[:, 0:64], in1=lamv[:, 64:128], op=ALU.mult)
        V("dve", "tensor_tensor", [lamv], [lamv], out=lamv[:, 128:192], in0=lamv[:, 128:192], in1=lamv[:, 192:256], op=ALU.mult)
        V("dve", "reduce_sum", [lamv], [lamt], out=lamt[:, 0:1], in_=lamv[:, 0:64], axis=AX.X)
        V("dve", "reduce_sum", [lamv], [lamt], out=lamt[:, 1:2], in_=lamv[:, 128:192], axis=AX.X)
        V("act", "activation", [lamt], [lamt], out=lamt[:, 2:4], in_=lamt[:, 0:2], func=AF.Exp)
        V("dve", "tensor_tensor", [lamt], [lamt], out=lamt[:, 4:5], in0=lamt[:, 3:4], in1=lamt[:, 2:3], op=ALU.subtract)
        V("dve", "tensor_scalar_add", [lamt], [lamt], out=lamt[:, 4:5], in0=lamt[:, 4:5], scalar1=float(-lam_init))
    k.load(x32[:, 0:4, :], mask_d.rearrange("j p t -> p j t"), x32)
    if odd:
        V("dve", "tensor_scalar", [x32], [masks], out=masks[:], in0=x32[:, 0:4, :], scalar1=-1.0, scalar2=30000.0, op0=ALU.add, op1=ALU.mult)
    else:
        V("act", "copy", [x32], [masks], out=masks[:], in_=x32[:, 0:4, :])

    def load_w(dst, dcols, src, rows0, nrows, ncols, col0):
        nk = nrows // 128
        st = x32[:].rearrange("p a b -> p (a b)")[:, 0:nk * ncols].rearrange("p (a b) -> p a b", a=nk)
        k.load(st, src[rows0:rows0 + nrows, col0:col0 + ncols].rearrange("(a p) n -> p a n", p=128), x32)
        V("act", "copy", [x32], [dst], out=dcols, in_=st)

    for g in range(NFG):
        for h2 in range(2):
            load_w(wf16, wf16[:, h2 * 4:(h2 + 1) * 4, g * 128:(g + 1) * 128], wf_d, h2 * 512, 512, 128, g * 128)
    for g in range(NTG):
        for h2 in range(2):
            load_w(wt16, wt16[:, h2 * 4:(h2 + 1) * 4, g * 128:(g + 1) * 128], wt_d, h2 * 512, 512, 128, g * 128)
    for hh_ in range(4):
        load_w(wout16, wout16[:, hh_:hh_ + 1, :], wout_d, hh_ * 128, 128, D, 0)

    pji = [0]

    def proj_fm(g):
        p = pj[pji[0] % 2]
        pji[0] += 1
        for c in range(8):
            k.mm(p[:], wf16[:, c, g * 128:(g + 1) * 128], hn16[:, c], c == 0, c == 7, [wf16, hn16], p)
        return p

    def proj_tm(g):
        p = pj[pji[0] % 2]
        pji[0] += 1
        for t4 in range(4):
            for c in range(8):
                k.mm(p[:, t4 * 128:(t4 + 1) * 128], hn16[:, c, t4 * 128:(t4 + 1) * 128], wt16[:, c, g * 128:(g + 1) * 128],
                     c == 0, c == 7, [wt16, hn16], p)
        return p

    def conv_silu(p, ci, blk):
        xi = cvin[ci]
        V("act", "copy", [p], [xi], out=xi[:, 3:515], in_=p[:])
        o = cvo[ci]
        V("dve", "tensor_scalar_mul", [xi, convw], [o], out=o[:], in0=xi[:, 0:512], scalar1=convw[:, ci * 4:ci * 4 + 1])
        for j in range(1, 4):
            V("dve", "scalar_tensor_tensor", [xi, convw, o], [o], out=o[:], in0=xi[:, j:j + 512],
              scalar=convw[:, ci * 4 + j:ci * 4 + j + 1], in1=o[:], op0=ALU.mult, op1=ALU.add)
        V("act", "activation", [o], [o], out=o[:], in_=o[:], func=AF.Silu)
        V("dve", "tensor_copy", [xi], [xi], out=xi[:, 0:3], in_=xi[:, 512:515])
        return o

    def l2n(o, scale):
        V("act", "activation", [o], [sq[0]], out=sq[0][:], in_=o[:], func=AF.Square)
        k.mm(pz[:], ones16[:], sq[0][:], True, True, [ones16, sq[0]], pz)
        V("act", "activation", [pz, eps1], [rz], out=rz[:], in_=pz[:], func=AF.Sqrt, bias=eps1[:, 0:1], scale=1.0)
        V("dve", "reciprocal", [rz], [rz], out=rz[:], in_=rz[:])
        V("dve", "scalar_tensor_tensor", [o, rz], [o], out=o[:], in0=o[:], scalar=float(scale), in1=rz[:], op0=ALU.mult, op1=ALU.mult)

    sti = [0]
    ei = [0]

    def attn(i, blk, qparts, scale, bias_rows=None):
        outs = []
        nkb = 4 * blk + 4
        for ci, psl in enumerate(qparts):
            for kb in range(nkb):
                st = pst[sti[0] % 2]
                sti[0] += 1
                k.mm(st[:], kcache[i][psl, kb * 128:(kb + 1) * 128], qa[i][psl, :], True, bias_rows is None, [kcache[i], qa[i]], st)
                E = Et[ei[0] % 3]
                ei[0] += 1
                if bias_rows is not None:
                    nc_, rq_ = bias_rows
                    k.mm(st[:], ones32[0:1, 0:128], rq_[0:1, :], False, True, [ones32, rq_], st)
                    if kb >= 4 * blk:
                        V("dve", "tensor_tensor", [st, masks], [rz], out=rz[:], in0=st[:], in1=masks[:, kb - 4 * blk, :], op=ALU.add)
                        V("act", "activation", [rz, nc_], [E], out=E[:], in_=rz[:], func=AF.Exp, bias=nc_[:, kb:kb + 1], scale=float(scale))
                    else:
                        V("act", "activation", [st, nc_], [E], out=E[:], in_=st[:], func=AF.Exp, bias=nc_[:, kb:kb + 1], scale=float(scale))
                else:
                    V("act", "activation", [st], [E], out=E[:], in_=st[:], func=AF.Exp, scale=float(scale))
                    if kb >= 4 * blk:
                        V("dve", "tensor_tensor", [E, masks], [E], out=E[:], in0=E[:], in1=masks[:, kb - 4 * blk, :], op=ALU.mult)
                k.mm(po[:], vcache[i][:, kb, :], E[:], kb == 0, kb == nkb - 1, [vcache[i], E], po)
                k.mm(pz[:], ones16[:], E[:], kb == 0, kb == nkb - 1, [ones16, E], pz)
            R = Rt[ci]
            V("dve", "reciprocal", [pz], [rz], out=rz[:], in_=pz[:])
            V("dve", "tensor_tensor", [po, rz], [R], out=R[:], in0=po[:], in1=rz[:], op=ALU.mult)
            outs.append(R)
        return outs

    def decay_prep(lfcol, lf2, i):
        V("dve", "tensor_scalar_mul", [ones32, lf, gts], [sm["lfb"]], out=sm["lfb"][:], in0=ones32[:], scalar1=lfcol)
        k.mm(pA[:], sm["lfb"][:], Um[:], True, True, [sm["lfb"], Um], pA)
        V("act", "copy", [pA], [sm["crep"]], out=sm["crep"][:], in_=pA[:])
        V("dve", "tensor_tensor", [sm["crep"], ident], [sm["junk"]], out=sm["junk"][:], in0=sm["crep"][:], in1=ident[:], op=ALU.mult)
        V("dve", "reduce_sum", [sm["junk"]], [sm["bc"]], out=sm["bc"][:, 0:1], in_=sm["junk"][:], axis=AX.X)
        V("dve", "tensor_copy", [sm["crep"]], [sm["bc"]], out=sm["bc"][0:64, 1:2], in_=sm["crep"][0:64, 63:64])
        V("dve", "tensor_copy", [sm["crep"]], [sm["bc"]], out=sm["bc"][64:128, 1:2], in_=sm["crep"][64:128, 127:128])
        V("act", "activation", [sm["crep"]], [sm["ecr"]], out=sm["ecr"][:], in_=sm["crep"][:], func=AF.Exp)

    def post_out(i, t4, src_ps, gate_func):
        V("act", "activation", [sm["hh"]], [sm["junk"], sm["ssq"]], out=sm["junk"][:], in_=sm["hh"][:], func=AF.Square,
          accum_out=sm["ssq"][:])
        V("act", "activation", [sm["ssq"], eps1], [sm["rs"]], out=sm["rs"][:], in_=sm["ssq"][:], func=AF.Sqrt, bias=eps1[:, 0:1],
          scale=1.0 / 128.0)
        V("dve", "reciprocal", [sm["rs"]], [sm["rs"]], out=sm["rs"][:], in_=sm["rs"][:])
        V("dve", "scalar_tensor_tensor", [sm["hh"], sm["rs"], nrmrep], [sm["hn"]], out=sm["hn"][:], in0=sm["hh"][:],
          scalar=sm["rs"][:, 0:1], in1=nrmrep[:], op0=ALU.mult, op1=ALU.mult)
        V("act", "activation", [gtok[i]], [sm["sgo"]], out=sm["sgo"][:], in_=gtok[i][:, t4, :], func=gate_func)
        V("dve", "tensor_tensor", [sm["hn"], sm["sgo"]], [sm["ob"]], out=sm["ob"][:], in0=sm["hn"][:], in1=sm["sgo"][:], op=ALU.mult)
        k.tr(pG[:], sm["ob"][:], ident[:], [sm["ob"], ident], pG)
        V("act", "copy", [pG], [oT], out=oT[:, 2 + i, t4 * 128:(t4 + 1) * 128], in_=pG[:])

    V("dve", "memset", [], [oT], ap=oT[:], constant=0.0)
    for blk in range(NB):
        tok = slice(blk * 512, (blk + 1) * 512)
        if stop == 0:
            k.store(io["out"](blk), x32[:], x32, ores)
            continue
        k.load(x32[:], io["h"](blk), x32, reads=io["h_res"])
        for c in range(8):
            s_ = sq[c % 2]
            V("act", "activation", [x32], [s_], out=s_[:], in_=x32[:, c], func=AF.Square)
            k.mm(pz[:], ones16[:], s_[:], c == 0, c == 7, [ones16, s_], pz)
        V("act", "activation", [pz, epsb], [rstd], out=rstd[:], in_=pz[:], func=AF.Sqrt, bias=epsb[:, 0:1], scale=1.0)
        V("dve", "reciprocal", [rstd], [rstd], out=rstd[:], in_=rstd[:])
        for c in range(8):
            V("dve", "scalar_tensor_tensor", [x32, gain, rstd], [x32], out=x32[:, c], in0=x32[:, c], scalar=gain[:, c:c + 1],
              in1=rstd[:], op0=ALU.mult, op1=ALU.mult)
        V("act", "copy", [x32], [hn16], out=hn16[:], in_=x32[:])
        if stop == 1:
            k.store(io["out"](blk), x32[:], x32, ores)
            continue
        for t4 in range(4):
            for c in range(8):
                k.mm(pH[:, t4 * NG:(t4 + 1) * NG], x32[:, c, t4 * 128:(t4 + 1) * 128], wg32[:, c * NG:(c + 1) * NG], c == 0, c == 7,
                     [x32, wg32], pH)
        V("dve", "tensor_tensor", [pH, gbias], [gts], out=gts[:], in0=pH[:, 0:4 * NG].rearrange("p (a b) -> p a b", a=4),
          in1=gbias[:].unsqueeze(1).to_broadcast([128, 4, NG]), op=ALU.add)

        if stop == 2:
            k.store(io["out"](blk), x32[:], x32, ores)
            continue
        if not odd:
            k.load(cost[:], cos_d[:, tok], cost)
            k.load(sint[:], sin_d[:, tok], sint)
            for i in range(2):
                for which, dst in ((0, qa[i]), (2, None)):
                    p = proj_fm(i * 4 + which)
                    V("dve", "tensor_tensor", [p, cost], [rt1], out=rt1[:], in0=p[:], in1=cost[:], op=ALU.mult)
                    p2 = proj_fm(i * 4 + which + 1)
                    V("dve", "tensor_tensor", [p2, sint], [rt2], out=rt2[:], in0=p2[:], in1=sint[:], op=ALU.mult)
                    if dst is not None:
                        V("dve", "tensor_tensor", [rt1, rt2], [dst], out=dst[:], in0=rt1[:], in1=rt2[:], op=ALU.add)
                    else:
                        V("dve", "tensor_tensor", [rt1, rt2], [kcache[i]], out=kcache[i][:, tok], in0=rt1[:], in1=rt2[:], op=ALU.add)
                p = proj_tm(i)
                V("act", "copy", [p], [vcache[i]], out=vcache[i][:, blk * 4:(blk + 1) * 4, :], in_=p[:].rearrange("p (a b) -> p a b", a=4))
            if stop == 3:
                k.store(io["out"](blk), x32[:], x32, ores)
                continue
            for i in range(0 if skip_attn else 2):
                R = attn(i, blk, [slice(0, 64), slice(64, 128)], 64 ** -0.5)
                V("dve", "scalar_tensor_tensor", [R[0], R[1], lamt], [Xt], out=Xt[:], in0=R[1][:], scalar=lamt[:, 4:5], in1=R[0][:],
                  op0=ALU.mult, op1=ALU.add)
                V("act", "activation", [Xt], [sq[0]], out=sq[0][:], in_=Xt[:], func=AF.Square)
                k.mm(pz[:], ones16[:], sq[0][:], True, True, [ones16, sq[0]], pz)
                V("act", "activation", [pz, eps1], [rz], out=rz[:], in_=pz[:], func=AF.Sqrt, bias=eps1[:, 0:1], scale=1.0 / 128.0)
                V("dve", "reciprocal", [rz], [rz], out=rz[:], in_=rz[:])
                V("dve", "scalar_tensor_tensor", [Xt, subc, rz], [oT], out=oT[:, i, :], in0=Xt[:], scalar=subc[:, 0:1], in1=rz[:],
                  op0=ALU.mult, op1=ALU.mult)
            V("act", "activation", [gts], [gt2], out=gt2[:, :, 2:4], in_=gts[:, :, 2:4], func=AF.Exp, scale=-1.0)
            V("act", "activation", [gt2, onec], [gt2], out=gt2[:, :, 2:4], in_=gt2[:, :, 2:4], func=AF.Ln, bias=onec[:, 0:1], scale=1.0)
            V("dve", "tensor_scalar_mul", [gt2], [lf], out=lf[:, :, 2:4], in0=gt2[:, :, 2:4], scalar1=-1.0)
            if stop == 4:
                k.store(io["out"](blk), x32[:], x32, ores)
                continue
            for i in range(0 if skip_rec else 2):
                qc = conv_silu(proj_fm(8 + 2 * i), 2 * i, blk)
                kc = conv_silu(proj_fm(8 + 2 * i + 1), 2 * i + 1, blk)
                p = proj_tm(2 + 2 * i)
                V("act", "copy", [p], [vaug[i]], out=vaug[i][:, :, 0:128], in_=p[:].rearrange("p (a b) -> p a b", a=4))
                p = proj_tm(2 + 2 * i + 1)
                V("act", "copy", [p], [gtok[i]], out=gtok[i][:], in_=p[:].rearrange("p (a b) -> p a b", a=4))
                for t4 in range(4 if stop > 10 else 0):
                    cs = slice(t4 * 128, (t4 + 1) * 128)
                    decay_prep(lf[:, t4, 2 + i:3 + i], lf[:, t4, 0:4], 2 + i)
                    if stop == 105:
                        continue
                    V("dve", "tensor_tensor", [sm["bc"], gts], [sm["rcol"]], out=sm["rcol"][:], in0=sm["bc"][:, 0:1], in1=gts[:, t4, i:i + 1],
                      op=ALU.subtract)
                    V("dve", "tensor_tensor", [sm["bc"], sm["rcol"]], [sm["e1"]], out=sm["e1"][:], in0=sm["bc"][:, 1:2], in1=sm["rcol"][:],
                      op=ALU.subtract)
                    V("act", "activation", [sm["e1"]], [sm["e1"]], out=sm["e1"][:], in_=sm["e1"][:], func=AF.Exp)
                    V("dve", "tensor_scalar_mul", [sm["e1"]], [sm["e1"]], out=sm["e1"][:], in0=sm["e1"][:], scalar1=float(128 ** -0.5))
                    if stop == 11:
                        continue
                    V("dve", "scalar_tensor_tensor", [sm["crep"], sm["rcol"], MBu], [sm["Dm"]], out=sm["Dm"][:], in0=sm["crep"][:],
                      scalar=sm["rcol"][:, 0:1], in1=MBu[:], op0=ALU.subtract, op1=ALU.add)
                    V("act", "activation", [sm["Dm"]], [sm["G"]], out=sm["G"][:], in_=sm["Dm"][:], func=AF.Exp)
                    V("dve", "tensor_tensor", [qc, sm["ecr"]], [qhat[i]], out=qhat[i][:, 0, 0:64], in0=qc[:, t4 * 128:t4 * 128 + 64],
                      in1=sm["ecr"][:, 0:64], op=ALU.mult)
                    V("dve", "tensor_tensor", [qc, sm["ecr"]], [qhat[i]], out=qhat[i][:, 1, 64:128], in0=qc[:, t4 * 128 + 64:(t4 + 1) * 128],
                      in1=sm["ecr"][:, 64:128], op=ALU.mult)
                    k.mm(pC[:], kc[:, cs], qc[:, cs], True, True, [kc, qc], pC)
                    V("dve", "scalar_tensor_tensor", [pC, sm["G"]], [sm["AT"]], out=sm["AT"][:], in0=pC[:], scalar=float(128 ** -0.5),
                      in1=sm["G"][:], op0=ALU.mult, op1=ALU.mult)
                    if stop == 12:
                        continue
                    k.tr(pD[:], kc[:, cs], ident[:], [kc, ident], pD)
                    V("dve", "tensor_scalar_mul", [pD, sm["e1"]], [sm["k2"]], out=sm["k2"][:], in0=pD[:], scalar1=sm["e1"][:, 0:1])
                    if stop == 13:
                        continue
                    S0, S1 = Sst[i]
                    k.mm(pE[:], sm["AT"][:], vaug[i][:, t4, :], True, False, [sm["AT"], vaug[i]], pE)
                    k.mm(pE[:], qhat[i][:, 0, :], S0[:], False, False, [qhat[i], S0], pE)
                    k.mm(pF[:], sm["k2"][0:64, :], vaug[i][0:64, t4, :], True, True, [sm["k2"], vaug[i]], pF)
                    V("dve", "scalar_tensor_tensor", [S0, sm["ecr"], pF], [S1], out=S1[:], in0=S0[:], scalar=sm["ecr"][:, 63:64], in1=pF[:],
                      op0=ALU.mult, op1=ALU.add)
                    k.mm(pE[:], qhat[i][:, 1, :], S1[:], False, True, [qhat[i], S1], pE)
                    k.mm(pF[:], sm["k2"][64:128, :], vaug[i][64:128, t4, :], True, True, [sm["k2"], vaug[i]], pF)
                    V("dve", "scalar_tensor_tensor", [S1, sm["ecr"], pF], [S0], out=S0[:], in0=S1[:], scalar=sm["ecr"][:, 127:128], in1=pF[:],
                      op0=ALU.mult, op1=ALU.add)
                    if stop == 14:
                        continue
                    V("act", "activation", [pE], [sm["dm"]], out=sm["dm"][:], in_=pE[:, 128:129], func=AF.Abs)
                    V("dve", "tensor_scalar_max", [sm["dm"]], [sm["dm"]], out=sm["dm"][:], in0=sm["dm"][:], scalar1=1.0)
                    V("dve", "reciprocal", [sm["dm"]], [sm["dm"]], out=sm["dm"][:], in_=sm["dm"][:])
                    V("dve", "tensor_scalar_mul", [pE, sm["dm"]], [sm["hh"]], out=sm["hh"][:], in0=pE[:, 0:128], scalar1=sm["dm"][:, 0:1])
                    if stop == 15:
                        continue
                    post_out(i, t4, None, AF.Sigmoid)
        else:
            V("act", "activation", [gts], [gt2], out=gt2[:, :, 0:2], in_=gts[:, :, 0:2], func=AF.Exp)
            V("act", "activation", [gts], [gt2], out=gt2[:, :, 4:6], in_=gts[:, :, 4:6], func=AF.Exp, scale=-1.0)
            V("act", "activation", [gt2, onec], [gt2], out=gt2[:, :, 0:2], in_=gt2[:, :, 0:2], func=AF.Ln, bias=onec[:, 0:1], scale=1.0)
            V("act", "activation", [gt2, onec], [gt2], out=gt2[:, :, 4:6], in_=gt2[:, :, 4:6], func=AF.Ln, bias=onec[:, 0:1], scale=1.0)
            V("dve", "tensor_tensor", [gt2, alog], [lf], out=lf[:, :, 0:2], in0=gt2[:, :, 0:2],
              in1=alog[:].unsqueeze(1).to_broadcast([128, 4, 2]), op=ALU.mult)
            V("dve", "tensor_scalar_mul", [gt2], [lf], out=lf[:, :, 4:6], in0=gt2[:, :, 4:6], scalar1=-1.0)
            V("act", "activation", [gts], [lf], out=lf[:, :, 2:4], in_=gts[:, :, 2:4], func=AF.Sigmoid)
            for t4 in range(4):
                for i in range(2):
                    V("dve", "tensor_scalar_mul", [ones32, lf], [sm["lfb"]], out=sm["lfb"][:], in0=ones32[:], scalar1=lf[:, t4, 4 + i:5 + i])
                    k.mm(pA[:], sm["lfb"][:], Ufm[:], True, True, [sm["lfb"], Ufm], pA)
                    V("dve", "tensor_scalar", [pA, carry], [Rq[i]], out=Rq[i][0:1, t4 * 128:(t4 + 1) * 128], in0=pA[0:1, :],
                      scalar1=carry[0:1, i:i + 1], scalar2=None, op0=ALU.add)
                    V("dve", "tensor_tensor", [pA, ident], [sm["junk"]], out=sm["junk"][:], in0=pA[:], in1=ident[:], op=ALU.mult)
                    V("dve", "reduce_sum", [sm["junk"]], [sm["tmpc"]], out=sm["tmpc"][:], in_=sm["junk"][:], axis=AX.X)
                    V("dve", "tensor_scalar", [sm["tmpc"], carry], [ncum[i]], out=ncum[i][:, blk * 4 + t4:blk * 4 + t4 + 1], in0=sm["tmpc"][:],
                      scalar1=carry[:, i:i + 1], scalar2=-1.0, op0=ALU.add, op1=ALU.mult)
                    V("dve", "tensor_tensor", [pA, carry], [carry], out=carry[:, i:i + 1], in0=carry[:, i:i + 1], in1=pA[:, 127:128], op=ALU.add)
            for i in range(2):
                p = proj_fm(6 + 2 * i)
                V("act", "mul", [p], [qa[i]], out=qa[i][:], in_=p[:], mul=float(128 ** -0.5))
                p = proj_fm(6 + 2 * i + 1)
                V("act", "copy", [p], [kcache[i]], out=kcache[i][:, tok], in_=p[:])
                p = proj_tm(2 + i)
                V("act", "copy", [p], [vcache[i]], out=vcache[i][:, blk * 4:(blk + 1) * 4, :], in_=p[:].rearrange("p (a b) -> p a b", a=4))
            for i in range(0 if skip_attn else 2):
                R = attn(i, blk, [slice(0, 128)], 1.0, bias_rows=(ncum[i], Rq[i]))
                V("act", "copy", [R[0]], [oT], out=oT[:, i, :], in_=R[0][:])
            for i in range(0 if skip_rec else 2):
                qc = conv_silu(proj_fm(3 * i), 3 * i, blk)
                kc = conv_silu(proj_fm(3 * i + 1), 3 * i + 1, blk)
                vc = conv_silu(proj_fm(3 * i + 2), 3 * i + 2, blk)
                l2n(qc, 128 ** -0.5)
                l2n(kc, 1.0)
                p = proj_tm(i)
                V("act", "copy", [p], [gtok[i]], out=gtok[i][:], in_=p[:].rearrange("p (a b) -> p a b", a=4))
                for t4 in range(4):
                    cs = slice(t4 * 128, (t4 + 1) * 128)
                    decay_prep(lf[:, t4, i:i + 1], lf[:, t4, 0:4], i)
                    bcol = sm["bc"][:, 0:1]
                    beta = lf[:, t4, 2 + i:3 + i]
                    V("dve", "scalar_tensor_tensor", [sm["crep"], sm["bc"], MBu], [sm["Dm"]], out=sm["Dm"][:], in0=sm["crep"][:],
                      scalar=bcol, in1=MBu[:], op0=ALU.subtract, op1=ALU.add)
                    V("act", "activation", [sm["Dm"]], [sm["G"]], out=sm["G"][:], in_=sm["Dm"][:], func=AF.Exp)
                    V("dve", "scalar_tensor_tensor", [sm["crep"], sm["bc"], MBl], [sm["Dl"]], out=sm["Dl"][:], in0=sm["crep"][:],
                      scalar=bcol, in1=MBl[:], op0=ALU.subtract, op1=ALU.add)
                    V("act", "activation", [sm["Dl"]], [sm["Gl"]], out=sm["Gl"][:], in_=sm["Dl"][:], func=AF.Exp, scale=-1.0)
                    k.tr(pD[:], kc[:, cs], ident[:], [kc, ident], pD)
                    V("act", "copy", [pD], [sm["ktok"]], out=sm["ktok"][:], in_=pD[:])
                    k.tr(pD[:], vc[:, cs], ident[:], [vc, ident], pD)
                    V("dve", "tensor_scalar_mul", [pD, lf], [sm["vb"]], out=sm["vb"][:], in0=pD[:], scalar1=beta)
                    V("act", "activation", [sm["bc"]], [sm["bcol"]], out=sm["bcol"][:], in_=sm["bc"][:, 0:1], func=AF.Exp)
                    V("dve", "tensor_tensor", [sm["bcol"], lf], [sm["bebc"]], out=sm["bebc"][:], in0=sm["bcol"][:], in1=beta, op=ALU.mult)
                    V("dve", "tensor_tensor", [sm["bc"]], [sm["ebd"]], out=sm["ebd"][:], in0=sm["bc"][:, 1:2], in1=sm["bc"][:, 0:1],
                      op=ALU.subtract)
                    V("act", "activation", [sm["ebd"]], [sm["ebd"]], out=sm["ebd"][:], in_=sm["ebd"][:], func=AF.Exp)
                    V("dve", "tensor_scalar_mul", [sm["ktok"], sm["bebc"]], [sm["kp"]], out=sm["kp"][:], in0=sm["ktok"][:],
                      scalar1=sm["bebc"][:, 0:1])
                    V("dve", "tensor_scalar_mul", [sm["ktok"], sm["ebd"]], [sm["kd"]], out=sm["kd"][:], in0=sm["ktok"][:],
                      scalar1=sm["ebd"][:, 0:1])
                    k.mm(pC[:], kc[:, cs], kc[:, cs], True, True, [kc], pC)
                    V("dve", "scalar_tensor_tensor", [pC, lf, sm["Gl"]], [sm["N"]], out=sm["N"][:], in0=pC[:], scalar=beta, in1=sm["Gl"][:],
                      op0=ALU.mult, op1=ALU.mult)
                    k.tr(pC[:], sm["N"][:], ident[:], [sm["N"], ident], pC)
                    V("act", "copy", [pC], [sm["M"]], out=sm["M"][:], in_=pC[:])
                    V("dve", "tensor_tensor", [ident, sm["M"]], [sm["Y"]], out=sm["Y"][:], in0=ident[:], in1=sm["M"][:], op=ALU.subtract)
                    Pc, Mc = sm["N"], sm["M"]
                    Pn, Mn = sm["P"], sm["Mk"]
                    for lev in range(5):
                        k.mm(pC[:], Mc[:], Pc[:], True, True, [Mc, Pc], pC)
                        if lev < 4:
                            k.mm(pD[:], Pc[:], Mc[:], True, True, [Mc, Pc], pD)
                        V("act", "copy", [pC], [Pn], out=Pn[:], in_=pC[:])
                        if lev < 4:
                            V("dve", "tensor_copy", [pD], [Mn], out=Mn[:], in_=pD[:])
                        k.mm(pA[:], Pn[:], sm["Y"][:], True, True, [Pn, sm["Y"]], pA)
                        V("dve", "tensor_tensor", [sm["Y"], pA], [sm["Y"]], out=sm["Y"][:], in0=sm["Y"][:], in1=pA[:], op=ALU.add)
                        Pc, Pn = Pn, Pc
                        Mc, Mn = Mn, Mc
                    k.mm(pC[:], sm["Y"][:], sm["vb"][:], True, True, [sm["Y"], sm["vb"]], pC)
                    V("act", "copy", [pC], [sm["u"]], out=sm["u"][:], in_=pC[:])
                    k.mm(pD[:], sm["kp"][:], sm["Y"][:], True, True, [sm["Y"], sm["kp"]], pD)
                    V("dve", "memset", [], [sm["wT0"]], ap=sm["wT0"][:], constant=0.0)
                    V("dve", "memset", [], [sm["wT1"]], ap=sm["wT1"][:], constant=0.0)
                    V("dve", "tensor_copy", [pD], [sm["wT0"]], out=sm["wT0"][:, 0:64], in_=pD[:, 0:64])
                    V("dve", "tensor_copy", [pD], [sm["wT1"]], out=sm["wT1"][:, 64:128], in_=pD[:, 64:128])
                    k.mm(pC[:], kc[:, cs], qc[:, cs], True, True, [kc, qc], pC)
                    V("dve", "tensor_tensor", [pC, sm["G"]], [sm["AT"]], out=sm["AT"][:], in0=pC[:], in1=sm["G"][:], op=ALU.mult)
                    V("dve", "tensor_tensor", [qc, sm["ecr"]], [qhat[i]], out=qhat[i][:, 0, 0:64], in0=qc[:, t4 * 128:t4 * 128 + 64],
                      in1=sm["ecr"][:, 0:64], op=ALU.mult)
                    V("dve", "tensor_tensor", [qc, sm["ecr"]], [qhat[i]], out=qhat[i][:, 1, 64:128], in0=qc[:, t4 * 128 + 64:(t4 + 1) * 128],
                      in1=sm["ecr"][:, 64:128], op=ALU.mult)
                    S0, S1 = Sst[i]
                    k.mm(pA[:], sm["wT0"][:], S0[:, 0:128], True, True, [sm["wT0"], S0], pA)
                    V("dve", "tensor_tensor", [sm["u"], pA], [sm["vn"]], out=sm["vn"][0:64, :], in0=sm["u"][0:64, :], in1=pA[0:64, :],
                      op=ALU.subtract)
                    k.mm(pE[:, 0:128], qhat[i][:, 0, :], S0[:, 0:128], True, False, [qhat[i], S0], pE)
                    k.mm(pF[:, 0:128], sm["kd"][0:64, :], sm["vn"][0:64, :], True, True, [sm["kd"], sm["vn"]], pF)
                    V("dve", "scalar_tensor_tensor", [S0, sm["ecr"], pF], [S1], out=S1[:, 0:128], in0=S0[:, 0:128], scalar=sm["ecr"][:, 63:64],
                      in1=pF[:, 0:128], op0=ALU.mult, op1=ALU.add)
                    k.mm(pA[:], sm["wT1"][:], S1[:, 0:128], True, True, [sm["wT1"], S1], pA)
                    V("dve", "tensor_tensor", [sm["u"], pA], [sm["vn"]], out=sm["vn"][64:128, :], in0=sm["u"][64:128, :], in1=pA[64:128, :],
                      op=ALU.subtract)
                    k.mm(pE[:, 0:128], qhat[i][:, 1, :], S1[:, 0:128], False, False, [qhat[i], S1], pE)
                    k.mm(pE[:, 0:128], sm["AT"][:], sm["vn"][:], False, True, [sm["AT"], sm["vn"]], pE)
                    k.mm(pF[:, 0:128], sm["kd"][64:128, :], sm["vn"][64:128, :], True, True, [sm["kd"], sm["vn"]], pF)
                    V("dve", "scalar_tensor_tensor", [S1, sm["ecr"], pF], [S0], out=S0[:, 0:128], in0=S1[:, 0:128], scalar=sm["ecr"][:, 127:128],
                      in1=pF[:, 0:128], op0=ALU.mult, op1=ALU.add)
                    V("act", "copy", [pE], [sm["hh"]], out=sm["hh"][:], in_=pE[:, 0:128])
                    post_out(i, t4, None, AF.Silu)

        for c in range(8):
            p = pj[pji[0] % 2]
            pji[0] += 1
            for hs in range(4):
                k.mm(p[:], wout16[:, hs, c * 128:(c + 1) * 128], oT[:, hs, :], hs == 0, hs == 3, [wout16, oT], p)
            V("act", "copy", [p], [x32], out=x32[:, c], in_=p[:])
        k.store(io["out"](blk), x32[:], x32, ores)
    if not standalone:
        return None, k
    nc = k.finish([ores])
    return nc, k


def _kc(w):
    n = w.shape[1]
    return np.ascontiguousarray(w.reshape(8, 128, n).transpose(1, 0, 2).reshape(128, 8 * n))


def mix_consts(S, odd):
    idx = np.arange(128)
    same = (idx[:, None] // 64) == (idx[None, :] // 64)
    U = ((idx[:, None] <= idx[None, :]) & same).astype(np.float32)
    BD = same.astype(np.float32)
    MBu = np.where((idx[None, :] >= idx[:, None]) & same, 0.0, -30000.0).astype(np.float32)
    MBl = np.where((idx[:, None] > idx[None, :]) & same, 0.0, 30000.0).astype(np.float32)
    Uf = (idx[:, None] <= idx[None, :]).astype(np.float32)
    t = np.arange(512)
    masks = np.zeros((4, 128, 512), np.float32)
    for j in range(4):
        key = 128 * j + idx
        if odd:
            masks[j] = (key[:, None] <= t[None, :])
        else:
            masks[j] = ((key[:, None] // 64) <= (t[None, :] // 64))
    d = {"ident": np.eye(128, dtype=np.float32), "U": U, "BD": BD, "MBu": MBu, "MBl": MBl, "masks": masks}
    if odd:
        d["Uf"] = Uf
    else:
        inv = (10000.0 ** (-np.arange(0, 64, 2, dtype=np.float32) / np.float32(64))).astype(np.float32)
        ang = np.arange(S, dtype=np.float32)[None, :] * inv[:, None]
        cos, sin = np.cos(ang).astype(np.float32), np.sin(ang).astype(np.float32)
        p = np.arange(128)
        sign = np.where((p % 64) < 32, -1.0, 1.0).astype(np.float32)
        d["cosT"] = np.ascontiguousarray(cos[p % 32])
        d["sinT"] = np.ascontiguousarray(sin[p % 32] * sign[:, None])
    return d


def mix_inputs_even(hh, w_in, w_out, lq1, lk1, lq2, lk2, subln, conv_b, ig, fg, b_norm, gain):
    hs = [2 * hh, 2 * hh + 1]
    r = np.arange(128)
    swap = np.concatenate([r[32:64], r[0:32], r[96:128], r[64:96]])
    fcols = []
    for a in hs:
        fcols += [128 * a + r, 128 * a + swap, 512 + 128 * a + r, 512 + 128 * a + swap]
    for b in hs:
        fcols += [1536 + 128 * b + r, 2048 + 128 * b + r]
    tcols = [1024 + 128 * a + r for a in hs]
    for b in hs:
        tcols += [2560 + 128 * b + r, 3072 + 128 * b + r]
    gcols = [3584 + hs[0], 3584 + hs[1], 3588 + hs[0], 3588 + hs[1]]
    orow = np.concatenate([128 * hs[0] + r, 128 * hs[1] + r, 512 + 128 * hs[0] + r, 512 + 128 * hs[1] + r])
    cw = np.zeros((128, 16), np.float32)
    for i, b in enumerate(hs):
        cw[:, (2 * i) * 4:(2 * i) * 4 + 4] = conv_b[:, 128 * b + r].T
        cw[:, (2 * i + 1) * 4:(2 * i + 1) * 4 + 4] = conv_b[:, 512 + 128 * b + r].T
    return {"gain": chunk_cols(gain), "wf": np.ascontiguousarray(w_in[:, np.concatenate(fcols)]),
            "wt": np.ascontiguousarray(w_in[:, np.concatenate(tcols)]), "wgt": _kc(w_in[:, gcols]),
            "gbias": np.array([[ig[hs[0]], ig[hs[1]], fg[hs[0]], fg[hs[1]]]], np.float32),
            "wout": np.ascontiguousarray(w_out[orow]), "convw": cw, "nrm": np.ascontiguousarray(b_norm[None, :]),
            "lamv": np.concatenate([lq1, lk1, lq2, lk2])[None, :].astype(np.float32), "subln": np.ascontiguousarray(subln[:, None])}


def mix_inputs_odd(hh, w_in, w_out, conv_c, a_log, dt_bias, c_norm, fd_bias, gain):
    hs = [2 * hh, 2 * hh + 1]
    r = np.arange(128)
    fcols = []
    for c in hs:
        fcols += [128 * c + r, 512 + 128 * c + r, 1024 + 128 * c + r]
    for d in hs:
        fcols += [2056 + 128 * d + r, 2568 + 128 * d + r]
    tcols = [1536 + 128 * c + r for c in hs] + [3080 + 128 * d + r for d in hs]
    gcols = [2048 + hs[0], 2048 + hs[1], 2052 + hs[0], 2052 + hs[1], 3592 + hs[0], 3592 + hs[1]]
    orow = np.concatenate([512 + 128 * hs[0] + r, 512 + 128 * hs[1] + r, 128 * hs[0] + r, 128 * hs[1] + r])
    cw = np.zeros((128, 24), np.float32)
    for i, c in enumerate(hs):
        for j, off in enumerate((0, 512, 1024)):
            g = 3 * i + j
            cw[:, g * 4:g * 4 + 4] = conv_c[:, off + 128 * c + r].T
    return {"gain": chunk_cols(gain), "wf": np.ascontiguousarray(w_in[:, np.concatenate(fcols)]),
            "wt": np.ascontiguousarray(w_in[:, np.concatenate(tcols)]), "wgt": _kc(w_in[:, gcols]),
            "gbias": np.array([[dt_bias[hs[0]], dt_bias[hs[1]], 0.0, 0.0, fd_bias[hs[0]], fd_bias[hs[1]]]], np.float32),
            "wout": np.ascontiguousarray(w_out[orow]), "convw": cw, "nrm": np.ascontiguousarray(c_norm[None, :]),
            "alog": np.array([[a_log[hs[0]], a_log[hs[1]]]], np.float32)}


import math

_GROUPS = [[0, 1], [2, 3], [4, 5], [6, 7]]


def build_fused(S):
    T = S // 2
    NBH = T // 512
    k = KB()
    c8 = lambda ap, tok: ap[:, tok].rearrange("(c p) t -> p c t", p=128)
    xT = k.din("xT", [D, S])
    xh = k.din("xh", [D, T])
    outT = k.dout("outT", [D, T])
    part = [k.dint("part%d" % l, [2 * D, T]) for l in range(2)]
    psum = [k.dint("psumd%d" % l, [D, T]) for l in range(2)]
    h1 = k.dint("h1", [D, T])
    h1f = k.dint("h1f", [2 * D, T])
    part_r = [Res("part%d" % l) for l in range(2)]
    psum_r = [Res("psumd%d" % l) for l in range(2)]
    h1_r, h1f_r, out_r = Res("h1"), Res("h1f"), Res("out")

    def halfblk(ap):
        return lambda blk: ap[(blk // NBH) * D:(blk // NBH + 1) * D, (blk % NBH) * 512:(blk % NBH + 1) * 512].rearrange(
            "(c p) t -> p c t", p=128)

    k.pfx = "L0m_"
    build_mix(S, False, lam_init=0.8 - 0.6 * math.exp(-0.3 * 0), k=k,
              io={"h": lambda blk: c8(xT, slice(blk * 512, (blk + 1) * 512)), "h_res": [], "out": halfblk(part[0]), "out_res": part_r[0]})
    k.collective("ReduceScatter", ALU.add, part[0][:, :], psum[0][:, :], part_r[0], psum_r[0], _GROUPS)
    k.new_section("L0f_")
    build_ffn(T, False, SBT=min(1024, T), k=k,
              io={"h": lambda tok: c8(xh, tok), "h_res": [], "pins": [lambda tok: c8(psum[0], tok)], "pin_res": [psum_r[0]],
                  "out": lambda tok: c8(h1, tok), "out_res": h1_r})
    k.collective("AllGather", ALU.bypass, h1[:, :], h1f[:, :], h1_r, h1f_r, _GROUPS)
    k.new_section("L1m_")
    build_mix(S, True, k=k, io={"h": halfblk(h1f), "h_res": [h1f_r], "out": halfblk(part[1]), "out_res": part_r[1]})
    k.collective("ReduceScatter", ALU.add, part[1][:, :], psum[1][:, :], part_r[1], psum_r[1], _GROUPS)
    k.new_section("L1f_")
    build_ffn(T, True, SBT=min(1024, T), k=k,
              io={"h": lambda tok: c8(h1, tok), "h_res": [h1_r], "pins": [lambda tok: c8(psum[1], tok)], "pin_res": [psum_r[1]],
                  "out": lambda tok: c8(outT, tok), "out_res": out_r})
    nc = k.finish([out_r])
    return nc, k


_PROGS = {}


def build_fused4(S):
    k = KB()
    c8 = lambda ap, tok: ap[:, tok].rearrange("(c p) t -> p c t", p=128)
    blk8 = lambda ap: (lambda blk: c8(ap, slice(blk * 512, (blk + 1) * 512)))
    xT = k.din("xT", [D, S])
    outT = k.dout("outT", [D, S])
    pa = k.dint("partA", [D, S])
    pb = k.dint("partB", [D, S])
    h1 = k.dint("h1", [D, S])
    pa_r, pb_r, h1_r, out_r = Res("partA"), Res("partB"), Res("h1"), Res("out")
    first = True
    for layer in range(2):
        odd = layer % 2 == 1
        hsrc, hres = (xT, []) if layer == 0 else (h1, [h1_r])
        for tag, dst, dres in (("A", pa, pa_r), ("B", pb, pb_r)):
            if first:
                k.pfx = "L%dm%s_" % (layer, tag)
                first = False
            else:
                k.new_section("L%dm%s_" % (layer, tag))
            build_mix(S, odd, lam_init=0.8 - 0.6 * math.exp(-0.3 * layer), k=k,
                      io={"h": blk8(hsrc), "h_res": hres, "out": blk8(dst), "out_res": dres})
        k.new_section("L%df_" % layer)
        final = layer == 1
        odst, ores_ = (outT, out_r) if final else (h1, h1_r)
        build_ffn(S, final, SBT=min(1024, S), k=k,
                  io={"h": lambda tok, a=hsrc: c8(a, tok), "h_res": hres,
                      "pins": [lambda tok: c8(pa, tok), lambda tok: c8(pb, tok)], "pin_res": [pa_r, pb_r],
                      "out": lambda tok, a=odst: c8(a, tok), "out_res": ores_})
    nc = k.finish([out_r])
    return nc, k


def fused4_inputs(b, S, x, p, W):
    f = lambda a: np.asarray(a, np.float32)
    m = {"xT": np.ascontiguousarray(x[b].T)}
    ident, sel = ffn_consts()
    for layer in range(2):
        odd = layer % 2 == 1
        j = layer // 2
        cst = mix_consts(S, odd)
        for r, tag in ((0, "A"), (1, "B")):
            if odd:
                d = mix_inputs_odd(r, f(W["cd_w_in"][j]), f(W["cd_w_out"][j]), f(W["c_conv"][j]), f(W["c_a_log"][j]), f(W["c_dt_bias"][j]),
                                   f(W["c_norm"][j]), f(W["d_fgate_bias"][j]), f(W["norm_mix"][layer]))
            else:
                d = mix_inputs_even(r, f(W["ab_w_in"][j]), f(W["ab_w_out"][j]), f(W["a_lam_q1"][j]), f(W["a_lam_k1"][j]), f(W["a_lam_q2"][j]),
                                    f(W["a_lam_k2"][j]), f(W["a_subln"][j]), f(W["b_conv"][j]), f(W["b_igate_bias"][j]),
                                    f(W["b_fgate_bias"][j]), f(W["b_norm"][j]), f(W["norm_mix"][layer]))
            d.update(cst)
            for kk, vv in d.items():
                m["L%dm%s_%s" % (layer, tag, kk)] = vv
        Wr = np.concatenate([f(W["moe_w_group"][layer]), f(W["moe_w_router"][layer])], axis=1)
        fd = {"pT": np.ascontiguousarray(p[layer, b].T), "gain": chunk_cols(W["norm_ffn"][layer]), "gfin": chunk_cols(W["norm_final"]),
              "wr": np.ascontiguousarray(Wr.reshape(8, 128, 20).transpose(1, 0, 2).reshape(128, 160)),
              "br": np.concatenate([f(W["moe_b_group"][layer]), f(W["moe_b_router"][layer])])[None, :],
              "wg": tile_w(W["moe_w_gate"][layer]), "wu": tile_w(W["moe_w_up"][layer]), "wd": tile_w(W["moe_w_down"][layer]),
              "plg": f(W["ple_w_gate"][layer]), "plp": f(W["ple_w_proj"][layer]), "ident": ident, "sel": sel}
        for kk, vv in fd.items():
            m["L%df_%s" % (layer, kk)] = vv
    return m


def kernel(x, p, **W):
    x = np.asarray(x, np.float32)
    p = np.asarray(p, np.float32)
    B, S, _ = x.shape
    key = ("f4", S)
    if key not in _PROGS:
        _PROGS[key] = build_fused4(S)[0]
    nc = _PROGS[key]
    per_b = [fused4_inputs(b, S, x, p, W) for b in range(B)]
    maps = [per_b[c % B] for c in range(NCORES)]
    res = run_bass_kernel_spmd(nc, maps, core_ids=list(range(NCORES)))
    return np.stack([np.ascontiguousarray(res.results[b]["outT"].T) for b in range(B)]).astype(np.float32)


def fused_inputs(c, S, x, p, W):
    f = lambda a: np.asarray(a, np.float32)
    T = S // 2
    b, r = c // 2, c % 2
    ts = slice(r * T, (r + 1) * T)
    xTb = np.ascontiguousarray(x[b].T)
    m = {"xT": xTb, "xh": np.ascontiguousarray(xTb[:, ts])}
    ident, sel = ffn_consts()
    for layer in range(2):
        odd = layer % 2 == 1
        j = layer // 2
        if odd:
            d = mix_inputs_odd(r, f(W["cd_w_in"][j]), f(W["cd_w_out"][j]), f(W["c_conv"][j]), f(W["c_a_log"][j]), f(W["c_dt_bias"][j]),
                               f(W["c_norm"][j]), f(W["d_fgate_bias"][j]), f(W["norm_mix"][layer]))
        else:
            d = mix_inputs_even(r, f(W["ab_w_in"][j]), f(W["ab_w_out"][j]), f(W["a_lam_q1"][j]), f(W["a_lam_k1"][j]), f(W["a_lam_q2"][j]),
                                f(W["a_lam_k2"][j]), f(W["a_subln"][j]), f(W["b_conv"][j]), f(W["b_igate_bias"][j]),
                                f(W["b_fgate_bias"][j]), f(W["b_norm"][j]), f(W["norm_mix"][layer]))
        d.update(mix_consts(S, odd))
        for kk, vv in d.items():
            m["L%dm_%s" % (layer, kk)] = vv
        Wr = np.concatenate([f(W["moe_w_group"][layer]), f(W["moe_w_router"][layer])], axis=1)
        fd = {"pT": np.ascontiguousarray(p[layer, b, ts].T), "gain": chunk_cols(W["norm_ffn"][layer]), "gfin": chunk_cols(W["norm_final"]),
              "wr": np.ascontiguousarray(Wr.reshape(8, 128, 20).transpose(1, 0, 2).reshape(128, 160)),
              "br": np.concatenate([f(W["moe_b_group"][layer]), f(W["moe_b_router"][layer])])[None, :],
              "wg": tile_w(W["moe_w_gate"][layer]), "wu": tile_w(W["moe_w_up"][layer]), "wd": tile_w(W["moe_w_down"][layer]),
              "plg": f(W["ple_w_gate"][layer]), "plp": f(W["ple_w_proj"][layer]), "ident": ident, "sel": sel}
        for kk, vv in fd.items():
            m["L%df_%s" % (layer, kk)] = vv
    return m


def kernel_cc(x, p, **W):
    x = np.asarray(x, np.float32)
    p = np.asarray(p, np.float32)
    B, S, _ = x.shape
    T = S // 2
    if S not in _PROGS:
        _PROGS[S] = build_fused(S)[0]
    nc = _PROGS[S]
    maps = [fused_inputs(c, S, x, p, W) for c in range(NCORES)]
    res = run_bass_kernel_spmd(nc, maps, core_ids=list(range(NCORES)))
    out = np.empty((B, S, D), np.float32)
    for c in range(NCORES):
        b, r = c // 2, c % 2
        out[b, r * T:(r + 1) * T, :] = res.results[c]["outT"].T
    return out
```

```python
import numpy as np
from contextlib import ExitStack
import concourse.bass as bass
import concourse.mybir as mybir
from concourse.bass_utils import run_bass_kernel_spmd

F32 = mybir.dt.float32
BF16 = mybir.dt.bfloat16
AF = mybir.ActivationFunctionType
ALU = mybir.AluOpType
AX = mybir.AxisListType

D = 1024
NCORES = 8
RMS_EPS = 1e-6


class Res:
    __slots__ = ("name", "lw", "rd", "dsem", "dcnt")

    def __init__(self, name):
        self.name = name
        self.lw = None
        self.rd = {}
        self.dsem = None
        self.dcnt = 0


class Sched:
    ENGS = ("pe", "act", "dve", "pool", "sp")

    def __init__(self, nc, es):
        self.nc = nc
        self.es = es
        self.prog = {e: [] for e in self.ENGS}
        self.cnt = {e: 0 for e in self.ENGS}
        self.sems = {}
        for e in self.ENGS:
            self.sems[e] = es.enter_context(nc.semaphore("s_" + e))
        self.waited = {e: {} for e in self.ENGS}
        self.nsem = 0
        self.n_inst = 0
        self.n_wait = 0
        self.dcount = {}

    def new_sem(self, name):
        k = "d_" + name
        if k not in self.sems:
            self.nsem += 1
            self.sems[k] = self.es.enter_context(self.nc.semaphore(k))
            self.dcount[k] = 0
        return k

    def barrier(self):
        for e in self.ENGS:
            for f in self.ENGS:
                if f != e:
                    self._wait(e, f, self.cnt[f])
            for key, c in self.dcount.items():
                self._wait(e, key, c)
        for e in self.ENGS:
            if e != "pe":
                self._wait(e, e, self.cnt[e])

    def _wait(self, eng, key, val):
        if val <= 0:
            return
        if key == eng and eng == "pe":
            return
        w = self.waited[eng]
        if w.get(key, 0) >= val:
            return
        w[key] = val
        self.prog[eng].append(("w", key, val))
        self.n_wait += 1

    def _deps(self, eng, reads, writes):
        for r in reads:
            if r.lw is not None:
                self._wait(eng, r.lw[0], r.lw[1])
        for w in writes:
            if w.lw is not None:
                self._wait(eng, w.lw[0], w.lw[1])
            for k, v in w.rd.items():
                self._wait(eng, k, v)

    def op(self, eng, fn, reads=(), writes=()):
        self._deps(eng, reads, writes)
        self.cnt[eng] += 1
        c = self.cnt[eng]
        self.prog[eng].append(("i", fn, eng, 1))
        for r in reads:
            if r.rd.get(eng, 0) < c:
                r.rd[eng] = c
        for w in writes:
            w.lw = (eng, c)
            w.rd = {}
        self.n_inst += 1

    def dma(self, q, fn, reads=(), writes=(), sem_res=None, inc=16):
        self._deps(q, reads, writes)
        sr = sem_res if sem_res is not None else (writes[0] if writes else reads[0])
        if sr.dsem is None:
            sr.dsem = self.new_sem(sr.name)
        key = sr.dsem
        self.dcount[key] += inc
        c = self.dcount[key]
        sr.dcnt = c
        self.prog[q].append(("i", fn, key, inc))
        for r in reads:
            if r.rd.get(key, 0) < c:
                r.rd[key] = c
        for w in writes:
            w.lw = (key, c)
            w.rd = {}
        self.n_inst += 1

    def wait_all(self, eng, ress):
        for r in ress:
            if r.lw is not None:
                self._wait(eng, r.lw[0], r.lw[1])
            for k, v in r.rd.items():
                self._wait(eng, k, v)

    def emit(self):
        nc = self.nc
        sems = self.sems
        prog = self.prog

        def run(engh, lst):
            for it in lst:
                if it[0] == "w":
                    engh.wait_ge(sems[it[1]], it[2])
                else:
                    it[1](engh).then_inc(sems[it[2]], it[3])

        with nc.Block() as block:
            @block.tensor
            def _(e):
                run(e, prog["pe"])

            @block.scalar
            def _(e):
                run(e, prog["act"])

            @block.vector
            def _(e):
                run(e, prog["dve"])

            @block.gpsimd
            def _(e):
                run(e, prog["pool"])

            @block.sync
            def _(e):
                run(e, prog["sp"])


class Tile:
    def __init__(self, t, name):
        self.t = t
        self.r = Res(name)

    def __getitem__(self, k):
        return self.t[k]


class KB:
    def __init__(self):
        self.nc = bass.Bass("TRN2", target_bir_lowering=False)
        self.es = ExitStack()
        self.S = Sched(self.nc, self.es)
        self.tes = ExitStack()
        self.pfx = ""
        self.in_names = []

    def new_section(self, pfx):
        self.S.barrier()
        self.tes.close()
        self.tes = ExitStack()
        self.pfx = pfx

    def din(self, name, shape, dt=F32):
        self.in_names.append(self.pfx + name)
        return self.nc.dram_tensor(self.pfx + name, list(shape), dt, kind="ExternalInput").ap()

    def dout(self, name, shape, dt=F32):
        return self.nc.dram_tensor(self.pfx + name, list(shape), dt, kind="ExternalOutput").ap()

    def dint(self, name, shape, dt=F32):
        return self.nc.dram_tensor(name, list(shape), dt).ap()

    def sb(self, name, shape, dt=F32):
        return Tile(self.tes.enter_context(self.nc.sbuf_tensor(self.pfx + name, list(shape), dt)), self.pfx + name)

    def ps(self, name, shape, dt=F32):
        return Tile(self.tes.enter_context(self.nc.psum_tensor(self.pfx + name, list(shape), dt)), self.pfx + name)

    def op(self, eng, fn, reads, writes):
        self.S.op(eng, fn, [x.r for x in reads], [x.r for x in writes])

    def load(self, out_ap, in_ap, wt, q="sp", reads=()):
        self.S.dma(q, lambda e: e.dma_start(out=out_ap, in_=in_ap), [x if isinstance(x, Res) else x.r for x in reads], [wt.r])

    def store(self, out_ap, in_ap, rt, ores, q="sp"):
        for kk, vv in ores.rd.items():
            self.S._wait(q, kk, vv)
        self.S.dma(q, lambda e: e.dma_start(out=out_ap, in_=in_ap), [rt.r], [], sem_res=ores)
        ores.lw = (ores.dsem, self.S.dcount[ores.dsem])

    def collective(self, kind, op, in_ap, out_ap, in_res, out_res, groups):
        import os
        if os.environ.get("MK_NOCC"):
            return
        self.S.dma("pool", lambda e: e.collective_compute(kind, op, replica_groups=groups, ins=[in_ap], outs=[out_ap]),
                   [in_res], [out_res], inc=1)

    def mm(self, out_ap, lhsT, rhs, start, stop, reads, wt):
        self.op("pe", lambda e: e.matmul(out_ap, lhsT, rhs, start=start, stop=stop), reads, [wt])

    def tr(self, out_ap, in_ap, ident_ap, reads, wt):
        self.op("pe", lambda e: e.transpose(out_ap, in_ap, ident_ap), reads, [wt])

    def finish(self, out_res_list):
        self.S.wait_all("sp", out_res_list)
        self.S.emit()
        self.tes.close()
        self.es.close()
        return self.nc


def build_ffn(T, final, SBT=1024, k=None, io=None):
    NB = T // 512
    NSB = max(1, T // SBT)
    BPS = NB // NSB
    standalone = k is None
    c8 = lambda ap, tok: ap[:, tok].rearrange("(c p) t -> p c t", p=128)
    if standalone:
        k = KB()
        hT = k.din("hT", [D, T])
        p0T = k.din("p0T", [D, T])
        p1T = k.din("p1T", [D, T])
        io = {"h": lambda tok: c8(hT, tok), "h_res": [], "pins": [lambda tok: c8(p0T, tok), lambda tok: c8(p1T, tok)], "pin_res": []}
    pT = k.din("pT", [256, T])
    gain_d = k.din("gain", [128, 8])
    gfin_d = k.din("gfin", [128, 8])
    wr_d = k.din("wr", [128, 8 * 20])
    br_d = k.din("br", [1, 20])
    wg_d = k.din("wg", [16, 128, 4096])
    wu_d = k.din("wu", [16, 128, 4096])
    wd_d = k.din("wd", [16, 128, 4096])
    plg_d = k.din("plg", [D, D])
    plp_d = k.din("plp", [256, D])
    ident_d = k.din("ident", [128, 128])
    sel_d = k.din("sel", [16, 16 * 128])
    if standalone:
        outT = k.dout("outT", [D, T])
        ores = Res("out")
        io["out"] = lambda tok: c8(outT, tok)
        io["out_res"] = ores
    ores = io["out_res"]

    acc = k.sb("acc", [128, BPS, 8, 512])
    hn16 = k.sb("hn16", [128, BPS, 8, 512], BF16)
    big32 = k.sb("big32", [128, 8, 512])
    combT = k.sb("combT", [16, BPS * 512])
    wbuf = [k.sb("wbuf%d" % i, [128, 12288], BF16) for i in range(2)]
    stage = [k.sb("stage%d" % i, [128, 2048]) for i in range(3)]
    p16 = k.sb("p16", [128, 2, 512], BF16)
    h2b = k.sb("h2b", [128, 8, 512], BF16)
    sg = [k.sb("sg%d" % i, [128, 512]) for i in range(2)]
    tt = [k.sb("tt%d" % i, [128, 512]) for i in range(2)]
    he = [k.sb("he%d" % i, [128, 4, 512], BF16) for i in range(2)]
    sq = [k.sb("sq%d" % i, [128, 512], BF16) for i in range(2)]
    rstd = k.sb("rstd", [128, 512])
    gain = k.sb("gain_s", [128, 8])
    gfin = k.sb("gfin_s", [128, 8])
    wr = k.sb("wr_s", [128, 8 * 20])
    br = k.sb("br_s", [128, 20])
    ident = k.sb("ident_s", [128, 128])
    sel = k.sb("sel_s", [16, 16 * 128])
    ones16 = k.sb("ones16", [128, 128], BF16)
    rt = {n: k.sb("rt_" + n, [128, w]) for n, w in
          [("L", 20), ("gmax", 1), ("ngmax", 1), ("gm", 4), ("ex", 4), ("se", 1), ("ptop", 1), ("t44", 16), ("esel", 4),
           ("m1", 1), ("k1", 4), ("e2", 4), ("m2", 1), ("k2", 4), ("d", 1), ("ed", 1), ("w1", 1), ("w2", 1), ("t1", 4),
           ("cl", 4), ("comb", 16)]}
    ps_gu = [k.ps("ps_gu%d" % i, [128, 512]) for i in range(4)]
    ps_y = [k.ps("ps_y%d" % i, [128, 512]) for i in range(2)]
    ps_c = k.ps("ps_c", [128, 512])
    ps_m = k.ps("ps_m", [128, 512])

    k.load(gain[:], gain_d[:, :], gain)
    k.load(gfin[:], gfin_d[:, :], gfin)
    k.load(wr[:], wr_d[:, :], wr)
    k.load(br[:], br_d.partition_broadcast(128), br)
    k.load(ident[:], ident_d[:, :], ident)
    k.load(sel[:], sel_d[:, :], sel)
    k.op("dve", lambda e: e.memset(ones16[:], 1.0), [], [ones16])
    epsb = k.sb("epsb", [128, 1])
    k.op("dve", lambda e: e.memset(epsb[:], float(D * RMS_EPS)), [], [epsb])
    k.op("dve", lambda e: e.tensor_scalar_mul(out=gain[:], in0=gain[:], scalar1=32.0), [gain], [gain])
    k.op("dve", lambda e: e.tensor_scalar_mul(out=gfin[:], in0=gfin[:], scalar1=32.0), [gfin], [gfin])

    stage_i = [0]

    def load_cast(dst_ap, src_ap, dst_tile, shape3=None):
        st = stage[stage_i[0] % 3]
        stage_i[0] += 1
        if shape3 is None:
            k.load(st[:], src_ap, st)
            k.op("act", lambda e: e.copy(out=dst_ap, in_=st[:]), [st], [dst_tile])
        else:
            a, b = shape3
            k.load(st[:].rearrange("p (a b) -> p a b", a=a), src_ap, st)
            k.op("act", lambda e: e.copy(out=dst_ap, in_=st[:]), [st], [dst_tile])

    def rmsnorm_stats(src_chunks, src_tile):
        for c in range(8):
            s = sq[c % 2]
            k.op("act", (lambda s, c: lambda e: e.activation(out=s[:], in_=src_chunks(c), func=AF.Square))(s, c),
                 [src_tile], [s])
            k.mm(ps_m[:], ones16[:], s[:], c == 0, c == 7, [ones16, s], ps_m)
        k.op("act", lambda e: e.activation(out=rstd[:], in_=ps_m[:], func=AF.Sqrt, bias=epsb[:, 0:1], scale=1.0),
             [ps_m, epsb], [rstd])
        k.op("dve", lambda e: e.reciprocal(out=rstd[:], in_=rstd[:]), [rstd], [rstd])

    for sb_i in range(NSB):
        for j in range(BPS):
            tok = slice((sb_i * BPS + j) * 512, (sb_i * BPS + j + 1) * 512)
            k.load(acc[:, j], io["h"](tok), acc, reads=io["h_res"])
            for src in io["pins"]:
                k.load(big32[:], src(tok), big32, reads=io["pin_res"])
                k.op("dve", (lambda j: lambda e: e.tensor_tensor(out=acc[:, j], in0=acc[:, j], in1=big32[:], op=ALU.add))(j),
                     [acc, big32], [acc])
            rmsnorm_stats(lambda c, j=j: acc[:, j, c], acc)
            for c in range(8):
                k.op("dve", (lambda j, c: lambda e: e.scalar_tensor_tensor(
                    out=big32[:, c], in0=acc[:, j, c], scalar=gain[:, c:c + 1], in1=rstd[:], op0=ALU.mult, op1=ALU.mult))(j, c),
                    [acc, gain, rstd], [big32])
            k.op("act", (lambda j: lambda e: e.copy(out=hn16[:, j], in_=big32[:]))(j), [big32], [hn16])
            for t4 in range(4):
                for c in range(8):
                    k.mm(ps_m[:, 0:20], big32[:, c, t4 * 128:(t4 + 1) * 128], wr[:, c * 20:(c + 1) * 20], c == 0, c == 7,
                         [big32, wr], ps_m)
                R = rt
                V = "dve"
                k.op(V, lambda e: e.tensor_tensor(out=R["L"][:], in0=ps_m[:, 0:20], in1=br[:], op=ALU.add), [ps_m, br], [R["L"]])
                k.op(V, lambda e: e.reduce_max(out=R["gmax"][:], in_=R["L"][:, 0:4], axis=AX.X), [R["L"]], [R["gmax"]])
                k.op(V, lambda e: e.tensor_tensor(out=R["gm"][:], in0=R["L"][:, 0:4], in1=R["gmax"][:, 0:1].to_broadcast([128, 4]),
                                                  op=ALU.is_ge), [R["L"], R["gmax"]], [R["gm"]])
                k.op(V, lambda e: e.tensor_scalar_mul(out=R["ngmax"][:], in0=R["gmax"][:], scalar1=-1.0), [R["gmax"]], [R["ngmax"]])
                k.op("act", lambda e: e.activation(out=R["ex"][:], in_=R["L"][:, 0:4], func=AF.Exp, bias=R["ngmax"][:, 0:1],
                                                   scale=1.0, accum_out=R["se"][:]), [R["L"], R["ngmax"]], [R["ex"], R["se"]])
                k.op(V, lambda e: e.reciprocal(out=R["ptop"][:], in_=R["se"][:]), [R["se"]], [R["ptop"]])
                k.op(V, lambda e: e.tensor_tensor(
                    out=R["t44"][:].rearrange("p (g x) -> p g x", g=4),
                    in0=R["L"][:, 4:20].rearrange("p (g x) -> p g x", g=4),
                    in1=R["gm"][:].unsqueeze(2).to_broadcast([128, 4, 4]), op=ALU.mult), [R["L"], R["gm"]], [R["t44"]])
                k.op(V, lambda e: e.tensor_reduce(out=R["esel"][:], in_=R["t44"][:].rearrange("p (g x) -> p x g", g=4),
                                                  axis=AX.X, op=ALU.add), [R["t44"]], [R["esel"]])
                k.op(V, lambda e: e.reduce_max(out=R["m1"][:], in_=R["esel"][:], axis=AX.X), [R["esel"]], [R["m1"]])
                k.op(V, lambda e: e.tensor_tensor(out=R["k1"][:], in0=R["esel"][:], in1=R["m1"][:, 0:1].to_broadcast([128, 4]),
                                                  op=ALU.is_ge), [R["esel"], R["m1"]], [R["k1"]])
                k.op(V, lambda e: e.scalar_tensor_tensor(out=R["e2"][:], in0=R["k1"][:], scalar=-1e30, in1=R["esel"][:],
                                                         op0=ALU.mult, op1=ALU.add), [R["k1"], R["esel"]], [R["e2"]])
                k.op(V, lambda e: e.reduce_max(out=R["m2"][:], in_=R["e2"][:], axis=AX.X), [R["e2"]], [R["m2"]])
                k.op(V, lambda e: e.tensor_tensor(out=R["k2"][:], in0=R["e2"][:], in1=R["m2"][:, 0:1].to_broadcast([128, 4]),
                                                  op=ALU.is_ge), [R["e2"], R["m2"]], [R["k2"]])
                k.op(V, lambda e: e.tensor_tensor(out=R["d"][:], in0=R["m2"][:], in1=R["m1"][:], op=ALU.subtract),
                     [R["m1"], R["m2"]], [R["d"]])
                k.op("act", lambda e: e.activation(out=R["ed"][:], in_=R["d"][:], func=AF.Exp), [R["d"]], [R["ed"]])
                k.op(V, lambda e: e.tensor_scalar_add(out=R["w1"][:], in0=R["ed"][:], scalar1=1.0), [R["ed"]], [R["w1"]])
                k.op(V, lambda e: e.reciprocal(out=R["w1"][:], in_=R["w1"][:]), [R["w1"]], [R["w1"]])
                k.op(V, lambda e: e.tensor_tensor(out=R["w1"][:], in0=R["w1"][:], in1=R["ptop"][:], op=ALU.mult),
                     [R["w1"], R["ptop"]], [R["w1"]])
                k.op(V, lambda e: e.tensor_tensor(out=R["w2"][:], in0=R["w1"][:], in1=R["ed"][:], op=ALU.mult),
                     [R["w1"], R["ed"]], [R["w2"]])
                k.op(V, lambda e: e.tensor_scalar(out=R["t1"][:], in0=R["k1"][:], scalar1=R["w1"][:, 0:1], scalar2=None,
                                                  op0=ALU.mult), [R["k1"], R["w1"]], [R["t1"]])
                k.op(V, lambda e: e.scalar_tensor_tensor(out=R["cl"][:], in0=R["k2"][:], scalar=R["w2"][:, 0:1], in1=R["t1"][:],
                                                         op0=ALU.mult, op1=ALU.add), [R["k2"], R["w2"], R["t1"]], [R["cl"]])
                k.op(V, lambda e: e.tensor_tensor(
                    out=R["comb"][:].rearrange("p (g x) -> p g x", g=4),
                    in0=R["gm"][:].unsqueeze(2).to_broadcast([128, 4, 4]),
                    in1=R["cl"][:].unsqueeze(1).to_broadcast([128, 4, 4]), op=ALU.mult), [R["gm"], R["cl"]], [R["comb"]])
                k.tr(ps_m[0:16, 128:256], R["comb"][:], ident[:], [R["comb"], ident], ps_m)
                k.op(V, (lambda j, t4: lambda e: e.tensor_copy(out=combT[:, j * 512 + t4 * 128: j * 512 + (t4 + 1) * 128],
                                                               in_=ps_m[0:16, 128:256]))(j, t4), [ps_m], [combT])

        def load_expert(e):
            wb = wbuf[e % 2]
            for mi, src in enumerate((wg_d, wu_d, wd_d)):
                for half in range(2):
                    load_cast(wb[:, mi * 4096 + half * 2048: mi * 4096 + (half + 1) * 2048], src[e, :, half * 2048:(half + 1) * 2048], wb)

        load_expert(0)
        gi = 0
        yi = 0
        for e in range(16):
            if e + 1 < 16:
                load_expert(e + 1)
            wb = wbuf[e % 2]
            for j in range(BPS):
                k.mm(ps_c[:], sel[:, e * 128:(e + 1) * 128], combT[:, j * 512:(j + 1) * 512], True, True, [sel, combT], ps_c)
                hb = he[(e * BPS + j) % 2]
                for f in range(4):
                    pg = ps_gu[gi % 4]
                    pu = ps_gu[(gi + 1) % 4]
                    gi += 2
                    for c in range(8):
                        k.mm(pg[:], wb[:, c * 512 + f * 128: c * 512 + (f + 1) * 128], hn16[:, j, c], c == 0, c == 7, [wb, hn16], pg)
                    for c in range(8):
                        k.mm(pu[:], wb[:, 4096 + c * 512 + f * 128: 4096 + c * 512 + (f + 1) * 128], hn16[:, j, c], c == 0, c == 7,
                             [wb, hn16], pu)
                    s_ = sg[f % 2]
                    t_ = tt[f % 2]
                    k.op("act", (lambda s_, pg: lambda e: e.activation(out=s_[:], in_=pg[:], func=AF.Silu))(s_, pg), [pg], [s_])
                    k.op("dve", (lambda t_, s_, pu: lambda e: e.tensor_tensor(out=t_[:], in0=s_[:], in1=pu[:], op=ALU.mult))(t_, s_, pu),
                         [s_, pu], [t_])
                    k.op("dve", (lambda hb, f, t_: lambda e: e.tensor_tensor(out=hb[:, f], in0=t_[:], in1=ps_c[:], op=ALU.mult))(hb, f, t_),
                         [t_, ps_c], [hb])
                for c in range(8):
                    py = ps_y[yi % 2]
                    yi += 1
                    for f in range(4):
                        k.mm(py[:], wb[:, 8192 + f * 1024 + c * 128: 8192 + f * 1024 + (c + 1) * 128], hb[:, f], f == 0, f == 3,
                             [wb, hb], py)
                    k.op("dve", (lambda j, c, py: lambda e: e.tensor_tensor(out=acc[:, j, c], in0=acc[:, j, c], in1=py[:], op=ALU.add))(j, c, py),
                         [acc, py], [acc])

        wp = wbuf[0]
        for q4 in range(4):
            load_cast(wp[:, q4 * 2048:(q4 + 1) * 2048],
                      plg_d[q4 * 256:(q4 + 1) * 256, :].rearrange("(k p) n -> p k n", p=128), wp, (2, 1024))
        load_cast(wp[:, 8192:10240], plp_d[:, :].rearrange("(k p) n -> p k n", p=128), wp, (2, 1024))
        for j in range(BPS):
            tok = slice((sb_i * BPS + j) * 512, (sb_i * BPS + j + 1) * 512)
            st = stage[stage_i[0] % 3]
            stage_i[0] += 1
            k.load(st[:, 0:1024].rearrange("p (a b) -> p a b", a=2), pT[:, tok].rearrange("(c p) t -> p c t", p=128), st)
            k.op("act", (lambda st: lambda e: e.copy(out=p16[:], in_=st[:, 0:1024]))(st), [st], [p16])
            k.op("act", (lambda j: lambda e: e.copy(out=h2b[:], in_=acc[:, j]))(j), [acc], [h2b])
            for c in range(8):
                pg = ps_gu[gi % 4]
                pu = ps_gu[(gi + 1) % 4]
                gi += 2
                for kk in range(8):
                    k.mm(pg[:], wp[:, kk * 1024 + c * 128: kk * 1024 + (c + 1) * 128], h2b[:, kk], kk == 0, kk == 7, [wp, h2b], pg)
                for kk in range(2):
                    k.mm(pu[:], wp[:, 8192 + kk * 1024 + c * 128: 8192 + kk * 1024 + (c + 1) * 128], p16[:, kk], kk == 0, kk == 1,
                         [wp, p16], pu)
                s_ = sg[c % 2]
                t_ = tt[c % 2]
                k.op("act", (lambda s_, pg: lambda e: e.activation(out=s_[:], in_=pg[:], func=AF.Sigmoid))(s_, pg), [pg], [s_])
                k.op("dve", (lambda t_, s_, pu: lambda e: e.tensor_tensor(out=t_[:], in0=s_[:], in1=pu[:], op=ALU.mult))(t_, s_, pu),
                     [s_, pu], [t_])
                k.op("dve", (lambda j, c, t_: lambda e: e.tensor_tensor(out=acc[:, j, c], in0=acc[:, j, c], in1=t_[:], op=ALU.add))(j, c, t_),
                     [acc, t_], [acc])
            if final:
                rmsnorm_stats(lambda c, j=j: acc[:, j, c], acc)
                for c in range(8):
                    k.op("dve", (lambda j, c: lambda e: e.scalar_tensor_tensor(
                        out=big32[:, c], in0=acc[:, j, c], scalar=gfin[:, c:c + 1], in1=rstd[:], op0=ALU.mult, op1=ALU.mult))(j, c),
                        [acc, gfin, rstd], [big32])
                k.store(io["out"](tok), big32[:], big32, ores)
            else:
                k.store(io["out"](tok), acc[:, j], acc, ores)
    if not standalone:
        return None, k
    nc = k.finish([ores])
    return nc, k


def ffn_consts():
    ident = np.eye(128, dtype=np.float32)
    sel = np.zeros((16, 16 * 128), np.float32)
    for e in range(16):
        sel[e, e * 128:(e + 1) * 128] = 1.0
    return ident, sel


def tile_w(w):
    w = np.asarray(w, np.float32)
    E, K_, N_ = w.shape
    return np.ascontiguousarray(w.reshape(E, K_ // 128, 128, N_).transpose(0, 2, 1, 3).reshape(E, 128, (K_ // 128) * N_))


def chunk_cols(v):
    return np.ascontiguousarray(np.asarray(v, np.float32).reshape(8, 128).T)


class Sub:
    def __init__(self, ap, name, res=None):
        self.ap = ap
        self.r = res if res is not None else Res(name)

    def __getitem__(self, k):
        return self.ap[k]


def _v(k, eng, meth, reads, writes, **kw):
    k.S.op(eng, lambda e: getattr(e, meth)(**kw), [x.r for x in reads], [x.r for x in writes])


def build_mix(S, odd, lam_init=0.2, skip_attn=False, skip_rec=False, stop=99, k=None, io=None):
    NB = S // 512
    NKB = S // 128
    standalone = k is None
    if standalone:
        k = KB()
    V = lambda *a, **kw: _v(k, *a, **kw)
    NFG = 10 if odd else 12
    NTG = 4 if odd else 6
    NG = 6 if odd else 4
    NCV = 6 if odd else 4
    if standalone:
        hT = k.din("hT", [D, S])
        io = {"h": lambda blk: hT[:, blk * 512:(blk + 1) * 512].rearrange("(c p) t -> p c t", p=128), "h_res": []}
    gain_d = k.din("gain", [128, 8])
    wf_d = k.din("wf", [D, NFG * 128])
    wt_d = k.din("wt", [D, NTG * 128])
    wgt_d = k.din("wgt", [128, 8 * NG])
    gb_d = k.din("gbias", [1, NG])
    wout_d = k.din("wout", [512, D])
    convw_d = k.din("convw", [128, NCV * 4])
    nrm_d = k.din("nrm", [1, 128])
    ident_d = k.din("ident", [128, 128])
    U_d = k.din("U", [128, 128])
    BD_d = k.din("BD", [128, 128])
    MBu_d = k.din("MBu", [128, 128])
    MBl_d = k.din("MBl", [128, 128])
    mask_d = k.din("masks", [4, 128, 512])
    if odd:
        alog_d = k.din("alog", [1, 2])
        Uf_d = k.din("Uf", [128, 128])
    else:
        lamv_d = k.din("lamv", [1, 256])
        subln_d = k.din("subln", [128, 1])
        cos_d = k.din("cosT", [128, S])
        sin_d = k.din("sinT", [128, S])
    if standalone:
        partT = k.dout("partT", [D, S])
        io["out"] = lambda blk: partT[:, blk * 512:(blk + 1) * 512].rearrange("(c p) t -> p c t", p=128)
        io["out_res"] = Res("out")
    ores = io["out_res"]

    x32 = k.sb("x32", [128, 8, 512])
    hn16 = k.sb("hn16", [128, 8, 512], BF16)
    wf16 = k.sb("wf16", [128, 8, NFG * 128], BF16)
    wt16 = k.sb("wt16", [128, 8, NTG * 128], BF16)
    wout16 = k.sb("wout16", [128, 4, D], BF16)
    wg32 = k.sb("wg32", [128, 8 * NG])
    gbias = k.sb("gbias_s", [128, NG])
    gain = k.sb("gain_s", [128, 8])
    convw = k.sb("convw_s", [128, NCV * 4])
    nrmrep = k.sb("nrmrep", [128, 128])
    ident = k.sb("ident_s", [128, 128])
    Um = k.sb("U_s", [128, 128])
    BDm = k.sb("BD_s", [128, 128])
    MBu = k.sb("MBu_s", [128, 128])
    MBl = k.sb("MBl_s", [128, 128])
    masks = k.sb("masks_s", [128, 4, 512], F32 if odd else BF16)
    ones16 = k.sb("ones16", [128, 128], BF16)
    ones32 = k.sb("ones32", [128, 128])
    epsb = k.sb("epsb", [128, 1])
    eps1 = k.sb("eps1", [128, 1])
    onec = k.sb("onec", [128, 1])
    sq = [k.sb("sq%d" % i, [128, 512], BF16) for i in range(2)]
    rstd = k.sb("rstd", [128, 512])
    Xt = rstd
    kcache = [k.sb("kc%d" % i, [128, S], BF16) for i in range(2)]
    vcache = [k.sb("vc%d" % i, [128, NKB, 128], BF16) for i in range(2)]
    qa = [k.sb("qa%d" % i, [128, 512], BF16) for i in range(2)]
    oT = k.sb("oT", [128, 4, 512], BF16)
    Et = [k.sb("E%d" % i, [128, 512], BF16) for i in range(3)]
    Rt = [k.sb("R%d" % i, [128, 512]) for i in range(2)]
    rz = k.sb("rz", [128, 512])
    cvin = [k.sb("cvin%d" % i, [128, 515]) for i in range(NCV)]
    cvo = [k.sb("cvo%d" % i, [128, 512]) for i in range(NCV)]
    vaug = [k.sb("vaug%d" % i, [128, 4, 130] if not odd else [128, 2]) for i in range(2)]
    gtok = [k.sb("gtok%d" % i, [128, 4, 128]) for i in range(2)]
    gts = k.sb("gts", [128, 4, NG])
    lf = k.sb("lf", [128, 4, NG])
    gt2 = k.sb("gt2", [128, 4, NG])
    Sst = [[k.sb("S%d_%d" % (i, j), [128, 130]) for j in range(2)] for i in range(2)]
    qhat = [k.sb("qhat%d" % i, [128, 2, 128]) for i in range(2)]
    smh = [{n: k.sb("sm%d_" % hh_ + n, [128, w]) for n, w in
          [("lfb", 128), ("crep", 128), ("bc", 8), ("rcol", 1), ("e1", 1), ("Dm", 128), ("G", 128), ("ecr", 128), ("AT", 128),
           ("k2", 128), ("dm", 1), ("hh", 128), ("junk", 128), ("ssq", 1), ("rs", 1), ("hn", 128), ("sgo", 128), ("ob", 128)] +
          ([("Dl", 128), ("Gl", 128), ("N", 128), ("M", 128), ("P", 128), ("Mk", 128), ("Y", 128), ("vb", 128), ("kp", 128),
            ("u", 128), ("wT0", 128), ("wT1", 128), ("vn", 128), ("ktok", 128), ("bcol", 1), ("bebc", 1), ("ebd", 1), ("kd", 128),
            ("tmpc", 1)] if odd else [])} for hh_ in range(1)]
    smh = [smh[0], smh[0]]
    sm = smh[0]
    if odd:
        alog = k.sb("alog_s", [128, 2])
        Ufm = k.sb("Uf_s", [128, 128])
        carry = k.sb("carry", [128, 2])
        ncum = [k.sb("ncum%d" % i, [128, NKB]) for i in range(2)]
        Rq = [k.sb("Rq%d" % i, [1, 512]) for i in range(2)]
    else:
        lamv = k.sb("lamv_s", [128, 256])
        lamt = k.sb("lamt", [128, 8])
        subc = k.sb("subc", [128, 1])
        cost = k.sb("cost", [128, 512])
        sint = k.sb("sint", [128, 512])
        rt1 = Rt[0]
        rt2 = Rt[1]
    pj = [k.ps("pj%d" % i, [128, 512]) for i in range(2)]
    pst = [k.ps("pst%d" % i, [128, 512]) for i in range(2)]
    po = k.ps("po", [128, 512])
    pz = k.ps("pz", [128, 512])
    pr0 = k.ps("pr0", [128, 512])
    pr1 = k.ps("pr1", [128, 512])
    pA = Sub(pr0.t[:, 0:128], "pA", pr0.r)
    pF = Sub(pr0.t[:, 128:258], "pF", pr0.r)
    pD = Sub(pr0.t[:, 384:512], "pD", pr0.r)
    pC = Sub(pr1.t[:, 0:128], "pC", pr1.r)
    pG = Sub(pr1.t[:, 128:256], "pG", pr1.r)
    pH = Sub(pr1.t[:, 256:384], "pH", pr1.r)
    pEh = [Sub(pj[i_].t[:, 0:130], "pE%d" % i_, pj[i_].r) for i_ in range(2)]

    for t_, d_ in ((gain, gain_d), (wg32, wgt_d), (convw, convw_d), (ident, ident_d), (Um, U_d), (BDm, BD_d), (MBu, MBu_d), (MBl, MBl_d)):
        k.load(t_[:], d_[:, :], t_)
    k.load(gbias[:], gb_d.partition_broadcast(128), gbias)
    k.load(nrmrep[:], nrm_d.partition_broadcast(128), nrmrep)
    V("dve", "memset", [], [ones16], ap=ones16[:], constant=1.0)
    V("dve", "memset", [], [ones32], ap=ones32[:], constant=1.0)
    V("dve", "memset", [], [epsb], ap=epsb[:], constant=float(D * RMS_EPS))
    V("dve", "memset", [], [eps1], ap=eps1[:], constant=float(RMS_EPS))
    V("dve", "memset", [], [onec], ap=onec[:], constant=1.0)
    V("dve", "tensor_scalar_mul", [gain], [gain], out=gain[:], in0=gain[:], scalar1=32.0)
    for i in range(2):
        V("dve", "memset", [], [qhat[i]], ap=qhat[i][:], constant=0.0)
        V("dve", "memset", [], [vaug[i]], ap=vaug[i][:], constant=1.0)
        for j in range(2):
            V("dve", "memset", [], [Sst[i][j]], ap=Sst[i][j][:], constant=0.0)
    for c_ in cvin:
        V("dve", "memset", [], [c_], ap=c_[:], constant=0.0)
    V("dve", "memset", [], [lf], ap=lf[:], constant=0.0)
    if odd:
        k.load(alog[:], alog_d.partition_broadcast(128), alog)
        k.load(Ufm[:], Uf_d[:, :], Ufm)
        V("act", "activation", [alog], [alog], out=alog[:], in_=alog[:], func=AF.Exp)
        V("dve", "tensor_scalar_mul", [alog], [alog], out=alog[:], in0=alog[:], scalar1=-1.0)
        V("dve", "memset", [], [carry], ap=carry[:], constant=0.0)
    else:
        k.load(lamv[:], lamv_d.partition_broadcast(128), lamv)
        k.load(subc[:], subln_d[:, :], subc)
        V("dve", "tensor_scalar_mul", [subc], [subc], out=subc[:], in0=subc[:], scalar1=float(1.0 - lam_init))
        V("dve", "tensor_tensor", [lamv], [lamv], out=lamv[:, 0:64], in0=lamv[:, 0:64], in1=lamv[:, 64:128], op=ALU.mult)
        V("dve", "tensor_tensor", [lamv], [lamv], out=lamv[:, 128:192], in0=lamv[:, 128:192], in1=lamv[:, 192:256], op=ALU.mult)
        V("dve", "reduce_sum", [lamv], [lamt], out=lamt[:, 0:1], in_=lamv[:, 0:64], axis=AX.X)
        V("dve", "reduce_sum", [lamv], [lamt], out=lamt[:, 1:2], in_=lamv[:, 128:192], axis=AX.X)
        V("act", "activation", [lamt], [lamt], out=lamt[:, 2:4], in_=lamt[:, 0:2], func=AF.Exp)
        V("dve", "tensor_tensor", [lamt], [lamt], out=lamt[:, 4:5], in0=lamt[:, 3:4], in1=lamt[:, 2:3], op=ALU.subtract)
        V("dve", "tensor_scalar_add", [lamt], [lamt], out=lamt[:, 4:5], in0=lamt[:, 4:5], scalar1=float(-lam_init))
    k.load(x32[:, 0:4, :], mask_d.rearrange("j p t -> p j t"), x32)
    if odd:
        V("dve", "tensor_scalar", [x32], [masks], out=masks[:], in0=x32[:, 0:4, :], scalar1=-1.0, scalar2=30000.0, op0=ALU.add, op1=ALU.mult)
    else:
        V("act", "copy", [x32], [masks], out=masks[:], in_=x32[:, 0:4, :])

    def load_w(dst, dcols, src, rows0, nrows, ncols, col0):
        nk = nrows // 128
        st = x32[:].rearrange("p a b -> p (a b)")[:, 0:nk * ncols].rearrange("p (a b) -> p a b", a=nk)
        k.load(st, src[rows0:rows0 + nrows, col0:col0 + ncols].rearrange("(a p) n -> p a n", p=128), x32)
        V("act", "copy", [x32], [dst], out=dcols, in_=st)

    for g in range(NFG):
        for h2 in range(2):
            load_w(wf16, wf16[:, h2 * 4:(h2 + 1) * 4, g * 128:(g + 1) * 128], wf_d, h2 * 512, 512, 128, g * 128)
    for g in range(NTG):
        for h2 in range(2):
            load_w(wt16, wt16[:, h2 * 4:(h2 + 1) * 4, g * 128:(g + 1) * 128], wt_d, h2 * 512, 512, 128, g * 128)
    for hh_ in range(4):
        load_w(wout16, wout16[:, hh_:hh_ + 1, :], wout_d, hh_ * 128, 128, D, 0)

    pji = [0]

    def proj_fm(g):
        p = pj[pji[0] % 2]
        pji[0] += 1
        for c in range(8):
            k.mm(p[:], wf16[:, c, g * 128:(g + 1) * 128], hn16[:, c], c == 0, c == 7, [wf16, hn16], p)
        return p

    def proj_tm(g):
        p = pj[pji[0] % 2]
        pji[0] += 1
        for t4 in range(4):
            for c in range(8):
                k.mm(p[:, t4 * 128:(t4 + 1) * 128], hn16[:, c, t4 * 128:(t4 + 1) * 128], wt16[:, c, g * 128:(g + 1) * 128],
                     c == 0, c == 7, [wt16, hn16], p)
        return p

    def conv_silu(p, ci, blk):
        xi = cvin[ci]
        V("act", "copy", [p], [xi], out=xi[:, 3:515], in_=p[:])
        o = cvo[ci]
        V("dve", "tensor_scalar_mul", [xi, convw], [o], out=o[:], in0=xi[:, 0:512], scalar1=convw[:, ci * 4:ci * 4 + 1])
        for j in range(1, 4):
            V("dve", "scalar_tensor_tensor", [xi, convw, o], [o], out=o[:], in0=xi[:, j:j + 512],
              scalar=convw[:, ci * 4 + j:ci * 4 + j + 1], in1=o[:], op0=ALU.mult, op1=ALU.add)
        V("act", "activation", [o], [o], out=o[:], in_=o[:], func=AF.Silu)
        V("dve", "tensor_copy", [xi], [xi], out=xi[:, 0:3], in_=xi[:, 512:515])
        return o

    def l2n(o, scale):
        V("act", "activation", [o], [sq[0]], out=sq[0][:], in_=o[:], func=AF.Square)
        k.mm(pz[:], ones16[:], sq[0][:], True, True, [ones16, sq[0]], pz)
        V("act", "activation", [pz, eps1], [rz], out=rz[:], in_=pz[:], func=AF.Sqrt, bias=eps1[:, 0:1], scale=1.0)
        V("dve", "reciprocal", [rz], [rz], out=rz[:], in_=rz[:])
        V("dve", "scalar_tensor_tensor", [o, rz], [o], out=o[:], in0=o[:], scalar=float(scale), in1=rz[:], op0=ALU.mult, op1=ALU.mult)

    sti = [0]
    ei = [0]

    def attn(i, blk, qparts, scale, bias_rows=None):
        outs = []
        nkb = 4 * blk + 4
        for ci, psl in enumerate(qparts):
            for kb in range(nkb):
                st = pst[sti[0] % 2]
                sti[0] += 1
                k.mm(st[:], kcache[i][psl, kb * 128:(kb + 1) * 128], qa[i][psl, :], True, bias_rows is None, [kcache[i], qa[i]], st)
                E = Et[ei[0] % 3]
                ei[0] += 1
                if bias_rows is not None:
                    nc_, rq_ = bias_rows
                    k.mm(st[:], ones32[0:1, 0:128], rq_[0:1, :], False, True, [ones32, rq_], st)
                    if kb >= 4 * blk:
                        V("dve", "tensor_tensor", [st, masks], [rz], out=rz[:], in0=st[:], in1=masks[:, kb - 4 * blk, :], op=ALU.add)
                        V("act", "activation", [rz, nc_], [E], out=E[:], in_=rz[:], func=AF.Exp, bias=nc_[:, kb:kb + 1], scale=float(scale))
                    else:
                        V("act", "activation", [st, nc_], [E], out=E[:], in_=st[:], func=AF.Exp, bias=nc_[:, kb:kb + 1], scale=float(scale))
                else:
                    V("act", "activation", [st], [E], out=E[:], in_=st[:], func=AF.Exp, scale=float(scale))
                    if kb >= 4 * blk:
                        V("dve", "tensor_tensor", [E, masks], [E], out=E[:], in0=E[:], in1=masks[:, kb - 4 * blk, :], op=ALU.mult)
                k.mm(po[:], vcache[i][:, kb, :], E[:], kb == 0, kb == nkb - 1, [vcache[i], E], po)
                k.mm(pz[:], ones16[:], E[:], kb == 0, kb == nkb - 1, [ones16, E], pz)
            R = Rt[ci]
            V("dve", "reciprocal", [pz], [rz], out=rz[:], in_=pz[:])
            V("dve", "tensor_tensor", [po, rz], [R], out=R[:], in0=po[:], in1=rz[:], op=ALU.mult)
            outs.append(R)
        return outs

    def decay_prep(sm, lfcol, lf2, i):
        V("dve", "tensor_scalar_mul", [ones32, lf, gts], [sm["lfb"]], out=sm["lfb"][:], in0=ones32[:], scalar1=lfcol)
        k.mm(pA[:], sm["lfb"][:], Um[:], True, True, [sm["lfb"], Um], pA)
        V("act", "copy", [pA], [sm["crep"]], out=sm["crep"][:], in_=pA[:])
        V("dve", "tensor_tensor", [sm["crep"], ident], [sm["junk"]], out=sm["junk"][:], in0=sm["crep"][:], in1=ident[:], op=ALU.mult)
        V("dve", "reduce_sum", [sm["junk"]], [sm["bc"]], out=sm["bc"][:, 0:1], in_=sm["junk"][:], axis=AX.X)
        V("dve", "tensor_copy", [sm["crep"]], [sm["bc"]], out=sm["bc"][0:64, 1:2], in_=sm["crep"][0:64, 63:64])
        V("dve", "tensor_copy", [sm["crep"]], [sm["bc"]], out=sm["bc"][64:128, 1:2], in_=sm["crep"][64:128, 127:128])
        V("act", "activation", [sm["crep"]], [sm["ecr"]], out=sm["ecr"][:], in_=sm["crep"][:], func=AF.Exp)

    def post_out(sm, i, t4, src_ps, gate_func):
        V("act", "activation", [sm["hh"]], [sm["junk"], sm["ssq"]], out=sm["junk"][:], in_=sm["hh"][:], func=AF.Square,
          accum_out=sm["ssq"][:])
        V("act", "activation", [sm["ssq"], eps1], [sm["rs"]], out=sm["rs"][:], in_=sm["ssq"][:], func=AF.Sqrt, bias=eps1[:, 0:1],
          scale=1.0 / 128.0)
        V("dve", "reciprocal", [sm["rs"]], [sm["rs"]], out=sm["rs"][:], in_=sm["rs"][:])
        V("dve", "scalar_tensor_tensor", [sm["hh"], sm["rs"], nrmrep], [sm["hn"]], out=sm["hn"][:], in0=sm["hh"][:],
          scalar=sm["rs"][:, 0:1], in1=nrmrep[:], op0=ALU.mult, op1=ALU.mult)
        V("act", "activation", [gtok[i]], [sm["sgo"]], out=sm["sgo"][:], in_=gtok[i][:, t4, :], func=gate_func)
        V("dve", "tensor_tensor", [sm["hn"], sm["sgo"]], [sm["ob"]], out=sm["ob"][:], in0=sm["hn"][:], in1=sm["sgo"][:], op=ALU.mult)
        k.tr(pG[:], sm["ob"][:], ident[:], [sm["ob"], ident], pG)
        V("act", "copy", [pG], [oT], out=oT[:, 2 + i, t4 * 128:(t4 + 1) * 128], in_=pG[:])

    def record(fn):
        lst = []
        o_op, o_dma = k.S.op, k.S.dma
        k.S.op = lambda *a_, **kw_: lst.append((o_op, a_, kw_))
        k.S.dma = lambda *a_, **kw_: lst.append((o_dma, a_, kw_))
        try:
            fn()
        finally:
            k.S.op, k.S.dma = o_op, o_dma
        return lst

    def interleave(lists):
        lists = [l_ for l_ in lists if l_]
        if not lists:
            return
        import os
        if os.environ.get("MK_SEQ"):
            for l_ in lists:
                for f_, a_, kw_ in l_:
                    f_(*a_, **kw_)
            return
        n = max(len(l_) for l_ in lists)
        pos = [0] * len(lists)
        for tick in range(1, n + 1):
            for li, l_ in enumerate(lists):
                tgt = (tick * len(l_) + n - 1) // n
                while pos[li] < min(tgt, len(l_)):
                    f_, a_, kw_ = l_[pos[li]]
                    f_(*a_, **kw_)
                    pos[li] += 1

    V("dve", "memset", [], [oT], ap=oT[:], constant=0.0)
    for blk in range(NB):
        tok = slice(blk * 512, (blk + 1) * 512)
        if stop == 0:
            k.store(io["out"](blk), x32[:], x32, ores)
            continue
        k.load(x32[:], io["h"](blk), x32, reads=io["h_res"])
        for c in range(8):
            s_ = sq[c % 2]
            V("act", "activation", [x32], [s_], out=s_[:], in_=x32[:, c], func=AF.Square)
            k.mm(pz[:], ones16[:], s_[:], c == 0, c == 7, [ones16, s_], pz)
        V("act", "activation", [pz, epsb], [rstd], out=rstd[:], in_=pz[:], func=AF.Sqrt, bias=epsb[:, 0:1], scale=1.0)
        V("dve", "reciprocal", [rstd], [rstd], out=rstd[:], in_=rstd[:])
        for c in range(8):
            V("dve", "scalar_tensor_tensor", [x32, gain, rstd], [x32], out=x32[:, c], in0=x32[:, c], scalar=gain[:, c:c + 1],
              in1=rstd[:], op0=ALU.mult, op1=ALU.mult)
        V("act", "copy", [x32], [hn16], out=hn16[:], in_=x32[:])
        if stop == 1:
            k.store(io["out"](blk), x32[:], x32, ores)
            continue
        for t4 in range(4):
            for c in range(8):
                k.mm(pH[:, t4 * NG:(t4 + 1) * NG], x32[:, c, t4 * 128:(t4 + 1) * 128], wg32[:, c * NG:(c + 1) * NG], c == 0, c == 7,
                     [x32, wg32], pH)
        V("dve", "tensor_tensor", [pH, gbias], [gts], out=gts[:], in0=pH[:, 0:4 * NG].rearrange("p (a b) -> p a b", a=4),
          in1=gbias[:].unsqueeze(1).to_broadcast([128, 4, NG]), op=ALU.add)

        if stop == 2:
            k.store(io["out"](blk), x32[:], x32, ores)
            continue
        if not odd:
            k.load(cost[:], cos_d[:, tok], cost)
            k.load(sint[:], sin_d[:, tok], sint)
            for i in range(2):
                for which, dst in ((0, qa[i]), (2, None)):
                    p = proj_fm(i * 4 + which)
                    V("dve", "tensor_tensor", [p, cost], [rt1], out=rt1[:], in0=p[:], in1=cost[:], op=ALU.mult)
                    p2 = proj_fm(i * 4 + which + 1)
                    V("dve", "tensor_tensor", [p2, sint], [rt2], out=rt2[:], in0=p2[:], in1=sint[:], op=ALU.mult)
                    if dst is not None:
                        V("dve", "tensor_tensor", [rt1, rt2], [dst], out=dst[:], in0=rt1[:], in1=rt2[:], op=ALU.add)
                    else:
                        V("dve", "tensor_tensor", [rt1, rt2], [kcache[i]], out=kcache[i][:, tok], in0=rt1[:], in1=rt2[:], op=ALU.add)
                p = proj_tm(i)
                V("act", "copy", [p], [vcache[i]], out=vcache[i][:, blk * 4:(blk + 1) * 4, :], in_=p[:].rearrange("p (a b) -> p a b", a=4))
            V("act", "activation", [gts], [gt2], out=gt2[:, :, 2:4], in_=gts[:, :, 2:4], func=AF.Exp, scale=-1.0)
            V("act", "activation", [gt2, onec], [gt2], out=gt2[:, :, 2:4], in_=gt2[:, :, 2:4], func=AF.Ln, bias=onec[:, 0:1], scale=1.0)
            V("dve", "tensor_scalar_mul", [gt2], [lf], out=lf[:, :, 2:4], in0=gt2[:, :, 2:4], scalar1=-1.0)
            def att_task():
                for i in range(0 if skip_attn else 2):
                    R = attn(i, blk, [slice(0, 64), slice(64, 128)], 64 ** -0.5)
                    V("dve", "scalar_tensor_tensor", [R[0], R[1], lamt], [Xt], out=Xt[:], in0=R[1][:], scalar=lamt[:, 4:5], in1=R[0][:],
                      op0=ALU.mult, op1=ALU.add)
                    V("act", "activation", [Xt], [sq[0]], out=sq[0][:], in_=Xt[:], func=AF.Square)
                    k.mm(pz[:], ones16[:], sq[0][:], True, True, [ones16, sq[0]], pz)
                    V("act", "activation", [pz, eps1], [rz], out=rz[:], in_=pz[:], func=AF.Sqrt, bias=eps1[:, 0:1], scale=1.0 / 128.0)
                    V("dve", "reciprocal", [rz], [rz], out=rz[:], in_=rz[:])
                    V("dve", "scalar_tensor_tensor", [Xt, subc, rz], [oT], out=oT[:, i, :], in0=Xt[:], scalar=subc[:, 0:1], in1=rz[:],
                      op0=ALU.mult, op1=ALU.mult)

            preps = {}
            for i in range(0 if skip_rec else 2):
                qc = conv_silu(proj_fm(8 + 2 * i), 2 * i, blk)
                kc = conv_silu(proj_fm(8 + 2 * i + 1), 2 * i + 1, blk)
                p = proj_tm(2 + 2 * i)
                V("act", "copy", [p], [vaug[i]], out=vaug[i][:, :, 0:128], in_=p[:].rearrange("p (a b) -> p a b", a=4))
                p = proj_tm(2 + 2 * i + 1)
                V("act", "copy", [p], [gtok[i]], out=gtok[i][:], in_=p[:].rearrange("p (a b) -> p a b", a=4))
                preps[i] = (qc, kc)

            def rec_task(i):
                qc, kc = preps[i]
                sm = smh[i]
                pE = pEh[i]
                for t4 in range(4 if stop > 10 else 0):
                    cs = slice(t4 * 128, (t4 + 1) * 128)
                    decay_prep(sm, lf[:, t4, 2 + i:3 + i], lf[:, t4, 0:4], 2 + i)
                    if stop == 105:
                        continue
                    V("dve", "tensor_tensor", [sm["bc"], gts], [sm["rcol"]], out=sm["rcol"][:], in0=sm["bc"][:, 0:1], in1=gts[:, t4, i:i + 1],
                      op=ALU.subtract)
                    V("dve", "tensor_tensor", [sm["bc"], sm["rcol"]], [sm["e1"]], out=sm["e1"][:], in0=sm["bc"][:, 1:2], in1=sm["rcol"][:],
                      op=ALU.subtract)
                    V("act", "activation", [sm["e1"]], [sm["e1"]], out=sm["e1"][:], in_=sm["e1"][:], func=AF.Exp)
                    V("dve", "tensor_scalar_mul", [sm["e1"]], [sm["e1"]], out=sm["e1"][:], in0=sm["e1"][:], scalar1=float(128 ** -0.5))
                    if stop == 11:
                        continue
                    V("dve", "scalar_tensor_tensor", [sm["crep"], sm["rcol"], MBu], [sm["Dm"]], out=sm["Dm"][:], in0=sm["crep"][:],
                      scalar=sm["rcol"][:, 0:1], in1=MBu[:], op0=ALU.subtract, op1=ALU.add)
                    V("act", "activation", [sm["Dm"]], [sm["G"]], out=sm["G"][:], in_=sm["Dm"][:], func=AF.Exp)
                    V("dve", "tensor_tensor", [qc, sm["ecr"]], [qhat[i]], out=qhat[i][:, 0, 0:64], in0=qc[:, t4 * 128:t4 * 128 + 64],
                      in1=sm["ecr"][:, 0:64], op=ALU.mult)
                    V("dve", "tensor_tensor", [qc, sm["ecr"]], [qhat[i]], out=qhat[i][:, 1, 64:128], in0=qc[:, t4 * 128 + 64:(t4 + 1) * 128],
                      in1=sm["ecr"][:, 64:128], op=ALU.mult)
                    k.mm(pC[:], kc[:, cs], qc[:, cs], True, True, [kc, qc], pC)
                    V("dve", "scalar_tensor_tensor", [pC, sm["G"]], [sm["AT"]], out=sm["AT"][:], in0=pC[:], scalar=float(128 ** -0.5),
                      in1=sm["G"][:], op0=ALU.mult, op1=ALU.mult)
                    if stop == 12:
                        continue
                    k.tr(pD[:], kc[:, cs], ident[:], [kc, ident], pD)
                    V("dve", "tensor_scalar_mul", [pD, sm["e1"]], [sm["k2"]], out=sm["k2"][:], in0=pD[:], scalar1=sm["e1"][:, 0:1])
                    if stop == 13:
                        continue
                    S0, S1 = Sst[i]
                    k.mm(pE[:], sm["AT"][:], vaug[i][:, t4, :], True, False, [sm["AT"], vaug[i]], pE)
                    k.mm(pE[:], qhat[i][:, 0, :], S0[:], False, False, [qhat[i], S0], pE)
                    k.mm(pF[:], sm["k2"][0:64, :], vaug[i][0:64, t4, :], True, True, [sm["k2"], vaug[i]], pF)
                    V("dve", "scalar_tensor_tensor", [S0, sm["ecr"], pF], [S1], out=S1[:], in0=S0[:], scalar=sm["ecr"][:, 63:64], in1=pF[:],
                      op0=ALU.mult, op1=ALU.add)
                    k.mm(pE[:], qhat[i][:, 1, :], S1[:], False, True, [qhat[i], S1], pE)
                    k.mm(pF[:], sm["k2"][64:128, :], vaug[i][64:128, t4, :], True, True, [sm["k2"], vaug[i]], pF)
                    V("dve", "scalar_tensor_tensor", [S1, sm["ecr"], pF], [S0], out=S0[:], in0=S1[:], scalar=sm["ecr"][:, 127:128], in1=pF[:],
                      op0=ALU.mult, op1=ALU.add)
                    if stop == 14:
                        continue
                    V("act", "activation", [pE], [sm["dm"]], out=sm["dm"][:], in_=pE[:, 128:129], func=AF.Abs)
                    V("dve", "tensor_scalar_max", [sm["dm"]], [sm["dm"]], out=sm["dm"][:], in0=sm["dm"][:], scalar1=1.0)
                    V("dve", "reciprocal", [sm["dm"]], [sm["dm"]], out=sm["dm"][:], in_=sm["dm"][:])
                    V("dve", "tensor_scalar_mul", [pE, sm["dm"]], [sm["hh"]], out=sm["hh"][:], in0=pE[:, 0:128], scalar1=sm["dm"][:, 0:1])
                    if stop == 15:
                        continue
                    post_out(sm, i, t4, None, AF.Sigmoid)

            def rec_all():
                for i in range(0 if skip_rec else 2):
                    rec_task(i)
            tl = [record(att_task), record(rec_all)]
            interleave(tl)
        else:
            V("act", "activation", [gts], [gt2], out=gt2[:, :, 0:2], in_=gts[:, :, 0:2], func=AF.Exp)
            V("act", "activation", [gts], [gt2], out=gt2[:, :, 4:6], in_=gts[:, :, 4:6], func=AF.Exp, scale=-1.0)
            V("act", "activation", [gt2, onec], [gt2], out=gt2[:, :, 0:2], in_=gt2[:, :, 0:2], func=AF.Ln, bias=onec[:, 0:1], scale=1.0)
            V("act", "activation", [gt2, onec], [gt2], out=gt2[:, :, 4:6], in_=gt2[:, :, 4:6], func=AF.Ln, bias=onec[:, 0:1], scale=1.0)
            V("dve", "tensor_tensor", [gt2, alog], [lf], out=lf[:, :, 0:2], in0=gt2[:, :, 0:2],
              in1=alog[:].unsqueeze(1).to_broadcast([128, 4, 2]), op=ALU.mult)
            V("dve", "tensor_scalar_mul", [gt2], [lf], out=lf[:, :, 4:6], in0=gt2[:, :, 4:6], scalar1=-1.0)
            V("act", "activation", [gts], [lf], out=lf[:, :, 2:4], in_=gts[:, :, 2:4], func=AF.Sigmoid)
            for t4 in range(4):
                for i in range(2):
                    V("dve", "tensor_scalar_mul", [ones32, lf], [sm["lfb"]], out=sm["lfb"][:], in0=ones32[:], scalar1=lf[:, t4, 4 + i:5 + i])
                    k.mm(pA[:], sm["lfb"][:], Ufm[:], True, True, [sm["lfb"], Ufm], pA)
                    V("dve", "tensor_scalar", [pA, carry], [Rq[i]], out=Rq[i][0:1, t4 * 128:(t4 + 1) * 128], in0=pA[0:1, :],
                      scalar1=carry[0:1, i:i + 1], scalar2=None, op0=ALU.add)
                    V("dve", "tensor_tensor", [pA, ident], [sm["junk"]], out=sm["junk"][:], in0=pA[:], in1=ident[:], op=ALU.mult)
                    V("dve", "reduce_sum", [sm["junk"]], [sm["tmpc"]], out=sm["tmpc"][:], in_=sm["junk"][:], axis=AX.X)
                    V("dve", "tensor_scalar", [sm["tmpc"], carry], [ncum[i]], out=ncum[i][:, blk * 4 + t4:blk * 4 + t4 + 1], in0=sm["tmpc"][:],
                      scalar1=carry[:, i:i + 1], scalar2=-1.0, op0=ALU.add, op1=ALU.mult)
                    V("dve", "tensor_tensor", [pA, carry], [carry], out=carry[:, i:i + 1], in0=carry[:, i:i + 1], in1=pA[:, 127:128], op=ALU.add)
            for i in range(2):
                p = proj_fm(6 + 2 * i)
                V("act", "mul", [p], [qa[i]], out=qa[i][:], in_=p[:], mul=float(128 ** -0.5))
                p = proj_fm(6 + 2 * i + 1)
                V("act", "copy", [p], [kcache[i]], out=kcache[i][:, tok], in_=p[:])
                p = proj_tm(2 + i)
                V("act", "copy", [p], [vcache[i]], out=vcache[i][:, blk * 4:(blk + 1) * 4, :], in_=p[:].rearrange("p (a b) -> p a b", a=4))
            def att_task():
                for i in range(0 if skip_attn else 2):
                    R = attn(i, blk, [slice(0, 128)], 1.0, bias_rows=(ncum[i], Rq[i]))
                    V("act", "copy", [R[0]], [oT], out=oT[:, i, :], in_=R[0][:])

            preps = {}
            for i in range(0 if skip_rec else 2):
                qc = conv_silu(proj_fm(3 * i), 3 * i, blk)
                kc = conv_silu(proj_fm(3 * i + 1), 3 * i + 1, blk)
                vc = conv_silu(proj_fm(3 * i + 2), 3 * i + 2, blk)
                l2n(qc, 128 ** -0.5)
                l2n(kc, 1.0)
                p = proj_tm(i)
                V("act", "copy", [p], [gtok[i]], out=gtok[i][:], in_=p[:].rearrange("p (a b) -> p a b", a=4))
                preps[i] = (qc, kc, vc)

            def rec_task(i):
                qc, kc, vc = preps[i]
                sm = smh[i]
                pE = pEh[i]
                for t4 in range(4):
                    cs = slice(t4 * 128, (t4 + 1) * 128)
                    decay_prep(sm, lf[:, t4, i:i + 1], lf[:, t4, 0:4], i)
                    bcol = sm["bc"][:, 0:1]
                    beta = lf[:, t4, 2 + i:3 + i]
                    V("dve", "scalar_tensor_tensor", [sm["crep"], sm["bc"], MBu], [sm["Dm"]], out=sm["Dm"][:], in0=sm["crep"][:],
                      scalar=bcol, in1=MBu[:], op0=ALU.subtract, op1=ALU.add)
                    V("act", "activation", [sm["Dm"]], [sm["G"]], out=sm["G"][:], in_=sm["Dm"][:], func=AF.Exp)
                    V("dve", "scalar_tensor_tensor", [sm["crep"], sm["bc"], MBl], [sm["Dl"]], out=sm["Dl"][:], in0=sm["crep"][:],
                      scalar=bcol, in1=MBl[:], op0=ALU.subtract, op1=ALU.add)
                    V("act", "activation", [sm["Dl"]], [sm["Gl"]], out=sm["Gl"][:], in_=sm["Dl"][:], func=AF.Exp, scale=-1.0)
                    k.tr(pD[:], kc[:, cs], ident[:], [kc, ident], pD)
                    V("act", "copy", [pD], [sm["ktok"]], out=sm["ktok"][:], in_=pD[:])
                    k.tr(pD[:], vc[:, cs], ident[:], [vc, ident], pD)
                    V("dve", "tensor_scalar_mul", [pD, lf], [sm["vb"]], out=sm["vb"][:], in0=pD[:], scalar1=beta)
                    V("act", "activation", [sm["bc"]], [sm["bcol"]], out=sm["bcol"][:], in_=sm["bc"][:, 0:1], func=AF.Exp)
                    V("dve", "tensor_tensor", [sm["bcol"], lf], [sm["bebc"]], out=sm["bebc"][:], in0=sm["bcol"][:], in1=beta, op=ALU.mult)
                    V("dve", "tensor_tensor", [sm["bc"]], [sm["ebd"]], out=sm["ebd"][:], in0=sm["bc"][:, 1:2], in1=sm["bc"][:, 0:1],
                      op=ALU.subtract)
                    V("act", "activation", [sm["ebd"]], [sm["ebd"]], out=sm["ebd"][:], in_=sm["ebd"][:], func=AF.Exp)
                    V("dve", "tensor_scalar_mul", [sm["ktok"], sm["bebc"]], [sm["kp"]], out=sm["kp"][:], in0=sm["ktok"][:],
                      scalar1=sm["bebc"][:, 0:1])
                    V("dve", "tensor_scalar_mul", [sm["ktok"], sm["ebd"]], [sm["kd"]], out=sm["kd"][:], in0=sm["ktok"][:],
                      scalar1=sm["ebd"][:, 0:1])
                    k.mm(pC[:], kc[:, cs], kc[:, cs], True, True, [kc], pC)
                    V("dve", "scalar_tensor_tensor", [pC, lf, sm["Gl"]], [sm["N"]], out=sm["N"][:], in0=pC[:], scalar=beta, in1=sm["Gl"][:],
                      op0=ALU.mult, op1=ALU.mult)
                    k.tr(pC[:], sm["N"][:], ident[:], [sm["N"], ident], pC)
                    V("act", "copy", [pC], [sm["M"]], out=sm["M"][:], in_=pC[:])
                    V("dve", "tensor_tensor", [ident, sm["M"]], [sm["Y"]], out=sm["Y"][:], in0=ident[:], in1=sm["M"][:], op=ALU.subtract)
                    Pc, Mc = sm["N"], sm["M"]
                    Pn, Mn = sm["P"], sm["Mk"]
                    for lev in range(5):
                        k.mm(pC[:], Mc[:], Pc[:], True, True, [Mc, Pc], pC)
                        if lev < 4:
                            k.mm(pD[:], Pc[:], Mc[:], True, True, [Mc, Pc], pD)
                        V("act", "copy", [pC], [Pn], out=Pn[:], in_=pC[:])
                        if lev < 4:
                            V("dve", "tensor_copy", [pD], [Mn], out=Mn[:], in_=pD[:])
                        k.mm(pA[:], Pn[:], sm["Y"][:], True, True, [Pn, sm["Y"]], pA)
                        V("dve", "tensor_tensor", [sm["Y"], pA], [sm["Y"]], out=sm["Y"][:], in0=sm["Y"][:], in1=pA[:], op=ALU.add)
                        Pc, Pn = Pn, Pc
                        Mc, Mn = Mn, Mc
                    k.mm(pC[:], sm["Y"][:], sm["vb"][:], True, True, [sm["Y"], sm["vb"]], pC)
                    V("act", "copy", [pC], [sm["u"]], out=sm["u"][:], in_=pC[:])
                    k.mm(pD[:], sm["kp"][:], sm["Y"][:], True, True, [sm["Y"], sm["kp"]], pD)
                    V("dve", "memset", [], [sm["wT0"]], ap=sm["wT0"][:], constant=0.0)
                    V("dve", "memset", [], [sm["wT1"]], ap=sm["wT1"][:], constant=0.0)
                    V("dve", "tensor_copy", [pD], [sm["wT0"]], out=sm["wT0"][:, 0:64], in_=pD[:, 0:64])
                    V("dve", "tensor_copy", [pD], [sm["wT1"]], out=sm["wT1"][:, 64:128], in_=pD[:, 64:128])
                    k.mm(pC[:], kc[:, cs], qc[:, cs], True, True, [kc, qc], pC)
                    V("dve", "tensor_tensor", [pC, sm["G"]], [sm["AT"]], out=sm["AT"][:], in0=pC[:], in1=sm["G"][:], op=ALU.mult)
                    V("dve", "tensor_tensor", [qc, sm["ecr"]], [qhat[i]], out=qhat[i][:, 0, 0:64], in0=qc[:, t4 * 128:t4 * 128 + 64],
                      in1=sm["ecr"][:, 0:64], op=ALU.mult)
                    V("dve", "tensor_tensor", [qc, sm["ecr"]], [qhat[i]], out=qhat[i][:, 1, 64:128], in0=qc[:, t4 * 128 + 64:(t4 + 1) * 128],
                      in1=sm["ecr"][:, 64:128], op=ALU.mult)
                    S0, S1 = Sst[i]
                    k.mm(pA[:], sm["wT0"][:], S0[:, 0:128], True, True, [sm["wT0"], S0], pA)
                    V("dve", "tensor_tensor", [sm["u"], pA], [sm["vn"]], out=sm["vn"][0:64, :], in0=sm["u"][0:64, :], in1=pA[0:64, :],
                      op=ALU.subtract)
                    k.mm(pE[:, 0:128], qhat[i][:, 0, :], S0[:, 0:128], True, False, [qhat[i], S0], pE)
                    k.mm(pF[:, 0:128], sm["kd"][0:64, :], sm["vn"][0:64, :], True, True, [sm["kd"], sm["vn"]], pF)
                    V("dve", "scalar_tensor_tensor", [S0, sm["ecr"], pF], [S1], out=S1[:, 0:128], in0=S0[:, 0:128], scalar=sm["ecr"][:, 63:64],
                      in1=pF[:, 0:128], op0=ALU.mult, op1=ALU.add)
                    k.mm(pA[:], sm["wT1"][:], S1[:, 0:128], True, True, [sm["wT1"], S1], pA)
                    V("dve", "tensor_tensor", [sm["u"], pA], [sm["vn"]], out=sm["vn"][64:128, :], in0=sm["u"][64:128, :], in1=pA[64:128, :],
                      op=ALU.subtract)
                    k.mm(pE[:, 0:128], qhat[i][:, 1, :], S1[:, 0:128], False, False, [qhat[i], S1], pE)
                    k.mm(pE[:, 0:128], sm["AT"][:], sm["vn"][:], False, True, [sm["AT"], sm["vn"]], pE)
                    k.mm(pF[:, 0:128], sm["kd"][64:128, :], sm["vn"][64:128, :], True, True, [sm["kd"], sm["vn"]], pF)
                    V("dve", "scalar_tensor_tensor", [S1, sm["ecr"], pF], [S0], out=S0[:, 0:128], in0=S1[:, 0:128], scalar=sm["ecr"][:, 127:128],
                      in1=pF[:, 0:128], op0=ALU.mult, op1=ALU.add)
                    V("act", "copy", [pE], [sm["hh"]], out=sm["hh"][:], in_=pE[:, 0:128])
                    post_out(sm, i, t4, None, AF.Silu)


            def rec_all():
                for i in range(0 if skip_rec else 2):
                    rec_task(i)
            tl = [record(att_task), record(rec_all)]
            interleave(tl)

        for c in range(8):
            p = pj[pji[0] % 2]
            pji[0] += 1
            for hs in range(4):
                k.mm(p[:], wout16[:, hs, c * 128:(c + 1) * 128], oT[:, hs, :], hs == 0, hs == 3, [wout16, oT], p)
            V("act", "copy", [p], [x32], out=x32[:, c], in_=p[:])
        k.store(io["out"](blk), x32[:], x32, ores)
    if not standalone:
        return None, k
    nc = k.finish([ores])
    return nc, k


def _kc(w):
    n = w.shape[1]
    return np.ascontiguousarray(w.reshape(8, 128, n).transpose(1, 0, 2).reshape(128, 8 * n))


def mix_consts(S, odd):
    idx = np.arange(128)
    same = (idx[:, None] // 64) == (idx[None, :] // 64)
    U = ((idx[:, None] <= idx[None, :]) & same).astype(np.float32)
    BD = same.astype(np.float32)
    MBu = np.where((idx[None, :] >= idx[:, None]) & same, 0.0, -30000.0).astype(np.float32)
    MBl = np.where((idx[:, None] > idx[None, :]) & same, 0.0, 30000.0).astype(np.float32)
    Uf = (idx[:, None] <= idx[None, :]).astype(np.float32)
    t = np.arange(512)
    masks = np.zeros((4, 128, 512), np.float32)
    for j in range(4):
        key = 128 * j + idx
        if odd:
            masks[j] = (key[:, None] <= t[None, :])
        else:
            masks[j] = ((key[:, None] // 64) <= (t[None, :] // 64))
    d = {"ident": np.eye(128, dtype=np.float32), "U": U, "BD": BD, "MBu": MBu, "MBl": MBl, "masks": masks}
    if odd:
        d["Uf"] = Uf
    else:
        inv = (10000.0 ** (-np.arange(0, 64, 2, dtype=np.float32) / np.float32(64))).astype(np.float32)
        ang = np.arange(S, dtype=np.float32)[None, :] * inv[:, None]
        cos, sin = np.cos(ang).astype(np.float32), np.sin(ang).astype(np.float32)
        p = np.arange(128)
        sign = np.where((p % 64) < 32, -1.0, 1.0).astype(np.float32)
        d["cosT"] = np.ascontiguousarray(cos[p % 32])
        d["sinT"] = np.ascontiguousarray(sin[p % 32] * sign[:, None])
    return d


def mix_inputs_even(hh, w_in, w_out, lq1, lk1, lq2, lk2, subln, conv_b, ig, fg, b_norm, gain):
    hs = [2 * hh, 2 * hh + 1]
    r = np.arange(128)
    swap = np.concatenate([r[32:64], r[0:32], r[96:128], r[64:96]])
    fcols = []
    for a in hs:
        fcols += [128 * a + r, 128 * a + swap, 512 + 128 * a + r, 512 + 128 * a + swap]
    for b in hs:
        fcols += [1536 + 128 * b + r, 2048 + 128 * b + r]
    tcols = [1024 + 128 * a + r for a in hs]
    for b in hs:
        tcols += [2560 + 128 * b + r, 3072 + 128 * b + r]
    gcols = [3584 + hs[0], 3584 + hs[1], 3588 + hs[0], 3588 + hs[1]]
    orow = np.concatenate([128 * hs[0] + r, 128 * hs[1] + r, 512 + 128 * hs[0] + r, 512 + 128 * hs[1] + r])
    cw = np.zeros((128, 16), np.float32)
    for i, b in enumerate(hs):
        cw[:, (2 * i) * 4:(2 * i) * 4 + 4] = conv_b[:, 128 * b + r].T
        cw[:, (2 * i + 1) * 4:(2 * i + 1) * 4 + 4] = conv_b[:, 512 + 128 * b + r].T
    return {"gain": chunk_cols(gain), "wf": np.ascontiguousarray(w_in[:, np.concatenate(fcols)]),
            "wt": np.ascontiguousarray(w_in[:, np.concatenate(tcols)]), "wgt": _kc(w_in[:, gcols]),
            "gbias": np.array([[ig[hs[0]], ig[hs[1]], fg[hs[0]], fg[hs[1]]]], np.float32),
            "wout": np.ascontiguousarray(w_out[orow]), "convw": cw, "nrm": np.ascontiguousarray(b_norm[None, :]),
            "lamv": np.concatenate([lq1, lk1, lq2, lk2])[None, :].astype(np.float32), "subln": np.ascontiguousarray(subln[:, None])}


def mix_inputs_odd(hh, w_in, w_out, conv_c, a_log, dt_bias, c_norm, fd_bias, gain):
    hs = [2 * hh, 2 * hh + 1]
    r = np.arange(128)
    fcols = []
    for c in hs:
        fcols += [128 * c + r, 512 + 128 * c + r, 1024 + 128 * c + r]
    for d in hs:
        fcols += [2056 + 128 * d + r, 2568 + 128 * d + r]
    tcols = [1536 + 128 * c + r for c in hs] + [3080 + 128 * d + r for d in hs]
    gcols = [2048 + hs[0], 2048 + hs[1], 2052 + hs[0], 2052 + hs[1], 3592 + hs[0], 3592 + hs[1]]
    orow = np.concatenate([512 + 128 * hs[0] + r, 512 + 128 * hs[1] + r, 128 * hs[0] + r, 128 * hs[1] + r])
    cw = np.zeros((128, 24), np.float32)
    for i, c in enumerate(hs):
        for j, off in enumerate((0, 512, 1024)):
            g = 3 * i + j
            cw[:, g * 4:g * 4 + 4] = conv_c[:, off + 128 * c + r].T
    return {"gain": chunk_cols(gain), "wf": np.ascontiguousarray(w_in[:, np.concatenate(fcols)]),
            "wt": np.ascontiguousarray(w_in[:, np.concatenate(tcols)]), "wgt": _kc(w_in[:, gcols]),
            "gbias": np.array([[dt_bias[hs[0]], dt_bias[hs[1]], 0.0, 0.0, fd_bias[hs[0]], fd_bias[hs[1]]]], np.float32),
            "wout": np.ascontiguousarray(w_out[orow]), "convw": cw, "nrm": np.ascontiguousarray(c_norm[None, :]),
            "alog": np.array([[a_log[hs[0]], a_log[hs[1]]]], np.float32)}


import math

_GROUPS = [[0, 1], [2, 3], [4, 5], [6, 7]]


def build_fused(S):
    T = S // 2
    NBH = T // 512
    k = KB()
    c8 = lambda ap, tok: ap[:, tok].rearrange("(c p) t -> p c t", p=128)
    xT = k.din("xT", [D, S])
    xh = k.din("xh", [D, T])
    outT = k.dout("outT", [D, T])
    part = [k.dint("part%d" % l, [2 * D, T]) for l in range(2)]
    psum = [k.dint("psumd%d" % l, [D, T]) for l in range(2)]
    h1 = k.dint("h1", [D, T])
    h1f = k.dint("h1f", [2 * D, T])
    part_r = [Res("part%d" % l) for l in range(2)]
    psum_r = [Res("psumd%d" % l) for l in range(2)]
    h1_r, h1f_r, out_r = Res("h1"), Res("h1f"), Res("out")

    def halfblk(ap):
        return lambda blk: ap[(blk // NBH) * D:(blk // NBH + 1) * D, (blk % NBH) * 512:(blk % NBH + 1) * 512].rearrange(
            "(c p) t -> p c t", p=128)

    k.pfx = "L0m_"
    build_mix(S, False, lam_init=0.8 - 0.6 * math.exp(-0.3 * 0), k=k,
              io={"h": lambda blk: c8(xT, slice(blk * 512, (blk + 1) * 512)), "h_res": [], "out": halfblk(part[0]), "out_res": part_r[0]})
    k.collective("ReduceScatter", ALU.add, part[0][:, :], psum[0][:, :], part_r[0], psum_r[0], _GROUPS)
    k.new_section("L0f_")
    build_ffn(T, False, SBT=min(1024, T), k=k,
              io={"h": lambda tok: c8(xh, tok), "h_res": [], "pins": [lambda tok: c8(psum[0], tok)], "pin_res": [psum_r[0]],
                  "out": lambda tok: c8(h1, tok), "out_res": h1_r})
    k.collective("AllGather", ALU.bypass, h1[:, :], h1f[:, :], h1_r, h1f_r, _GROUPS)
    k.new_section("L1m_")
    build_mix(S, True, k=k, io={"h": halfblk(h1f), "h_res": [h1f_r], "out": halfblk(part[1]), "out_res": part_r[1]})
    k.collective("ReduceScatter", ALU.add, part[1][:, :], psum[1][:, :], part_r[1], psum_r[1], _GROUPS)
    k.new_section("L1f_")
    build_ffn(T, True, SBT=min(1024, T), k=k,
              io={"h": lambda tok: c8(h1, tok), "h_res": [h1_r], "pins": [lambda tok: c8(psum[1], tok)], "pin_res": [psum_r[1]],
                  "out": lambda tok: c8(outT, tok), "out_res": out_r})
    nc = k.finish([out_r])
    return nc, k


_PROGS = {}


def build_fused4(S):
    k = KB()
    c8 = lambda ap, tok: ap[:, tok].rearrange("(c p) t -> p c t", p=128)
    blk8 = lambda ap: (lambda blk: c8(ap, slice(blk * 512, (blk + 1) * 512)))
    xT = k.din("xT", [D, S])
    outT = k.dout("outT", [D, S])
    pa = k.dint("partA", [D, S])
    pb = k.dint("partB", [D, S])
    h1 = k.dint("h1", [D, S])
    pa_r, pb_r, h1_r, out_r = Res("partA"), Res("partB"), Res("h1"), Res("out")
    first = True
    for layer in range(2):
        odd = layer % 2 == 1
        hsrc, hres = (xT, []) if layer == 0 else (h1, [h1_r])
        for tag, dst, dres in (("A", pa, pa_r), ("B", pb, pb_r)):
            if first:
                k.pfx = "L%dm%s_" % (layer, tag)
                first = False
            else:
                k.new_section("L%dm%s_" % (layer, tag))
            build_mix(S, odd, lam_init=0.8 - 0.6 * math.exp(-0.3 * layer), k=k,
                      io={"h": blk8(hsrc), "h_res": hres, "out": blk8(dst), "out_res": dres})
        k.new_section("L%df_" % layer)
        final = layer == 1
        odst, ores_ = (outT, out_r) if final else (h1, h1_r)
        build_ffn(S, final, SBT=min(1024, S), k=k,
                  io={"h": lambda tok, a=hsrc: c8(a, tok), "h_res": hres,
                      "pins": [lambda tok: c8(pa, tok), lambda tok: c8(pb, tok)], "pin_res": [pa_r, pb_r],
                      "out": lambda tok, a=odst: c8(a, tok), "out_res": ores_})
    nc = k.finish([out_r])
    return nc, k


def fused4_inputs(b, S, x, p, W):
    f = lambda a: np.asarray(a, np.float32)
    m = {"xT": np.ascontiguousarray(x[b].T)}
    ident, sel = ffn_consts()
    for layer in range(2):
        odd = layer % 2 == 1
        j = layer // 2
        cst = mix_consts(S, odd)
        for r, tag in ((0, "A"), (1, "B")):
            if odd:
                d = mix_inputs_odd(r, f(W["cd_w_in"][j]), f(W["cd_w_out"][j]), f(W["c_conv"][j]), f(W["c_a_log"][j]), f(W["c_dt_bias"][j]),
                                   f(W["c_norm"][j]), f(W["d_fgate_bias"][j]), f(W["norm_mix"][layer]))
            else:
                d = mix_inputs_even(r, f(W["ab_w_in"][j]), f(W["ab_w_out"][j]), f(W["a_lam_q1"][j]), f(W["a_lam_k1"][j]), f(W["a_lam_q2"][j]),
                                    f(W["a_lam_k2"][j]), f(W["a_subln"][j]), f(W["b_conv"][j]), f(W["b_igate_bias"][j]),
                                    f(W["b_fgate_bias"][j]), f(W["b_norm"][j]), f(W["norm_mix"][layer]))
            d.update(cst)
            for kk, vv in d.items():
                m["L%dm%s_%s" % (layer, tag, kk)] = vv
        Wr = np.concatenate([f(W["moe_w_group"][layer]), f(W["moe_w_router"][layer])], axis=1)
        fd = {"pT": np.ascontiguousarray(p[layer, b].T), "gain": chunk_cols(W["norm_ffn"][layer]), "gfin": chunk_cols(W["norm_final"]),
              "wr": np.ascontiguousarray(Wr.reshape(8, 128, 20).transpose(1, 0, 2).reshape(128, 160)),
              "br": np.concatenate([f(W["moe_b_group"][layer]), f(W["moe_b_router"][layer])])[None, :],
              "wg": tile_w(W["moe_w_gate"][layer]), "wu": tile_w(W["moe_w_up"][layer]), "wd": tile_w(W["moe_w_down"][layer]),
              "plg": f(W["ple_w_gate"][layer]), "plp": f(W["ple_w_proj"][layer]), "ident": ident, "sel": sel}
        for kk, vv in fd.items():
            m["L%df_%s" % (layer, kk)] = vv
    return m


def kernel(x, p, **W):
    x = np.asarray(x, np.float32)
    p = np.asarray(p, np.float32)
    B, S, _ = x.shape
    key = ("f4", S)
    if key not in _PROGS:
        _PROGS[key] = build_fused4(S)[0]
    nc = _PROGS[key]
    per_b = [fused4_inputs(b, S, x, p, W) for b in range(B)]
    maps = [per_b[c % B] for c in range(NCORES)]
    res = run_bass_kernel_spmd(nc, maps, core_ids=list(range(NCORES)))
    return np.stack([np.ascontiguousarray(res.results[b]["outT"].T) for b in range(B)]).astype(np.float32)


def fused_inputs(c, S, x, p, W):
    f = lambda a: np.asarray(a, np.float32)
    T = S // 2
    b, r = c // 2, c % 2
    ts = slice(r * T, (r + 1) * T)
    xTb = np.ascontiguousarray(x[b].T)
    m = {"xT": xTb, "xh": np.ascontiguousarray(xTb[:, ts])}
    ident, sel = ffn_consts()
    for layer in range(2):
        odd = layer % 2 == 1
        j = layer // 2
        if odd:
            d = mix_inputs_odd(r, f(W["cd_w_in"][j]), f(W["cd_w_out"][j]), f(W["c_conv"][j]), f(W["c_a_log"][j]), f(W["c_dt_bias"][j]),
                               f(W["c_norm"][j]), f(W["d_fgate_bias"][j]), f(W["norm_mix"][layer]))
        else:
            d = mix_inputs_even(r, f(W["ab_w_in"][j]), f(W["ab_w_out"][j]), f(W["a_lam_q1"][j]), f(W["a_lam_k1"][j]), f(W["a_lam_q2"][j]),
                                f(W["a_lam_k2"][j]), f(W["a_subln"][j]), f(W["b_conv"][j]), f(W["b_igate_bias"][j]),
                                f(W["b_fgate_bias"][j]), f(W["b_norm"][j]), f(W["norm_mix"][layer]))
        d.update(mix_consts(S, odd))
        for kk, vv in d.items():
            m["L%dm_%s" % (layer, kk)] = vv
        Wr = np.concatenate([f(W["moe_w_group"][layer]), f(W["moe_w_router"][layer])], axis=1)
        fd = {"pT": np.ascontiguousarray(p[layer, b, ts].T), "gain": chunk_cols(W["norm_ffn"][layer]), "gfin": chunk_cols(W["norm_final"]),
              "wr": np.ascontiguousarray(Wr.reshape(8, 128, 20).transpose(1, 0, 2).reshape(128, 160)),
              "br": np.concatenate([f(W["moe_b_group"][layer]), f(W["moe_b_router"][layer])])[None, :],
              "wg": tile_w(W["moe_w_gate"][layer]), "wu": tile_w(W["moe_w_up"][layer]), "wd": tile_w(W["moe_w_down"][layer]),
              "plg": f(W["ple_w_gate"][layer]), "plp": f(W["ple_w_proj"][layer]), "ident": ident, "sel": sel}
        for kk, vv in fd.items():
            m["L%df_%s" % (layer, kk)] = vv
    return m


def kernel_cc(x, p, **W):
    x = np.asarray(x, np.float32)
    p = np.asarray(p, np.float32)
    B, S, _ = x.shape
    T = S // 2
    if S not in _PROGS:
        _PROGS[S] = build_fused(S)[0]
    nc = _PROGS[S]
    maps = [fused_inputs(c, S, x, p, W) for c in range(NCORES)]
    res = run_bass_kernel_spmd(nc, maps, core_ids=list(range(NCORES)))
    out = np.empty((B, S, D), np.float32)
    for c in range(NCORES):
        b, r = c // 2, c % 2
        out[b, r * T:(r + 1) * T, :] = res.results[c]["outT"].T
    return out
```

```python
import numpy as np
from contextlib import ExitStack
import concourse.bass as bass
import concourse.mybir as mybir
from concourse.bass_utils import run_bass_kernel_spmd

F32 = mybir.dt.float32
BF16 = mybir.dt.bfloat16
AF = mybir.ActivationFunctionType
ALU = mybir.AluOpType
AX = mybir.AxisListType

D = 1024
NCORES = 8
RMS_EPS = 1e-6


class Res:
    __slots__ = ("name", "lw", "rd", "dsem", "dcnt")

    def __init__(self, name):
        self.name = name
        self.lw = None
        self.rd = {}
        self.dsem = None
        self.dcnt = 0


class Sched:
    ENGS = ("pe", "act", "dve", "pool", "sp")

    def __init__(self, nc, es):
        self.nc = nc
        self.es = es
        self.prog = {e: [] for e in self.ENGS}
        self.cnt = {e: 0 for e in self.ENGS}
        self.sems = {}
        for e in self.ENGS:
            self.sems[e] = es.enter_context(nc.semaphore("s_" + e))
        self.waited = {e: {} for e in self.ENGS}
        self.epoch = {e: 0 for e in self.ENGS}
        self.nsem = 0
        self.n_inst = 0
        self.n_wait = 0
        self.dcount = {}

    def new_sem(self, name):
        k = "d_" + name
        if k not in self.sems:
            self.nsem += 1
            self.sems[k] = self.es.enter_context(self.nc.semaphore(k))
            self.dcount[k] = 0
        return k

    EPOCH = 10 ** 9

    def _ekey(self, e):
        ep = self.epoch[e]
        return e if ep == 0 else "%s#%d" % (e, ep)

    def barrier(self):
        for e in self.ENGS:
            for f in self.ENGS:
                if f != e:
                    self._wait(e, self._ekey(f), self.cnt[f])
            for key, c in self.dcount.items():
                self._wait(e, key, c)
        for e in self.ENGS:
            if e != "pe":
                self._wait(e, self._ekey(e), self.cnt[e])

    def _wait(self, eng, key, val):
        if val <= 0:
            return
        if eng == "pe" and key.split("#")[0] == "pe":
            return
        w = self.waited[eng]
        if w.get(key, 0) >= val:
            return
        w[key] = val
        self.prog[eng].append(("w", key, val))
        self.n_wait += 1

    def _deps(self, eng, reads, writes):
        for r in reads:
            if r.lw is not None:
                self._wait(eng, r.lw[0], r.lw[1])
        for w in writes:
            if w.lw is not None:
                self._wait(eng, w.lw[0], w.lw[1])
            for k, v in w.rd.items():
                self._wait(eng, k, v)

    def op(self, eng, fn, reads=(), writes=()):
        self._deps(eng, reads, writes)
        if self.cnt[eng] >= self.EPOCH:
            self.epoch[eng] += 1
            self.cnt[eng] = 0
            nk = self._ekey(eng)
            self.sems[nk] = self.es.enter_context(self.nc.semaphore("s_" + nk.replace("#", "_")))
        key = self._ekey(eng)
        self.cnt[eng] += 1
        c = self.cnt[eng]
        self.prog[eng].append(("i", fn, key, 1))
        for r in reads:
            if r.rd.get(key, 0) < c:
                r.rd[key] = c
        for w in writes:
            w.lw = (key, c)
            w.rd = {}
        self.n_inst += 1

    def dma(self, q, fn, reads=(), writes=(), sem_res=None, inc=16):
        self._deps(q, reads, writes)
        sr = sem_res if sem_res is not None else (writes[0] if writes else reads[0])
        if sr.dsem is None:
            sr.dsem = self.new_sem(sr.name)
        key = sr.dsem
        self.dcount[key] += inc
        c = self.dcount[key]
        sr.dcnt = c
        self.prog[q].append(("i", fn, key, inc))
        for r in reads:
            if r.rd.get(key, 0) < c:
                r.rd[key] = c
        for w in writes:
            w.lw = (key, c)
            w.rd = {}
        self.n_inst += 1

    def wait_all(self, eng, ress):
        for r in ress:
            if r.lw is not None:
                self._wait(eng, r.lw[0], r.lw[1])
            for k, v in r.rd.items():
                self._wait(eng, k, v)

    def emit(self):
        nc = self.nc
        sems = self.sems
        prog = self.prog

        def run(engh, lst):
            for it in lst:
                if it[0] == "w":
                    engh.wait_ge(sems[it[1]], it[2])
                else:
                    it[1](engh).then_inc(sems[it[2]], it[3])

        with nc.Block() as block:
            @block.tensor
            def _(e):
                run(e, prog["pe"])

            @block.scalar
            def _(e):
                run(e, prog["act"])

            @block.vector
            def _(e):
                run(e, prog["dve"])

            @block.gpsimd
            def _(e):
                run(e, prog["pool"])

            @block.sync
            def _(e):
                run(e, prog["sp"])


class Tile:
    def __init__(self, t, name):
        self.t = t
        self.r = Res(name)

    def __getitem__(self, k):
        return self.t[k]


class KB:
    def __init__(self):
        self.nc = bass.Bass("TRN2", target_bir_lowering=False)
        self.es = ExitStack()
        self.S = Sched(self.nc, self.es)
        self.tes = ExitStack()
        self.pfx = ""
        self.in_names = []

    def new_section(self, pfx):
        self.S.barrier()
        self.tes.close()
        self.tes = ExitStack()
        self.pfx = pfx

    def din(self, name, shape, dt=F32):
        self.in_names.append(self.pfx + name)
        return self.nc.dram_tensor(self.pfx + name, list(shape), dt, kind="ExternalInput").ap()

    def dout(self, name, shape, dt=F32):
        return self.nc.dram_tensor(self.pfx + name, list(shape), dt, kind="ExternalOutput").ap()

    def dint(self, name, shape, dt=F32):
        return self.nc.dram_tensor(name, list(shape), dt).ap()

    def sb(self, name, shape, dt=F32):
        return Tile(self.tes.enter_context(self.nc.sbuf_tensor(self.pfx + name, list(shape), dt)), self.pfx + name)

    def ps(self, name, shape, dt=F32):
        return Tile(self.tes.enter_context(self.nc.psum_tensor(self.pfx + name, list(shape), dt)), self.pfx + name)

    def op(self, eng, fn, reads, writes):
        self.S.op(eng, fn, [x.r for x in reads], [x.r for x in writes])

    def load(self, out_ap, in_ap, wt, q="sp", reads=()):
        self.S.dma(q, lambda e: e.dma_start(out=out_ap, in_=in_ap), [x if isinstance(x, Res) else x.r for x in reads], [wt.r])

    def store(self, out_ap, in_ap, rt, ores, q="sp"):
        for kk, vv in ores.rd.items():
            self.S._wait(q, kk, vv)
        self.S.dma(q, lambda e: e.dma_start(out=out_ap, in_=in_ap), [rt.r], [], sem_res=ores)
        ores.lw = (ores.dsem, self.S.dcount[ores.dsem])

    def collective(self, kind, op, in_ap, out_ap, in_res, out_res, groups):
        import os
        if os.environ.get("MK_NOCC"):
            return
        self.S.dma("pool", lambda e: e.collective_compute(kind, op, replica_groups=groups, ins=[in_ap], outs=[out_ap]),
                   [in_res], [out_res], inc=1)

    def mm(self, out_ap, lhsT, rhs, start, stop, reads, wt):
        self.op("pe", lambda e: e.matmul(out_ap, lhsT, rhs, start=start, stop=stop), reads, [wt])

    def tr(self, out_ap, in_ap, ident_ap, reads, wt):
        self.op("pe", lambda e: e.transpose(out_ap, in_ap, ident_ap), reads, [wt])

    def finish(self, out_res_list):
        self.S.wait_all("sp", out_res_list)
        self.S.emit()
        self.tes.close()
        self.es.close()
        return self.nc


def build_ffn(T, final, SBT=1024, k=None, io=None):
    NB = T // 512
    NSB = max(1, T // SBT)
    BPS = NB // NSB
    standalone = k is None
    c8 = lambda ap, tok: ap[:, tok].rearrange("(c p) t -> p c t", p=128)
    if standalone:
        k = KB()
        hT = k.din("hT", [D, T])
        p0T = k.din("p0T", [D, T])
        p1T = k.din("p1T", [D, T])
        io = {"h": lambda tok: c8(hT, tok), "h_res": [], "pins": [lambda tok: c8(p0T, tok), lambda tok: c8(p1T, tok)], "pin_res": []}
    pT = k.din("pT", [256, T])
    gain_d = k.din("gain", [128, 8])
    gfin_d = k.din("gfin", [128, 8])
    wr_d = k.din("wr", [128, 8 * 20])
    br_d = k.din("br", [1, 20])
    wg_d = k.din("wg", [16, 128, 4096])
    wu_d = k.din("wu", [16, 128, 4096])
    wd_d = k.din("wd", [16, 128, 4096])
    plg_d = k.din("plg", [D, D])
    plp_d = k.din("plp", [256, D])
    ident_d = k.din("ident", [128, 128])
    sel_d = k.din("sel", [16, 16 * 128])
    if standalone:
        outT = k.dout("outT", [D, T])
        ores = Res("out")
        io["out"] = lambda tok: c8(outT, tok)
        io["out_res"] = ores
    ores = io["out_res"]

    acc = k.sb("acc", [128, BPS, 8, 512])
    hn16 = k.sb("hn16", [128, BPS, 8, 512], BF16)
    big32 = k.sb("big32", [128, 8, 512])
    combT = k.sb("combT", [16, BPS * 512])
    wbuf = [k.sb("wbuf%d" % i, [128, 12288], BF16) for i in range(2)]
    stage = [k.sb("stage%d" % i, [128, 2048]) for i in range(3)]
    p16 = k.sb("p16", [128, 2, 512], BF16)
    h2b = k.sb("h2b", [128, 8, 512], BF16)
    sg = [k.sb("sg%d" % i, [128, 512]) for i in range(2)]
    tt = [k.sb("tt%d" % i, [128, 512]) for i in range(2)]
    he = [k.sb("he%d" % i, [128, 4, 512], BF16) for i in range(2)]
    sq = [k.sb("sq%d" % i, [128, 512], BF16) for i in range(2)]
    rstd = k.sb("rstd", [128, 512])
    gain = k.sb("gain_s", [128, 8])
    gfin = k.sb("gfin_s", [128, 8])
    wr = k.sb("wr_s", [128, 8 * 20])
    br = k.sb("br_s", [128, 20])
    ident = k.sb("ident_s", [128, 128])
    sel = k.sb("sel_s", [16, 16 * 128])
    ones16 = k.sb("ones16", [128, 128], BF16)
    rt = {n: k.sb("rt_" + n, [128, w]) for n, w in
          [("L", 20), ("gmax", 1), ("ngmax", 1), ("gm", 4), ("ex", 4), ("se", 1), ("ptop", 1), ("t44", 16), ("esel", 4),
           ("m1", 1), ("k1", 4), ("e2", 4), ("m2", 1), ("k2", 4), ("d", 1), ("ed", 1), ("w1", 1), ("w2", 1), ("t1", 4),
           ("cl", 4), ("comb", 16)]}
    ps_gu = [k.ps("ps_gu%d" % i, [128, 512]) for i in range(4)]
    ps_y = [k.ps("ps_y%d" % i, [128, 512]) for i in range(2)]
    ps_c = k.ps("ps_c", [128, 512])
    ps_m = k.ps("ps_m", [128, 512])

    k.load(gain[:], gain_d[:, :], gain)
    k.load(gfin[:], gfin_d[:, :], gfin)
    k.load(wr[:], wr_d[:, :], wr)
    k.load(br[:], br_d.partition_broadcast(128), br)
    k.load(ident[:], ident_d[:, :], ident)
    k.load(sel[:], sel_d[:, :], sel)
    k.op("dve", lambda e: e.memset(ones16[:], 1.0), [], [ones16])
    epsb = k.sb("epsb", [128, 1])
    k.op("dve", lambda e: e.memset(epsb[:], float(D * RMS_EPS)), [], [epsb])
    k.op("dve", lambda e: e.tensor_scalar_mul(out=gain[:], in0=gain[:], scalar1=32.0), [gain], [gain])
    k.op("dve", lambda e: e.tensor_scalar_mul(out=gfin[:], in0=gfin[:], scalar1=32.0), [gfin], [gfin])

    stage_i = [0]

    def load_cast(dst_ap, src_ap, dst_tile, shape3=None):
        st = stage[stage_i[0] % 3]
        stage_i[0] += 1
        if shape3 is None:
            k.load(st[:], src_ap, st)
            k.op("act", lambda e: e.copy(out=dst_ap, in_=st[:]), [st], [dst_tile])
        else:
            a, b = shape3
            k.load(st[:].rearrange("p (a b) -> p a b", a=a), src_ap, st)
            k.op("act", lambda e: e.copy(out=dst_ap, in_=st[:]), [st], [dst_tile])

    def rmsnorm_stats(src_chunks, src_tile):
        for c in range(8):
            s = sq[c % 2]
            k.op("act", (lambda s, c: lambda e: e.activation(out=s[:], in_=src_chunks(c), func=AF.Square))(s, c),
                 [src_tile], [s])
            k.mm(ps_m[:], ones16[:], s[:], c == 0, c == 7, [ones16, s], ps_m)
        k.op("act", lambda e: e.activation(out=rstd[:], in_=ps_m[:], func=AF.Sqrt, bias=epsb[:, 0:1], scale=1.0),
             [ps_m, epsb], [rstd])
        k.op("dve", lambda e: e.reciprocal(out=rstd[:], in_=rstd[:]), [rstd], [rstd])

    for sb_i in range(NSB):
        for j in range(BPS):
            tok = slice((sb_i * BPS + j) * 512, (sb_i * BPS + j + 1) * 512)
            k.load(acc[:, j], io["h"](tok), acc, reads=io["h_res"])
            for src in io["pins"]:
                k.load(big32[:], src(tok), big32, reads=io["pin_res"])
                k.op("dve", (lambda j: lambda e: e.tensor_tensor(out=acc[:, j], in0=acc[:, j], in1=big32[:], op=ALU.add))(j),
                     [acc, big32], [acc])
            rmsnorm_stats(lambda c, j=j: acc[:, j, c], acc)
            for c in range(8):
                k.op("dve", (lambda j, c: lambda e: e.scalar_tensor_tensor(
                    out=big32[:, c], in0=acc[:, j, c], scalar=gain[:, c:c + 1], in1=rstd[:], op0=ALU.mult, op1=ALU.mult))(j, c),
                    [acc, gain, rstd], [big32])
            k.op("act", (lambda j: lambda e: e.copy(out=hn16[:, j], in_=big32[:]))(j), [big32], [hn16])
            for t4 in range(4):
                for c in range(8):
                    k.mm(ps_m[:, 0:20], big32[:, c, t4 * 128:(t4 + 1) * 128], wr[:, c * 20:(c + 1) * 20], c == 0, c == 7,
                         [big32, wr], ps_m)
                R = rt
                V = "dve"
                k.op(V, lambda e: e.tensor_tensor(out=R["L"][:], in0=ps_m[:, 0:20], in1=br[:], op=ALU.add), [ps_m, br], [R["L"]])
                k.op(V, lambda e: e.reduce_max(out=R["gmax"][:], in_=R["L"][:, 0:4], axis=AX.X), [R["L"]], [R["gmax"]])
                k.op(V, lambda e: e.tensor_tensor(out=R["gm"][:], in0=R["L"][:, 0:4], in1=R["gmax"][:, 0:1].to_broadcast([128, 4]),
                                                  op=ALU.is_ge), [R["L"], R["gmax"]], [R["gm"]])
                k.op(V, lambda e: e.tensor_scalar_mul(out=R["ngmax"][:], in0=R["gmax"][:], scalar1=-1.0), [R["gmax"]], [R["ngmax"]])
                k.op("act", lambda e: e.activation(out=R["ex"][:], in_=R["L"][:, 0:4], func=AF.Exp, bias=R["ngmax"][:, 0:1],
                                                   scale=1.0, accum_out=R["se"][:]), [R["L"], R["ngmax"]], [R["ex"], R["se"]])
                k.op(V, lambda e: e.reciprocal(out=R["ptop"][:], in_=R["se"][:]), [R["se"]], [R["ptop"]])
                k.op(V, lambda e: e.tensor_tensor(
                    out=R["t44"][:].rearrange("p (g x) -> p g x", g=4),
                    in0=R["L"][:, 4:20].rearrange("p (g x) -> p g x", g=4),
                    in1=R["gm"][:].unsqueeze(2).to_broadcast([128, 4, 4]), op=ALU.mult), [R["L"], R["gm"]], [R["t44"]])
                k.op(V, lambda e: e.tensor_reduce(out=R["esel"][:], in_=R["t44"][:].rearrange("p (g x) -> p x g", g=4),
                                                  axis=AX.X, op=ALU.add), [R["t44"]], [R["esel"]])
                k.op(V, lambda e: e.reduce_max(out=R["m1"][:], in_=R["esel"][:], axis=AX.X), [R["esel"]], [R["m1"]])
                k.op(V, lambda e: e.tensor_tensor(out=R["k1"][:], in0=R["esel"][:], in1=R["m1"][:, 0:1].to_broadcast([128, 4]),
                                                  op=ALU.is_ge), [R["esel"], R["m1"]], [R["k1"]])
                k.op(V, lambda e: e.scalar_tensor_tensor(out=R["e2"][:], in0=R["k1"][:], scalar=-1e30, in1=R["esel"][:],
                                                         op0=ALU.mult, op1=ALU.add), [R["k1"], R["esel"]], [R["e2"]])
                k.op(V, lambda e: e.reduce_max(out=R["m2"][:], in_=R["e2"][:], axis=AX.X), [R["e2"]], [R["m2"]])
                k.op(V, lambda e: e.tensor_tensor(out=R["k2"][:], in0=R["e2"][:], in1=R["m2"][:, 0:1].to_broadcast([128, 4]),
                                                  op=ALU.is_ge), [R["e2"], R["m2"]], [R["k2"]])
                k.op(V, lambda e: e.tensor_tensor(out=R["d"][:], in0=R["m2"][:], in1=R["m1"][:], op=ALU.subtract),
                     [R["m1"], R["m2"]], [R["d"]])
                k.op("act", lambda e: e.activation(out=R["ed"][:], in_=R["d"][:], func=AF.Exp), [R["d"]], [R["ed"]])
                k.op(V, lambda e: e.tensor_scalar_add(out=R["w1"][:], in0=R["ed"][:], scalar1=1.0), [R["ed"]], [R["w1"]])
                k.op(V, lambda e: e.reciprocal(out=R["w1"][:], in_=R["w1"][:]), [R["w1"]], [R["w1"]])
                k.op(V, lambda e: e.tensor_tensor(out=R["w1"][:], in0=R["w1"][:], in1=R["ptop"][:], op=ALU.mult),
                     [R["w1"], R["ptop"]], [R["w1"]])
                k.op(V, lambda e: e.tensor_tensor(out=R["w2"][:], in0=R["w1"][:], in1=R["ed"][:], op=ALU.mult),
                     [R["w1"], R["ed"]], [R["w2"]])
                k.op(V, lambda e: e.tensor_scalar(out=R["t1"][:], in0=R["k1"][:], scalar1=R["w1"][:, 0:1], scalar2=None,
                                                  op0=ALU.mult), [R["k1"], R["w1"]], [R["t1"]])
                k.op(V, lambda e: e.scalar_tensor_tensor(out=R["cl"][:], in0=R["k2"][:], scalar=R["w2"][:, 0:1], in1=R["t1"][:],
                                                         op0=ALU.mult, op1=ALU.add), [R["k2"], R["w2"], R["t1"]], [R["cl"]])
                k.op(V, lambda e: e.tensor_tensor(
                    out=R["comb"][:].rearrange("p (g x) -> p g x", g=4),
                    in0=R["gm"][:].unsqueeze(2).to_broadcast([128, 4, 4]),
                    in1=R["cl"][:].unsqueeze(1).to_broadcast([128, 4, 4]), op=ALU.mult), [R["gm"], R["cl"]], [R["comb"]])
                k.tr(ps_m[0:16, 128:256], R["comb"][:], ident[:], [R["comb"], ident], ps_m)
                k.op(V, (lambda j, t4: lambda e: e.tensor_copy(out=combT[:, j * 512 + t4 * 128: j * 512 + (t4 + 1) * 128],
                                                               in_=ps_m[0:16, 128:256]))(j, t4), [ps_m], [combT])

        def load_expert(e):
            wb = wbuf[e % 2]
            for mi, src in enumerate((wg_d, wu_d, wd_d)):
                for half in range(2):
                    load_cast(wb[:, mi * 4096 + half * 2048: mi * 4096 + (half + 1) * 2048], src[e, :, half * 2048:(half + 1) * 2048], wb)

        load_expert(0)
        gi = 0
        yi = 0
        for e in range(16):
            if e + 1 < 16:
                load_expert(e + 1)
            wb = wbuf[e % 2]
            for j in range(BPS):
                k.mm(ps_c[:], sel[:, e * 128:(e + 1) * 128], combT[:, j * 512:(j + 1) * 512], True, True, [sel, combT], ps_c)
                hb = he[(e * BPS + j) % 2]
                for f in range(4):
                    pg = ps_gu[gi % 4]
                    pu = ps_gu[(gi + 1) % 4]
                    gi += 2
                    for c in range(8):
                        k.mm(pg[:], wb[:, c * 512 + f * 128: c * 512 + (f + 1) * 128], hn16[:, j, c], c == 0, c == 7, [wb, hn16], pg)
                    for c in range(8):
                        k.mm(pu[:], wb[:, 4096 + c * 512 + f * 128: 4096 + c * 512 + (f + 1) * 128], hn16[:, j, c], c == 0, c == 7,
                             [wb, hn16], pu)
                    s_ = sg[f % 2]
                    t_ = tt[f % 2]
                    k.op("act", (lambda s_, pg: lambda e: e.activation(out=s_[:], in_=pg[:], func=AF.Silu))(s_, pg), [pg], [s_])
                    k.op("dve", (lambda t_, s_, pu: lambda e: e.tensor_tensor(out=t_[:], in0=s_[:], in1=pu[:], op=ALU.mult))(t_, s_, pu),
                         [s_, pu], [t_])
                    k.op("dve", (lambda hb, f, t_: lambda e: e.tensor_tensor(out=hb[:, f], in0=t_[:], in1=ps_c[:], op=ALU.mult))(hb, f, t_),
                         [t_, ps_c], [hb])
                for c in range(8):
                    py = ps_y[yi % 2]
                    yi += 1
                    for f in range(4):
                        k.mm(py[:], wb[:, 8192 + f * 1024 + c * 128: 8192 + f * 1024 + (c + 1) * 128], hb[:, f], f == 0, f == 3,
                             [wb, hb], py)
                    k.op("dve", (lambda j, c, py: lambda e: e.tensor_tensor(out=acc[:, j, c], in0=acc[:, j, c], in1=py[:], op=ALU.add))(j, c, py),
                         [acc, py], [acc])

        wp = wbuf[0]
        for q4 in range(4):
            load_cast(wp[:, q4 * 2048:(q4 + 1) * 2048],
                      plg_d[q4 * 256:(q4 + 1) * 256, :].rearrange("(k p) n -> p k n", p=128), wp, (2, 1024))
        load_cast(wp[:, 8192:10240], plp_d[:, :].rearrange("(k p) n -> p k n", p=128), wp, (2, 1024))
        for j in range(BPS):
            tok = slice((sb_i * BPS + j) * 512, (sb_i * BPS + j + 1) * 512)
            st = stage[stage_i[0] % 3]
            stage_i[0] += 1
            k.load(st[:, 0:1024].rearrange("p (a b) -> p a b", a=2), pT[:, tok].rearrange("(c p) t -> p c t", p=128), st)
            k.op("act", (lambda st: lambda e: e.copy(out=p16[:], in_=st[:, 0:1024]))(st), [st], [p16])
            k.op("act", (lambda j: lambda e: e.copy(out=h2b[:], in_=acc[:, j]))(j), [acc], [h2b])
            for c in range(8):
                pg = ps_gu[gi % 4]
                pu = ps_gu[(gi + 1) % 4]
                gi += 2
                for kk in range(8):
                    k.mm(pg[:], wp[:, kk * 1024 + c * 128: kk * 1024 + (c + 1) * 128], h2b[:, kk], kk == 0, kk == 7, [wp, h2b], pg)
                for kk in range(2):
                    k.mm(pu[:], wp[:, 8192 + kk * 1024 + c * 128: 8192 + kk * 1024 + (c + 1) * 128], p16[:, kk], kk == 0, kk == 1,
                         [wp, p16], pu)
                s_ = sg[c % 2]
                t_ = tt[c % 2]
                k.op("act", (lambda s_, pg: lambda e: e.activation(out=s_[:], in_=pg[:], func=AF.Sigmoid))(s_, pg), [pg], [s_])
                k.op("dve", (lambda t_, s_, pu: lambda e: e.tensor_tensor(out=t_[:], in0=s_[:], in1=pu[:], op=ALU.mult))(t_, s_, pu),
                     [s_, pu], [t_])
                k.op("dve", (lambda j, c, t_: lambda e: e.tensor_tensor(out=acc[:, j, c], in0=acc[:, j, c], in1=t_[:], op=ALU.add))(j, c, t_),
                     [acc, t_], [acc])
            if final:
                rmsnorm_stats(lambda c, j=j: acc[:, j, c], acc)
                for c in range(8):
                    k.op("dve", (lambda j, c: lambda e: e.scalar_tensor_tensor(
                        out=big32[:, c], in0=acc[:, j, c], scalar=gfin[:, c:c + 1], in1=rstd[:], op0=ALU.mult, op1=ALU.mult))(j, c),
                        [acc, gfin, rstd], [big32])
                k.store(io["out"](tok), big32[:], big32, ores)
            else:
                k.store(io["out"](tok), acc[:, j], acc, ores)
    if not standalone:
        return None, k
    nc = k.finish([ores])
    return nc, k


def ffn_consts():
    ident = np.eye(128, dtype=np.float32)
    sel = np.zeros((16, 16 * 128), np.float32)
    for e in range(16):
        sel[e, e * 128:(e + 1) * 128] = 1.0
    return ident, sel


def tile_w(w):
    w = np.asarray(w, np.float32)
    E, K_, N_ = w.shape
    return np.ascontiguousarray(w.reshape(E, K_ // 128, 128, N_).transpose(0, 2, 1, 3).reshape(E, 128, (K_ // 128) * N_))


def chunk_cols(v):
    return np.ascontiguousarray(np.asarray(v, np.float32).reshape(8, 128).T)


class Sub:
    def __init__(self, ap, name, res=None):
        self.ap = ap
        self.r = res if res is not None else Res(name)

    def __getitem__(self, k):
        return self.ap[k]


def _v(k, eng, meth, reads, writes, **kw):
    k.S.op(eng, lambda e: getattr(e, meth)(**kw), [x.r for x in reads], [x.r for x in writes])


def build_mix(S, odd, lam_init=0.2, skip_attn=False, skip_rec=False, stop=99, k=None, io=None):
    NB = S // 512
    NKB = S // 128
    standalone = k is None
    if standalone:
        k = KB()
    V = lambda *a, **kw: _v(k, *a, **kw)
    NFG = 10 if odd else 12
    NTG = 4 if odd else 6
    NG = 6 if odd else 4
    NCV = 6 if odd else 4
    if standalone:
        hT = k.din("hT", [D, S])
        io = {"h": lambda blk: hT[:, blk * 512:(blk + 1) * 512].rearrange("(c p) t -> p c t", p=128), "h_res": []}
    gain_d = k.din("gain", [128, 8])
    wf_d = k.din("wf", [D, NFG * 128])
    wt_d = k.din("wt", [D, NTG * 128])
    wgt_d = k.din("wgt", [128, 8 * NG])
    gb_d = k.din("gbias", [1, NG])
    wout_d = k.din("wout", [512, D])
    convw_d = k.din("convw", [128, NCV * 4])
    nrm_d = k.din("nrm", [1, 128])
    ident_d = k.din("ident", [128, 128])
    U_d = k.din("U", [128, 128])
    BD_d = k.din("BD", [128, 128])
    MBu_d = k.din("MBu", [128, 128])
    MBl_d = k.din("MBl", [128, 128])
    mask_d = k.din("masks", [4, 128, 512])
    if odd:
        alog_d = k.din("alog", [1, 2])
        Uf_d = k.din("Uf", [128, 128])
    else:
        lamv_d = k.din("lamv", [1, 256])
        subln_d = k.din("subln", [128, 1])
        cos_d = k.din("cosT", [128, S])
        sin_d = k.din("sinT", [128, S])
    if standalone:
        partT = k.dout("partT", [D, S])
        io["out"] = lambda blk: partT[:, blk * 512:(blk + 1) * 512].rearrange("(c p) t -> p c t", p=128)
        io["out_res"] = Res("out")
    ores = io["out_res"]

    x32 = k.sb("x32", [128, 8, 512])
    hn16 = k.sb("hn16", [128, 8, 512], BF16)
    wf16 = k.sb("wf16", [128, 8, NFG * 128], BF16)
    wt16 = k.sb("wt16", [128, 8, NTG * 128], BF16)
    wout16 = k.sb("wout16", [128, 4, D], BF16)
    wg32 = k.sb("wg32", [128, 8 * NG])
    gbias = k.sb("gbias_s", [128, NG])
    gain = k.sb("gain_s", [128, 8])
    convw = k.sb("convw_s", [128, NCV * 4])
    nrmrep = k.sb("nrmrep", [128, 128])
    ident = k.sb("ident_s", [128, 128])
    Um = k.sb("U_s", [128, 128])
    BDm = k.sb("BD_s", [128, 128])
    MBu = k.sb("MBu_s", [128, 128])
    MBl = k.sb("MBl_s", [128, 128])
    masks = k.sb("masks_s", [128, 4, 512], BF16)
    ones16 = k.sb("ones16", [128, 128], BF16)
    ones32 = k.sb("ones32", [128, 128])
    epsb = k.sb("epsb", [128, 1])
    eps1 = k.sb("eps1", [128, 1])
    onec = k.sb("onec", [128, 1])
    sq = [k.sb("sq%d" % i, [128, 512], BF16) for i in range(2)]
    rstd = k.sb("rstd", [128, 512])
    Xt = rstd
    kcache = [k.sb("kc%d" % i, [128, S], BF16) for i in range(2)]
    vcache = [k.sb("vc%d" % i, [128, NKB, 128], BF16) for i in range(2)]
    qa = [k.sb("qa%d" % i, [128, 512], BF16) for i in range(2)]
    oT = k.sb("oT", [128, 4, 512], BF16)
    NET = 2 if odd else 3
    Et = [k.sb("E%d" % i, [128, 512], BF16) for i in range(NET)]
    Rt = [k.sb("R%d" % i, [128, 512]) for i in range(1 if odd else 2)]
    rz = k.sb("rz", [128, 512])
    cvin = [k.sb("cvin%d" % i, [128, 515]) for i in range(NCV)]
    cvo = [k.sb("cvo%d" % i, [128, 512]) for i in range(NCV)]
    vaug = [k.sb("vaug%d" % i, [128, 4, 130] if not odd else [128, 2]) for i in range(2)]
    gtok = [k.sb("gtok%d" % i, [128, 4, 128]) for i in range(2)]
    gts = k.sb("gts", [128, 4, NG])
    lf = k.sb("lf", [128, 4, NG])
    gt2 = k.sb("gt2", [128, 4, NG])
    Sst = [[k.sb("S%d_%d" % (i, j), [128, 130]) for j in range(2)] for i in range(2)]
    qhat = [k.sb("qhat%d" % i, [128, 2, 128]) for i in range(2)]
    smh = [{n: k.sb("sm%d_" % hh_ + n, [128, w]) for n, w in
          [("lfb", 128), ("crep", 128), ("bc", 8), ("rcol", 1), ("e1", 1), ("Dm", 128), ("G", 128), ("ecr", 128),
           ("k2", 128), ("dm", 1), ("hh", 128), ("junk", 128), ("ssq", 1), ("rs", 1)] +
          ([("Dl", 128), ("Gl", 128), ("N", 128), ("M", 128), ("P", 128), ("Mk", 128), ("Y", 128), ("vb", 128), ("kp", 128),
            ("u", 128), ("wT0", 128), ("wT1", 128), ("vn", 128), ("ktok", 128), ("bcol", 1), ("bebc", 1), ("ebd", 1), ("kd", 128),
            ("tmpc", 1)] if odd else [])} for hh_ in range(2)]
    for d_ in smh:
        d_["hn"] = d_["junk"]
        d_["ob"] = d_["hh"]
        d_["sgo"] = d_["G"]
        d_["AT"] = d_["Dm"]
    sm = smh[0]
    if odd:
        alog = k.sb("alog_s", [128, 2])
        Ufm = k.sb("Uf_s", [128, 128])
        carry = k.sb("carry", [128, 2])
        ncum = [k.sb("ncum%d" % i, [128, NKB]) for i in range(2)]
        Rq2 = k.sb("Rq2", [33, 512])
        Rq = [Sub(Rq2.t[32 * i_:32 * i_ + 1, :], "Rq%d" % i_, Rq2.r) for i_ in range(2)]
    else:
        lamv = k.sb("lamv_s", [128, 256])
        lamt = k.sb("lamt", [128, 8])
        subc = k.sb("subc", [128, 1])
        cost = k.sb("cost", [128, 512])
        sint = k.sb("sint", [128, 512])
        rt1 = Rt[0]
        rt2 = Rt[1]
    pj = [k.ps("pj%d" % i, [128, 512]) for i in range(2)]
    pst = [k.ps("pst%d" % i, [128, 512]) for i in range(2)]
    po = k.ps("po", [128, 512])
    pz = k.ps("pz", [128, 512])
    pr0 = k.ps("pr0", [128, 512])
    pr1 = k.ps("pr1", [128, 512])
    prb = [pr0, pr1]
    slots = [dict(A=Sub(prb[i_].t[:, 0:128], "pA%d" % i_, prb[i_].r), F=Sub(prb[i_].t[:, 128:258], "pF%d" % i_, prb[i_].r),
                  D=Sub(prb[i_].t[:, 384:512], "pD%d" % i_, prb[i_].r), E=Sub(pj[i_].t[:, 0:130], "pE%d" % i_, pj[i_].r),
                  C=Sub(pj[i_].t[:, 256:384], "pC%d" % i_, pj[i_].r), G=Sub(pj[i_].t[:, 384:512], "pG%d" % i_, pj[i_].r))
             for i_ in range(2)]
    pA = slots[0]["A"]
    pH = Sub(pst[0].t[:, 0:128], "pH", pst[0].r)

    for t_, d_ in ((gain, gain_d), (wg32, wgt_d), (convw, convw_d), (ident, ident_d), (Um, U_d), (BDm, BD_d), (MBu, MBu_d), (MBl, MBl_d)):
        k.load(t_[:], d_[:, :], t_)
    k.load(gbias[:], gb_d.partition_broadcast(128), gbias)
    k.load(nrmrep[:], nrm_d.partition_broadcast(128), nrmrep)
    V("dve", "memset", [], [ones16], ap=ones16[:], constant=1.0)
    V("dve", "memset", [], [ones32], ap=ones32[:], constant=1.0)
    V("dve", "memset", [], [epsb], ap=epsb[:], constant=float(D * RMS_EPS))
    V("dve", "memset", [], [eps1], ap=eps1[:], constant=float(RMS_EPS))
    V("dve", "memset", [], [onec], ap=onec[:], constant=1.0)
    V("dve", "tensor_scalar_mul", [gain], [gain], out=gain[:], in0=gain[:], scalar1=32.0)
    for i in range(2):
        V("dve", "memset", [], [qhat[i]], ap=qhat[i][:], constant=0.0)
        V("dve", "memset", [], [vaug[i]], ap=vaug[i][:], constant=1.0)
        for j in range(2):
            V("dve", "memset", [], [Sst[i][j]], ap=Sst[i][j][:], constant=0.0)
    for c_ in cvin:
        V("dve", "memset", [], [c_], ap=c_[:], constant=0.0)
    V("dve", "memset", [], [lf], ap=lf[:], constant=0.0)
    if odd:
        k.load(alog[:], alog_d.partition_broadcast(128), alog)
        k.load(Ufm[:], Uf_d[:, :], Ufm)
        V("act", "activation", [alog], [alog], out=alog[:], in_=alog[:], func=AF.Exp)
        V("dve", "tensor_scalar_mul", [alog], [alog], out=alog[:], in0=alog[:], scalar1=-1.0)
        V("dve", "memset", [], [carry], ap=carry[:], constant=0.0)
    else:
        k.load(lamv[:], lamv_d.partition_broadcast(128), lamv)
        k.load(subc[:], subln_d[:, :], subc)
        V("dve", "tensor_scalar_mul", [subc], [subc], out=subc[:], in0=subc[:], scalar1=float(1.0 - lam_init))
        V("dve", "tensor_tensor", [lamv], [lamv], out=lamv[:, 0:64], in0=lamv[:, 0:64], in1=lamv[:, 64:128], op=ALU.mult)
        V("dve", "tensor_tensor", [lamv], [lamv], out=lamv[:, 128:192], in0=lamv[:, 128:192], in1=lamv[:, 192:256], op=ALU.mult)
        V("dve", "reduce_sum", [lamv], [lamt], out=lamt[:, 0:1], in_=lamv[:, 0:64], axis=AX.X)
        V("dve", "reduce_sum", [lamv], [lamt], out=lamt[:, 1:2], in_=lamv[:, 128:192], axis=AX.X)
        V("act", "activation", [lamt], [lamt], out=lamt[:, 2:4], in_=lamt[:, 0:2], func=AF.Exp)
        V("dve", "tensor_tensor", [lamt], [lamt], out=lamt[:, 4:5], in0=lamt[:, 3:4], in1=lamt[:, 2:3], op=ALU.subtract)
        V("dve", "tensor_scalar_add", [lamt], [lamt], out=lamt[:, 4:5], in0=lamt[:, 4:5], scalar1=float(-lam_init))
    k.load(x32[:, 0:4, :], mask_d.rearrange("j p t -> p j t"), x32)
    if odd:
        V("dve", "tensor_scalar", [x32], [masks], out=masks[:], in0=x32[:, 0:4, :], scalar1=-1.0, scalar2=30000.0, op0=ALU.add, op1=ALU.mult)
    else:
        V("act", "copy", [x32], [masks], out=masks[:], in_=x32[:, 0:4, :])

    def load_w(dst, dcols, src, rows0, nrows, ncols, col0):
        nk = nrows // 128
        st = x32[:].rearrange("p a b -> p (a b)")[:, 0:nk * ncols].rearrange("p (a b) -> p a b", a=nk)
        k.load(st, src[rows0:rows0 + nrows, col0:col0 + ncols].rearrange("(a p) n -> p a n", p=128), x32)
        V("act", "copy", [x32], [dst], out=dcols, in_=st)

    for g in range(NFG):
        for h2 in range(2):
            load_w(wf16, wf16[:, h2 * 4:(h2 + 1) * 4, g * 128:(g + 1) * 128], wf_d, h2 * 512, 512, 128, g * 128)
    for g in range(NTG):
        for h2 in range(2):
            load_w(wt16, wt16[:, h2 * 4:(h2 + 1) * 4, g * 128:(g + 1) * 128], wt_d, h2 * 512, 512, 128, g * 128)
    for hh_ in range(4):
        load_w(wout16, wout16[:, hh_:hh_ + 1, :], wout_d, hh_ * 128, 128, D, 0)

    pji = [0]

    def proj_fm(g):
        p = pj[pji[0] % 2]
        pji[0] += 1
        for c in range(8):
            k.mm(p[:], wf16[:, c, g * 128:(g + 1) * 128], hn16[:, c], c == 0, c == 7, [wf16, hn16], p)
        return p

    def proj_tm(g):
        p = pj[pji[0] % 2]
        pji[0] += 1
        for t4 in range(4):
            for c in range(8):
                k.mm(p[:, t4 * 128:(t4 + 1) * 128], hn16[:, c, t4 * 128:(t4 + 1) * 128], wt16[:, c, g * 128:(g + 1) * 128],
                     c == 0, c == 7, [wt16, hn16], p)
        return p

    def conv_silu(p, ci, blk):
        xi = cvin[ci]
        V("act", "copy", [p], [xi], out=xi[:, 3:515], in_=p[:])
        o = cvo[ci]
        V("dve", "tensor_scalar_mul", [xi, convw], [o], out=o[:], in0=xi[:, 0:512], scalar1=convw[:, ci * 4:ci * 4 + 1])
        for j in range(1, 4):
            V("dve", "scalar_tensor_tensor", [xi, convw, o], [o], out=o[:], in0=xi[:, j:j + 512],
              scalar=convw[:, ci * 4 + j:ci * 4 + j + 1], in1=o[:], op0=ALU.mult, op1=ALU.add)
        V("act", "activation", [o], [o], out=o[:], in_=o[:], func=AF.Silu)
        V("dve", "tensor_copy", [xi], [xi], out=xi[:, 0:3], in_=xi[:, 512:515])
        return o

    def l2n(o, scale):
        V("act", "activation", [o], [sq[0]], out=sq[0][:], in_=o[:], func=AF.Square)
        k.mm(pz[:], ones16[:], sq[0][:], True, True, [ones16, sq[0]], pz)
        V("act", "activation", [pz, eps1], [rz], out=rz[:], in_=pz[:], func=AF.Sqrt, bias=eps1[:, 0:1], scale=1.0)
        V("dve", "reciprocal", [rz], [rz], out=rz[:], in_=rz[:])
        V("dve", "scalar_tensor_tensor", [o, rz], [o], out=o[:], in0=o[:], scalar=float(scale), in1=rz[:], op0=ALU.mult, op1=ALU.mult)

    sti = [0]
    ei = [0]

    def attn(i, blk, qparts, scale, bias_rows=None):
        outs = []
        nkb = 4 * blk + 4
        for ci, psl in enumerate(qparts):
            for kb in range(nkb):
                st = pst[sti[0] % 2]
                sti[0] += 1
                k.mm(st[:], kcache[i][psl, kb * 128:(kb + 1) * 128], qa[i][psl, :], True, bias_rows is None, [kcache[i], qa[i]], st)
                E = Et[ei[0] % NET]
                ei[0] += 1
                if bias_rows is not None:
                    nc_, rq_ = bias_rows
                    k.mm(st[:], ones32[32 * i:32 * i + 1, 0:128], rq_[:, :], False, True, [ones32, rq_], st)
                    if kb >= 4 * blk:
                        V("dve", "tensor_tensor", [st, masks], [rz], out=rz[:], in0=st[:], in1=masks[:, kb - 4 * blk, :], op=ALU.add)
                        V("act", "activation", [rz, nc_], [E], out=E[:], in_=rz[:], func=AF.Exp, bias=nc_[:, kb:kb + 1], scale=float(scale))
                    else:
                        V("act", "activation", [st, nc_], [E], out=E[:], in_=st[:], func=AF.Exp, bias=nc_[:, kb:kb + 1], scale=float(scale))
                else:
                    V("act", "activation", [st], [E], out=E[:], in_=st[:], func=AF.Exp, scale=float(scale))
                    if kb >= 4 * blk:
                        V("dve", "tensor_tensor", [E, masks], [E], out=E[:], in0=E[:], in1=masks[:, kb - 4 * blk, :], op=ALU.mult)
                k.mm(po[:], vcache[i][:, kb, :], E[:], kb == 0, kb == nkb - 1, [vcache[i], E], po)
                k.mm(pz[:], ones16[:], E[:], kb == 0, kb == nkb - 1, [ones16, E], pz)
            R = Rt[ci]
            V("dve", "reciprocal", [pz], [rz], out=rz[:], in_=pz[:])
            V("dve", "tensor_tensor", [po, rz], [R], out=R[:], in0=po[:], in1=rz[:], op=ALU.mult)
            outs.append(R)
        return outs

    def decay_prep(sm, pA, lfcol, lf2, i):
        V("dve", "tensor_scalar_mul", [ones32, lf, gts], [sm["lfb"]], out=sm["lfb"][:], in0=ones32[:], scalar1=lfcol)
        k.mm(pA[:], sm["lfb"][:], Um[:], True, True, [sm["lfb"], Um], pA)
        V("act", "copy", [pA], [sm["crep"]], out=sm["crep"][:], in_=pA[:])
        V("dve", "tensor_tensor", [sm["crep"], ident], [sm["junk"]], out=sm["junk"][:], in0=sm["crep"][:], in1=ident[:], op=ALU.mult)
        V("dve", "reduce_sum", [sm["junk"]], [sm["bc"]], out=sm["bc"][:, 0:1], in_=sm["junk"][:], axis=AX.X)
        V("dve", "tensor_copy", [sm["crep"]], [sm["bc"]], out=sm["bc"][0:64, 1:2], in_=sm["crep"][0:64, 63:64])
        V("dve", "tensor_copy", [sm["crep"]], [sm["bc"]], out=sm["bc"][64:128, 1:2], in_=sm["crep"][64:128, 127:128])
        V("act", "activation", [sm["crep"]], [sm["ecr"]], out=sm["ecr"][:], in_=sm["crep"][:], func=AF.Exp)

    def post_out(sm, pG, i, t4, src_ps, gate_func):
        V("act", "activation", [sm["hh"]], [sm["junk"], sm["ssq"]], out=sm["junk"][:], in_=sm["hh"][:], func=AF.Square,
          accum_out=sm["ssq"][:])
        V("act", "activation", [sm["ssq"], eps1], [sm["rs"]], out=sm["rs"][:], in_=sm["ssq"][:], func=AF.Sqrt, bias=eps1[:, 0:1],
          scale=1.0 / 128.0)
        V("dve", "reciprocal", [sm["rs"]], [sm["rs"]], out=sm["rs"][:], in_=sm["rs"][:])
        V("dve", "scalar_tensor_tensor", [sm["hh"], sm["rs"], nrmrep], [sm["hn"]], out=sm["hn"][:], in0=sm["hh"][:],
          scalar=sm["rs"][:, 0:1], in1=nrmrep[:], op0=ALU.mult, op1=ALU.mult)
        V("act", "activation", [gtok[i]], [sm["sgo"]], out=sm["sgo"][:], in_=gtok[i][:, t4, :], func=gate_func)
        V("dve", "tensor_tensor", [sm["hn"], sm["sgo"]], [sm["ob"]], out=sm["ob"][:], in0=sm["hn"][:], in1=sm["sgo"][:], op=ALU.mult)
        k.tr(pG[:], sm["ob"][:], ident[:], [sm["ob"], ident], pG)
        V("act", "copy", [pG], [oT], out=oT[:, 2 + i, t4 * 128:(t4 + 1) * 128], in_=pG[:])

    def record(fn):
        lst = []
        o_op, o_dma = k.S.op, k.S.dma
        k.S.op = lambda *a_, **kw_: lst.append((o_op, a_, kw_))
        k.S.dma = lambda *a_, **kw_: lst.append((o_dma, a_, kw_))
        try:
            fn()
        finally:
            k.S.op, k.S.dma = o_op, o_dma
        return lst

    def interleave(lists):
        lists = [l_ for l_ in lists if l_]
        if not lists:
            return
        import os
        if os.environ.get("MK_SEQ"):
            for l_ in lists:
                for f_, a_, kw_ in l_:
                    f_(*a_, **kw_)
            return
        n = max(len(l_) for l_ in lists)
        pos = [0] * len(lists)
        for tick in range(1, n + 1):
            for li, l_ in enumerate(lists):
                tgt = (tick * len(l_) + n - 1) // n
                while pos[li] < min(tgt, len(l_)):
                    f_, a_, kw_ = l_[pos[li]]
                    f_(*a_, **kw_)
                    pos[li] += 1

    V("dve", "memset", [], [oT], ap=oT[:], constant=0.0)
    for blk in range(NB):
        tok = slice(blk * 512, (blk + 1) * 512)
        if stop == 0:
            k.store(io["out"](blk), x32[:], x32, ores)
            continue
        k.load(x32[:], io["h"](blk), x32, reads=io["h_res"])
        for c in range(8):
            s_ = sq[c % 2]
            V("act", "activation", [x32], [s_], out=s_[:], in_=x32[:, c], func=AF.Square)
            k.mm(pz[:], ones16[:], s_[:], c == 0, c == 7, [ones16, s_], pz)
        V("act", "activation", [pz, epsb], [rstd], out=rstd[:], in_=pz[:], func=AF.Sqrt, bias=epsb[:, 0:1], scale=1.0)
        V("dve", "reciprocal", [rstd], [rstd], out=rstd[:], in_=rstd[:])
        for c in range(8):
            V("dve", "scalar_tensor_tensor", [x32, gain, rstd], [x32], out=x32[:, c], in0=x32[:, c], scalar=gain[:, c:c + 1],
              in1=rstd[:], op0=ALU.mult, op1=ALU.mult)
        V("act", "copy", [x32], [hn16], out=hn16[:], in_=x32[:])
        if stop == 1:
            k.store(io["out"](blk), x32[:], x32, ores)
            continue
        for t4 in range(4):
            for c in range(8):
                k.mm(pH[:, t4 * NG:(t4 + 1) * NG], x32[:, c, t4 * 128:(t4 + 1) * 128], wg32[:, c * NG:(c + 1) * NG], c == 0, c == 7,
                     [x32, wg32], pH)
        V("dve", "tensor_tensor", [pH, gbias], [gts], out=gts[:], in0=pH[:, 0:4 * NG].rearrange("p (a b) -> p a b", a=4),
          in1=gbias[:].unsqueeze(1).to_broadcast([128, 4, NG]), op=ALU.add)

        if stop == 2:
            k.store(io["out"](blk), x32[:], x32, ores)
            continue
        if not odd:
            k.load(cost[:], cos_d[:, tok], cost)
            k.load(sint[:], sin_d[:, tok], sint)
            for i in range(2):
                for which, dst in ((0, qa[i]), (2, None)):
                    p = proj_fm(i * 4 + which)
                    V("dve", "tensor_tensor", [p, cost], [rt1], out=rt1[:], in0=p[:], in1=cost[:], op=ALU.mult)
                    p2 = proj_fm(i * 4 + which + 1)
                    V("dve", "tensor_tensor", [p2, sint], [rt2], out=rt2[:], in0=p2[:], in1=sint[:], op=ALU.mult)
                    if dst is not None:
                        V("dve", "tensor_tensor", [rt1, rt2], [dst], out=dst[:], in0=rt1[:], in1=rt2[:], op=ALU.add)
                    else:
                        V("dve", "tensor_tensor", [rt1, rt2], [kcache[i]], out=kcache[i][:, tok], in0=rt1[:], in1=rt2[:], op=ALU.add)
                p = proj_tm(i)
                V("act", "copy", [p], [vcache[i]], out=vcache[i][:, blk * 4:(blk + 1) * 4, :], in_=p[:].rearrange("p (a b) -> p a b", a=4))
            V("act", "activation", [gts], [gt2], out=gt2[:, :, 2:4], in_=gts[:, :, 2:4], func=AF.Exp, scale=-1.0)
            V("act", "activation", [gt2, onec], [gt2], out=gt2[:, :, 2:4], in_=gt2[:, :, 2:4], func=AF.Ln, bias=onec[:, 0:1], scale=1.0)
            V("dve", "tensor_scalar_mul", [gt2], [lf], out=lf[:, :, 2:4], in0=gt2[:, :, 2:4], scalar1=-1.0)
            def att_task():
                for i in range(0 if skip_attn else 2):
                    R = attn(i, blk, [slice(0, 64), slice(64, 128)], 64 ** -0.5)
                    V("dve", "scalar_tensor_tensor", [R[0], R[1], lamt], [Xt], out=Xt[:], in0=R[1][:], scalar=lamt[:, 4:5], in1=R[0][:],
                      op0=ALU.mult, op1=ALU.add)
                    V("act", "activation", [Xt], [sq[0]], out=sq[0][:], in_=Xt[:], func=AF.Square)
                    k.mm(pz[:], ones16[:], sq[0][:], True, True, [ones16, sq[0]], pz)
                    V("act", "activation", [pz, eps1], [rz], out=rz[:], in_=pz[:], func=AF.Sqrt, bias=eps1[:, 0:1], scale=1.0 / 128.0)
                    V("dve", "reciprocal", [rz], [rz], out=rz[:], in_=rz[:])
                    V("dve", "scalar_tensor_tensor", [Xt, subc, rz], [oT], out=oT[:, i, :], in0=Xt[:], scalar=subc[:, 0:1], in1=rz[:],
                      op0=ALU.mult, op1=ALU.mult)

            preps = {}
            for i in range(0 if skip_rec else 2):
                qc = conv_silu(proj_fm(8 + 2 * i), 2 * i, blk)
                kc = conv_silu(proj_fm(8 + 2 * i + 1), 2 * i + 1, blk)
                p = proj_tm(2 + 2 * i)
                V("act", "copy", [p], [vaug[i]], out=vaug[i][:, :, 0:128], in_=p[:].rearrange("p (a b) -> p a b", a=4))
                p = proj_tm(2 + 2 * i + 1)
                V("act", "copy", [p], [gtok[i]], out=gtok[i][:], in_=p[:].rearrange("p (a b) -> p a b", a=4))
                preps[i] = (qc, kc)

            def rec_task(i):
                qc, kc = preps[i]
                sm = smh[i]
                sl_ = slots[i]
                pA, pF, pD, pE, pC, pG = sl_["A"], sl_["F"], sl_["D"], sl_["E"], sl_["C"], sl_["G"]
                for t4 in range(4 if stop > 10 else 0):
                    cs = slice(t4 * 128, (t4 + 1) * 128)
                    decay_prep(sm, pA, lf[:, t4, 2 + i:3 + i], lf[:, t4, 0:4], 2 + i)
                    if stop == 105:
                        continue
                    V("dve", "tensor_tensor", [sm["bc"], gts], [sm["rcol"]], out=sm["rcol"][:], in0=sm["bc"][:, 0:1], in1=gts[:, t4, i:i + 1],
                      op=ALU.subtract)
                    V("dve", "tensor_tensor", [sm["bc"], sm["rcol"]], [sm["e1"]], out=sm["e1"][:], in0=sm["bc"][:, 1:2], in1=sm["rcol"][:],
                      op=ALU.subtract)
                    V("act", "activation", [sm["e1"]], [sm["e1"]], out=sm["e1"][:], in_=sm["e1"][:], func=AF.Exp)
                    V("dve", "tensor_scalar_mul", [sm["e1"]], [sm["e1"]], out=sm["e1"][:], in0=sm["e1"][:], scalar1=float(128 ** -0.5))
                    if stop == 11:
                        continue
                    V("dve", "scalar_tensor_tensor", [sm["crep"], sm["rcol"], MBu], [sm["Dm"]], out=sm["Dm"][:], in0=sm["crep"][:],
                      scalar=sm["rcol"][:, 0:1], in1=MBu[:], op0=ALU.subtract, op1=ALU.add)
                    V("act", "activation", [sm["Dm"]], [sm["G"]], out=sm["G"][:], in_=sm["Dm"][:], func=AF.Exp)
                    V("dve", "tensor_tensor", [qc, sm["ecr"]], [qhat[i]], out=qhat[i][:, 0, 0:64], in0=qc[:, t4 * 128:t4 * 128 + 64],
                      in1=sm["ecr"][:, 0:64], op=ALU.mult)
                    V("dve", "tensor_tensor", [qc, sm["ecr"]], [qhat[i]], out=qhat[i][:, 1, 64:128], in0=qc[:, t4 * 128 + 64:(t4 + 1) * 128],
                      in1=sm["ecr"][:, 64:128], op=ALU.mult)
                    k.mm(pC[:], kc[:, cs], qc[:, cs], True, True, [kc, qc], pC)
                    V("dve", "scalar_tensor_tensor", [pC, sm["G"]], [sm["AT"]], out=sm["AT"][:], in0=pC[:], scalar=float(128 ** -0.5),
                      in1=sm["G"][:], op0=ALU.mult, op1=ALU.mult)
                    if stop == 12:
                        continue
                    k.tr(pD[:], kc[:, cs], ident[:], [kc, ident], pD)
                    V("dve", "tensor_scalar_mul", [pD, sm["e1"]], [sm["k2"]], out=sm["k2"][:], in0=pD[:], scalar1=sm["e1"][:, 0:1])
                    if stop == 13:
                        continue
                    S0, S1 = Sst[i]
                    k.mm(pE[:], sm["AT"][:], vaug[i][:, t4, :], True, False, [sm["AT"], vaug[i]], pE)
                    k.mm(pE[:], qhat[i][:, 0, :], S0[:], False, False, [qhat[i], S0], pE)
                    k.mm(pF[:], sm["k2"][0:64, :], vaug[i][0:64, t4, :], True, True, [sm["k2"], vaug[i]], pF)
                    V("dve", "scalar_tensor_tensor", [S0, sm["ecr"], pF], [S1], out=S1[:], in0=S0[:], scalar=sm["ecr"][:, 63:64], in1=pF[:],
                      op0=ALU.mult, op1=ALU.add)
                    k.mm(pE[:], qhat[i][:, 1, :], S1[:], False, True, [qhat[i], S1], pE)
                    k.mm(pF[:], sm["k2"][64:128, :], vaug[i][64:128, t4, :], True, True, [sm["k2"], vaug[i]], pF)
                    V("dve", "scalar_tensor_tensor", [S1, sm["ecr"], pF], [S0], out=S0[:], in0=S1[:], scalar=sm["ecr"][:, 127:128], in1=pF[:],
                      op0=ALU.mult, op1=ALU.add)
                    if stop == 14:
                        continue
                    V("act", "activation", [pE], [sm["dm"]], out=sm["dm"][:], in_=pE[:, 128:129], func=AF.Abs)
                    V("dve", "tensor_scalar_max", [sm["dm"]], [sm["dm"]], out=sm["dm"][:], in0=sm["dm"][:], scalar1=1.0)
                    V("dve", "reciprocal", [sm["dm"]], [sm["dm"]], out=sm["dm"][:], in_=sm["dm"][:])
                    V("dve", "tensor_scalar_mul", [pE, sm["dm"]], [sm["hh"]], out=sm["hh"][:], in0=pE[:, 0:128], scalar1=sm["dm"][:, 0:1])
                    if stop == 15:
                        continue
                    post_out(sm, pG, i, t4, None, AF.Sigmoid)

            tl = [record(att_task)] + [record(lambda i=i: rec_task(i)) for i in range(0 if skip_rec else 2)]
            interleave(tl)
        else:
            V("act", "activation", [gts], [gt2], out=gt2[:, :, 0:2], in_=gts[:, :, 0:2], func=AF.Exp)
            V("act", "activation", [gts], [gt2], out=gt2[:, :, 4:6], in_=gts[:, :, 4:6], func=AF.Exp, scale=-1.0)
            V("act", "activation", [gt2, onec], [gt2], out=gt2[:, :, 0:2], in_=gt2[:, :, 0:2], func=AF.Ln, bias=onec[:, 0:1], scale=1.0)
            V("act", "activation", [gt2, onec], [gt2], out=gt2[:, :, 4:6], in_=gt2[:, :, 4:6], func=AF.Ln, bias=onec[:, 0:1], scale=1.0)
            V("dve", "tensor_tensor", [gt2, alog], [lf], out=lf[:, :, 0:2], in0=gt2[:, :, 0:2],
              in1=alog[:].unsqueeze(1).to_broadcast([128, 4, 2]), op=ALU.mult)
            V("dve", "tensor_scalar_mul", [gt2], [lf], out=lf[:, :, 4:6], in0=gt2[:, :, 4:6], scalar1=-1.0)
            V("act", "activation", [gts], [lf], out=lf[:, :, 2:4], in_=gts[:, :, 2:4], func=AF.Sigmoid)
            for t4 in range(4):
                for i in range(2):
                    V("dve", "tensor_scalar_mul", [ones32, lf], [sm["lfb"]], out=sm["lfb"][:], in0=ones32[:], scalar1=lf[:, t4, 4 + i:5 + i])
                    k.mm(pA[:], sm["lfb"][:], Ufm[:], True, True, [sm["lfb"], Ufm], pA)
                    V("dve", "tensor_scalar", [pA, carry], [Rq[i]], out=Rq[i][:, t4 * 128:(t4 + 1) * 128], in0=pA[32 * i:32 * i + 1, :],
                      scalar1=carry[32 * i:32 * i + 1, i:i + 1], scalar2=None, op0=ALU.add)
                    V("dve", "tensor_tensor", [pA, ident], [sm["junk"]], out=sm["junk"][:], in0=pA[:], in1=ident[:], op=ALU.mult)
                    V("dve", "reduce_sum", [sm["junk"]], [sm["tmpc"]], out=sm["tmpc"][:], in_=sm["junk"][:], axis=AX.X)
                    V("dve", "tensor_scalar", [sm["tmpc"], carry], [ncum[i]], out=ncum[i][:, blk * 4 + t4:blk * 4 + t4 + 1], in0=sm["tmpc"][:],
                      scalar1=carry[:, i:i + 1], scalar2=-1.0, op0=ALU.add, op1=ALU.mult)
                    V("dve", "tensor_tensor", [pA, carry], [carry], out=carry[:, i:i + 1], in0=carry[:, i:i + 1], in1=pA[:, 127:128], op=ALU.add)
            for i in range(2):
                p = proj_fm(6 + 2 * i)
                V("act", "mul", [p], [qa[i]], out=qa[i][:], in_=p[:], mul=float(128 ** -0.5))
                p = proj_fm(6 + 2 * i + 1)
                V("act", "copy", [p], [kcache[i]], out=kcache[i][:, tok], in_=p[:])
                p = proj_tm(2 + i)
                V("act", "copy", [p], [vcache[i]], out=vcache[i][:, blk * 4:(blk + 1) * 4, :], in_=p[:].rearrange("p (a b) -> p a b", a=4))
            def att_task():
                for i in range(0 if skip_attn else 2):
                    R = attn(i, blk, [slice(0, 128)], 1.0, bias_rows=(ncum[i], Rq[i]))
                    V("act", "copy", [R[0]], [oT], out=oT[:, i, :], in_=R[0][:])

            preps = {}
            for i in range(0 if skip_rec else 2):
                qc = conv_silu(proj_fm(3 * i), 3 * i, blk)
                kc = conv_silu(proj_fm(3 * i + 1), 3 * i + 1, blk)
                vc = conv_silu(proj_fm(3 * i + 2), 3 * i + 2, blk)
                l2n(qc, 128 ** -0.5)
                l2n(kc, 1.0)
                p = proj_tm(i)
                V("act", "copy", [p], [gtok[i]], out=gtok[i][:], in_=p[:].rearrange("p (a b) -> p a b", a=4))
                preps[i] = (qc, kc, vc)

            def rec_task(i):
                qc, kc, vc = preps[i]
                sm = smh[i]
                sl_ = slots[i]
                pA, pF, pD, pE, pC, pG = sl_["A"], sl_["F"], sl_["D"], sl_["E"], sl_["C"], sl_["G"]
                for t4 in range(4):
                    cs = slice(t4 * 128, (t4 + 1) * 128)
                    decay_prep(sm, pA, lf[:, t4, i:i + 1], lf[:, t4, 0:4], i)
                    bcol = sm["bc"][:, 0:1]
                    beta = lf[:, t4, 2 + i:3 + i]
                    V("dve", "scalar_tensor_tensor", [sm["crep"], sm["bc"], MBu], [sm["Dm"]], out=sm["Dm"][:], in0=sm["crep"][:],
                      scalar=bcol, in1=MBu[:], op0=ALU.subtract, op1=ALU.add)
                    V("act", "activation", [sm["Dm"]], [sm["G"]], out=sm["G"][:], in_=sm["Dm"][:], func=AF.Exp)
                    V("dve", "scalar_tensor_tensor", [sm["crep"], sm["bc"], MBl], [sm["Dl"]], out=sm["Dl"][:], in0=sm["crep"][:],
                      scalar=bcol, in1=MBl[:], op0=ALU.subtract, op1=ALU.add)
                    V("act", "activation", [sm["Dl"]], [sm["Gl"]], out=sm["Gl"][:], in_=sm["Dl"][:], func=AF.Exp, scale=-1.0)
                    k.tr(pD[:], kc[:, cs], ident[:], [kc, ident], pD)
                    V("act", "copy", [pD], [sm["ktok"]], out=sm["ktok"][:], in_=pD[:])
                    k.tr(pD[:], vc[:, cs], ident[:], [vc, ident], pD)
                    V("dve", "tensor_scalar_mul", [pD, lf], [sm["vb"]], out=sm["vb"][:], in0=pD[:], scalar1=beta)
                    V("act", "activation", [sm["bc"]], [sm["bcol"]], out=sm["bcol"][:], in_=sm["bc"][:, 0:1], func=AF.Exp)
                    V("dve", "tensor_tensor", [sm["bcol"], lf], [sm["bebc"]], out=sm["bebc"][:], in0=sm["bcol"][:], in1=beta, op=ALU.mult)
                    V("dve", "tensor_tensor", [sm["bc"]], [sm["ebd"]], out=sm["ebd"][:], in0=sm["bc"][:, 1:2], in1=sm["bc"][:, 0:1],
                      op=ALU.subtract)
                    V("act", "activation", [sm["ebd"]], [sm["ebd"]], out=sm["ebd"][:], in_=sm["ebd"][:], func=AF.Exp)
                    V("dve", "tensor_scalar_mul", [sm["ktok"], sm["bebc"]], [sm["kp"]], out=sm["kp"][:], in0=sm["ktok"][:],
                      scalar1=sm["bebc"][:, 0:1])
                    V("dve", "tensor_scalar_mul", [sm["ktok"], sm["ebd"]], [sm["kd"]], out=sm["kd"][:], in0=sm["ktok"][:],
                      scalar1=sm["ebd"][:, 0:1])
                    k.mm(pC[:], kc[:, cs], kc[:, cs], True, True, [kc], pC)
                    V("dve", "scalar_tensor_tensor", [pC, lf, sm["Gl"]], [sm["N"]], out=sm["N"][:], in0=pC[:], scalar=beta, in1=sm["Gl"][:],
                      op0=ALU.mult, op1=ALU.mult)
                    k.tr(pC[:], sm["N"][:], ident[:], [sm["N"], ident], pC)
                    V("act", "copy", [pC], [sm["M"]], out=sm["M"][:], in_=pC[:])
                    V("dve", "tensor_tensor", [ident, sm["M"]], [sm["Y"]], out=sm["Y"][:], in0=ident[:], in1=sm["M"][:], op=ALU.subtract)
                    Pc, Mc = sm["N"], sm["M"]
                    Pn, Mn = sm["P"], sm["Mk"]
                    for lev in range(5):
                        k.mm(pC[:], Mc[:], Pc[:], True, True, [Mc, Pc], pC)
                        if lev < 4:
                            k.mm(pD[:], Pc[:], Mc[:], True, True, [Mc, Pc], pD)
                        V("act", "copy", [pC], [Pn], out=Pn[:], in_=pC[:])
                        if lev < 4:
                            V("dve", "tensor_copy", [pD], [Mn], out=Mn[:], in_=pD[:])
                        k.mm(pA[:], Pn[:], sm["Y"][:], True, True, [Pn, sm["Y"]], pA)
                        V("dve", "tensor_tensor", [sm["Y"], pA], [sm["Y"]], out=sm["Y"][:], in0=sm["Y"][:], in1=pA[:], op=ALU.add)
                        Pc, Pn = Pn, Pc
                        Mc, Mn = Mn, Mc
                    k.mm(pC[:], sm["Y"][:], sm["vb"][:], True, True, [sm["Y"], sm["vb"]], pC)
                    V("act", "copy", [pC], [sm["u"]], out=sm["u"][:], in_=pC[:])
                    k.mm(pD[:], sm["kp"][:], sm["Y"][:], True, True, [sm["Y"], sm["kp"]], pD)
                    V("dve", "memset", [], [sm["wT0"]], ap=sm["wT0"][:], constant=0.0)
                    V("dve", "memset", [], [sm["wT1"]], ap=sm["wT1"][:], constant=0.0)
                    V("dve", "tensor_copy", [pD], [sm["wT0"]], out=sm["wT0"][:, 0:64], in_=pD[:, 0:64])
                    V("dve", "tensor_copy", [pD], [sm["wT1"]], out=sm["wT1"][:, 64:128], in_=pD[:, 64:128])
                    k.mm(pC[:], kc[:, cs], qc[:, cs], True, True, [kc, qc], pC)
                    V("dve", "tensor_tensor", [pC, sm["G"]], [sm["AT"]], out=sm["AT"][:], in0=pC[:], in1=sm["G"][:], op=ALU.mult)
                    V("dve", "tensor_tensor", [qc, sm["ecr"]], [qhat[i]], out=qhat[i][:, 0, 0:64], in0=qc[:, t4 * 128:t4 * 128 + 64],
                      in1=sm["ecr"][:, 0:64], op=ALU.mult)
                    V("dve", "tensor_tensor", [qc, sm["ecr"]], [qhat[i]], out=qhat[i][:, 1, 64:128], in0=qc[:, t4 * 128 + 64:(t4 + 1) * 128],
                      in1=sm["ecr"][:, 64:128], op=ALU.mult)
                    S0, S1 = Sst[i]
                    k.mm(pA[:], sm["wT0"][:], S0[:, 0:128], True, True, [sm["wT0"], S0], pA)
                    V("dve", "tensor_tensor", [sm["u"], pA], [sm["vn"]], out=sm["vn"][0:64, :], in0=sm["u"][0:64, :], in1=pA[0:64, :],
                      op=ALU.subtract)
                    k.mm(pE[:, 0:128], qhat[i][:, 0, :], S0[:, 0:128], True, False, [qhat[i], S0], pE)
                    k.mm(pF[:, 0:128], sm["kd"][0:64, :], sm["vn"][0:64, :], True, True, [sm["kd"], sm["vn"]], pF)
                    V("dve", "scalar_tensor_tensor", [S0, sm["ecr"], pF], [S1], out=S1[:, 0:128], in0=S0[:, 0:128], scalar=sm["ecr"][:, 63:64],
                      in1=pF[:, 0:128], op0=ALU.mult, op1=ALU.add)
                    k.mm(pA[:], sm["wT1"][:], S1[:, 0:128], True, True, [sm["wT1"], S1], pA)
                    V("dve", "tensor_tensor", [sm["u"], pA], [sm["vn"]], out=sm["vn"][64:128, :], in0=sm["u"][64:128, :], in1=pA[64:128, :],
                      op=ALU.subtract)
                    k.mm(pE[:, 0:128], qhat[i][:, 1, :], S1[:, 0:128], False, False, [qhat[i], S1], pE)
                    k.mm(pE[:, 0:128], sm["AT"][:], sm["vn"][:], False, True, [sm["AT"], sm["vn"]], pE)
                    k.mm(pF[:, 0:128], sm["kd"][64:128, :], sm["vn"][64:128, :], True, True, [sm["kd"], sm["vn"]], pF)
                    V("dve", "scalar_tensor_tensor", [S1, sm["ecr"], pF], [S0], out=S0[:, 0:128], in0=S1[:, 0:128], scalar=sm["ecr"][:, 127:128],
                      in1=pF[:, 0:128], op0=ALU.mult, op1=ALU.add)
                    V("act", "copy", [pE], [sm["hh"]], out=sm["hh"][:], in_=pE[:, 0:128])
                    post_out(sm, pG, i, t4, None, AF.Silu)


            tl = [record(att_task)] + [record(lambda i=i: rec_task(i)) for i in range(0 if skip_rec else 2)]
            interleave(tl)

        for c in range(8):
            p = pj[pji[0] % 2]
            pji[0] += 1
            for hs in range(4):
                k.mm(p[:], wout16[:, hs, c * 128:(c + 1) * 128], oT[:, hs, :], hs == 0, hs == 3, [wout16, oT], p)
            V("act", "copy", [p], [x32], out=x32[:, c], in_=p[:])
        k.store(io["out"](blk), x32[:], x32, ores)
    if not standalone:
        return None, k
    nc = k.finish([ores])
    return nc, k


def _kc(w):
    n = w.shape[1]
    return np.ascontiguousarray(w.reshape(8, 128, n).transpose(1, 0, 2).reshape(128, 8 * n))


def mix_consts(S, odd):
    idx = np.arange(128)
    same = (idx[:, None] // 64) == (idx[None, :] // 64)
    U = ((idx[:, None] <= idx[None, :]) & same).astype(np.float32)
    BD = same.astype(np.float32)
    MBu = np.where((idx[None, :] >= idx[:, None]) & same, 0.0, -30000.0).astype(np.float32)
    MBl = np.where((idx[:, None] > idx[None, :]) & same, 0.0, 30000.0).astype(np.float32)
    Uf = (idx[:, None] <= idx[None, :]).astype(np.float32)
    t = np.arange(512)
    masks = np.zeros((4, 128, 512), np.float32)
    for j in range(4):
        key = 128 * j + idx
        if odd:
            masks[j] = (key[:, None] <= t[None, :])
        else:
            masks[j] = ((key[:, None] // 64) <= (t[None, :] // 64))
    d = {"ident": np.eye(128, dtype=np.float32), "U": U, "BD": BD, "MBu": MBu, "MBl": MBl, "masks": masks}
    if odd:
        d["Uf"] = Uf
    else:
        inv = (10000.0 ** (-np.arange(0, 64, 2, dtype=np.float32) / np.float32(64))).astype(np.float32)
        ang = np.arange(S, dtype=np.float32)[None, :] * inv[:, None]
        cos, sin = np.cos(ang).astype(np.float32), np.sin(ang).astype(np.float32)
        p = np.arange(128)
        sign = np.where((p % 64) < 32, -1.0, 1.0).astype(np.float32)
        d["cosT"] = np.ascontiguousarray(cos[p % 32])
        d["sinT"] = np.ascontiguousarray(sin[p % 32] * sign[:, None])
    return d


def mix_inputs_even(hh, w_in, w_out, lq1, lk1, lq2, lk2, subln, conv_b, ig, fg, b_norm, gain):
    hs = [2 * hh, 2 * hh + 1]
    r = np.arange(128)
    swap = np.concatenate([r[32:64], r[0:32], r[96:128], r[64:96]])
    fcols = []
    for a in hs:
        fcols += [128 * a + r, 128 * a + swap, 512 + 128 * a + r, 512 + 128 * a + swap]
    for b in hs:
        fcols += [1536 + 128 * b + r, 2048 + 128 * b + r]
    tcols = [1024 + 128 * a + r for a in hs]
    for b in hs:
        tcols += [2560 + 128 * b + r, 3072 + 128 * b + r]
    gcols = [3584 + hs[0], 3584 + hs[1], 3588 + hs[0], 3588 + hs[1]]
    orow = np.concatenate([128 * hs[0] + r, 128 * hs[1] + r, 512 + 128 * hs[0] + r, 512 + 128 * hs[1] + r])
    cw = np.zeros((128, 16), np.float32)
    for i, b in enumerate(hs):
        cw[:, (2 * i) * 4:(2 * i) * 4 + 4] = conv_b[:, 128 * b + r].T
        cw[:, (2 * i + 1) * 4:(2 * i + 1) * 4 + 4] = conv_b[:, 512 + 128 * b + r].T
    return {"gain": chunk_cols(gain), "wf": np.ascontiguousarray(w_in[:, np.concatenate(fcols)]),
            "wt": np.ascontiguousarray(w_in[:, np.concatenate(tcols)]), "wgt": _kc(w_in[:, gcols]),
            "gbias": np.array([[ig[hs[0]], ig[hs[1]], fg[hs[0]], fg[hs[1]]]], np.float32),
            "wout": np.ascontiguousarray(w_out[orow]), "convw": cw, "nrm": np.ascontiguousarray(b_norm[None, :]),
            "lamv": np.concatenate([lq1, lk1, lq2, lk2])[None, :].astype(np.float32), "subln": np.ascontiguousarray(subln[:, None])}


def mix_inputs_odd(hh, w_in, w_out, conv_c, a_log, dt_bias, c_norm, fd_bias, gain):
    hs = [2 * hh, 2 * hh + 1]
    r = np.arange(128)
    fcols = []
    for c in hs:
        fcols += [128 * c + r, 512 + 128 * c + r, 1024 + 128 * c + r]
    for d in hs:
        fcols += [2056 + 128 * d + r, 2568 + 128 * d + r]
    tcols = [1536 + 128 * c + r for c in hs] + [3080 + 128 * d + r for d in hs]
    gcols = [2048 + hs[0], 2048 + hs[1], 2052 + hs[0], 2052 + hs[1], 3592 + hs[0], 3592 + hs[1]]
    orow = np.concatenate([512 + 128 * hs[0] + r, 512 + 128 * hs[1] + r, 128 * hs[0] + r, 128 * hs[1] + r])
    cw = np.zeros((128, 24), np.float32)
    for i, c in enumerate(hs):
        for j, off in enumerate((0, 512, 1024)):
            g = 3 * i + j
            cw[:, g * 4:g * 4 + 4] = conv_c[:, off + 128 * c + r].T
    return {"gain": chunk_cols(gain), "wf": np.ascontiguousarray(w_in[:, np.concatenate(fcols)]),
            "wt": np.ascontiguousarray(w_in[:, np.concatenate(tcols)]), "wgt": _kc(w_in[:, gcols]),
            "gbias": np.array([[dt_bias[hs[0]], dt_bias[hs[1]], 0.0, 0.0, fd_bias[hs[0]], fd_bias[hs[1]]]], np.float32),
            "wout": np.ascontiguousarray(w_out[orow]), "convw": cw, "nrm": np.ascontiguousarray(c_norm[None, :]),
            "alog": np.array([[a_log[hs[0]], a_log[hs[1]]]], np.float32)}


import math

_GROUPS = [[0, 1], [2, 3], [4, 5], [6, 7]]


def build_fused(S):
    T = S // 2
    NBH = T // 512
    k = KB()
    c8 = lambda ap, tok: ap[:, tok].rearrange("(c p) t -> p c t", p=128)
    xT = k.din("xT", [D, S])
    xh = k.din("xh", [D, T])
    outT = k.dout("outT", [D, T])
    part = [k.dint("part%d" % l, [2 * D, T]) for l in range(2)]
    psum = [k.dint("psumd%d" % l, [D, T]) for l in range(2)]
    h1 = k.dint("h1", [D, T])
    h1f = k.dint("h1f", [2 * D, T])
    part_r = [Res("part%d" % l) for l in range(2)]
    psum_r = [Res("psumd%d" % l) for l in range(2)]
    h1_r, h1f_r, out_r = Res("h1"), Res("h1f"), Res("out")

    def halfblk(ap):
        return lambda blk: ap[(blk // NBH) * D:(blk // NBH + 1) * D, (blk % NBH) * 512:(blk % NBH + 1) * 512].rearrange(
            "(c p) t -> p c t", p=128)

    k.pfx = "L0m_"
    build_mix(S, False, lam_init=0.8 - 0.6 * math.exp(-0.3 * 0), k=k,
              io={"h": lambda blk: c8(xT, slice(blk * 512, (blk + 1) * 512)), "h_res": [], "out": halfblk(part[0]), "out_res": part_r[0]})
    k.collective("ReduceScatter", ALU.add, part[0][:, :], psum[0][:, :], part_r[0], psum_r[0], _GROUPS)
    k.new_section("L0f_")
    build_ffn(T, False, SBT=min(1024, T), k=k,
              io={"h": lambda tok: c8(xh, tok), "h_res": [], "pins": [lambda tok: c8(psum[0], tok)], "pin_res": [psum_r[0]],
                  "out": lambda tok: c8(h1, tok), "out_res": h1_r})
    k.collective("AllGather", ALU.bypass, h1[:, :], h1f[:, :], h1_r, h1f_r, _GROUPS)
    k.new_section("L1m_")
    build_mix(S, True, k=k, io={"h": halfblk(h1f), "h_res": [h1f_r], "out": halfblk(part[1]), "out_res": part_r[1]})
    k.collective("ReduceScatter", ALU.add, part[1][:, :], psum[1][:, :], part_r[1], psum_r[1], _GROUPS)
    k.new_section("L1f_")
    build_ffn(T, True, SBT=min(1024, T), k=k,
              io={"h": lambda tok: c8(h1, tok), "h_res": [h1_r], "pins": [lambda tok: c8(psum[1], tok)], "pin_res": [psum_r[1]],
                  "out": lambda tok: c8(outT, tok), "out_res": out_r})
    nc = k.finish([out_r])
    return nc, k


_PROGS = {}


def build_fused4(S):
    k = KB()
    c8 = lambda ap, tok: ap[:, tok].rearrange("(c p) t -> p c t", p=128)
    blk8 = lambda ap: (lambda blk: c8(ap, slice(blk * 512, (blk + 1) * 512)))
    xT = k.din("xT", [D, S])
    outT = k.dout("outT", [D, S])
    pa = k.dint("partA", [D, S])
    pb = k.dint("partB", [D, S])
    h1 = k.dint("h1", [D, S])
    pa_r, pb_r, h1_r, out_r = Res("partA"), Res("partB"), Res("h1"), Res("out")
    first = True
    for layer in range(2):
        odd = layer % 2 == 1
        hsrc, hres = (xT, []) if layer == 0 else (h1, [h1_r])
        for tag, dst, dres in (("A", pa, pa_r), ("B", pb, pb_r)):
            if first:
                k.pfx = "L%dm%s_" % (layer, tag)
                first = False
            else:
                k.new_section("L%dm%s_" % (layer, tag))
            build_mix(S, odd, lam_init=0.8 - 0.6 * math.exp(-0.3 * layer), k=k,
                      io={"h": blk8(hsrc), "h_res": hres, "out": blk8(dst), "out_res": dres})
        k.new_section("L%df_" % layer)
        final = layer == 1
        odst, ores_ = (outT, out_r) if final else (h1, h1_r)
        build_ffn(S, final, SBT=min(1024, S), k=k,
                  io={"h": lambda tok, a=hsrc: c8(a, tok), "h_res": hres,
                      "pins": [lambda tok: c8(pa, tok), lambda tok: c8(pb, tok)], "pin_res": [pa_r, pb_r],
                      "out": lambda tok, a=odst: c8(a, tok), "out_res": ores_})
    nc = k.finish([out_r])
    return nc, k


def fused4_inputs(b, S, x, p, W):
    f = lambda a: np.asarray(a, np.float32)
    m = {"xT": np.ascontiguousarray(x[b].T)}
    ident, sel = ffn_consts()
    for layer in range(2):
        odd = layer % 2 == 1
        j = layer // 2
        cst = mix_consts(S, odd)
        for r, tag in ((0, "A"), (1, "B")):
            if odd:
                d = mix_inputs_odd(r, f(W["cd_w_in"][j]), f(W["cd_w_out"][j]), f(W["c_conv"][j]), f(W["c_a_log"][j]), f(W["c_dt_bias"][j]),
                                   f(W["c_norm"][j]), f(W["d_fgate_bias"][j]), f(W["norm_mix"][layer]))
            else:
                d = mix_inputs_even(r, f(W["ab_w_in"][j]), f(W["ab_w_out"][j]), f(W["a_lam_q1"][j]), f(W["a_lam_k1"][j]), f(W["a_lam_q2"][j]),
                                    f(W["a_lam_k2"][j]), f(W["a_subln"][j]), f(W["b_conv"][j]), f(W["b_igate_bias"][j]),
                                    f(W["b_fgate_bias"][j]), f(W["b_norm"][j]), f(W["norm_mix"][layer]))
            d.update(cst)
            for kk, vv in d.items():
                m["L%dm%s_%s" % (layer, tag, kk)] = vv
        Wr = np.concatenate([f(W["moe_w_group"][layer]), f(W["moe_w_router"][layer])], axis=1)
        fd = {"pT": np.ascontiguousarray(p[layer, b].T), "gain": chunk_cols(W["norm_ffn"][layer]), "gfin": chunk_cols(W["norm_final"]),
              "wr": np.ascontiguousarray(Wr.reshape(8, 128, 20).transpose(1, 0, 2).reshape(128, 160)),
              "br": np.concatenate([f(W["moe_b_group"][layer]), f(W["moe_b_router"][layer])])[None, :],
              "wg": tile_w(W["moe_w_gate"][layer]), "wu": tile_w(W["moe_w_up"][layer]), "wd": tile_w(W["moe_w_down"][layer]),
              "plg": f(W["ple_w_gate"][layer]), "plp": f(W["ple_w_proj"][layer]), "ident": ident, "sel": sel}
        for kk, vv in fd.items():
            m["L%df_%s" % (layer, kk)] = vv
    return m


def kernel(x, p, **W):
    x = np.asarray(x, np.float32)
    p = np.asarray(p, np.float32)
    B, S, _ = x.shape
    key = ("f4", S)
    if key not in _PROGS:
        _PROGS[key] = build_fused4(S)[0]
    nc = _PROGS[key]
    per_b = [fused4_inputs(b, S, x, p, W) for b in range(B)]
    maps = [per_b[c % B] for c in range(NCORES)]
    res = run_bass_kernel_spmd(nc, maps, core_ids=list(range(NCORES)))
    return np.stack([np.ascontiguousarray(res.results[b]["outT"].T) for b in range(B)]).astype(np.float32)


def fused_inputs(c, S, x, p, W):
    f = lambda a: np.asarray(a, np.float32)
    T = S // 2
    b, r = c // 2, c % 2
    ts = slice(r * T, (r + 1) * T)
    xTb = np.ascontiguousarray(x[b].T)
    m = {"xT": xTb, "xh": np.ascontiguousarray(xTb[:, ts])}
    ident, sel = ffn_consts()
    for layer in range(2):
        odd = layer % 2 == 1
        j = layer // 2
        if odd:
            d = mix_inputs_odd(r, f(W["cd_w_in"][j]), f(W["cd_w_out"][j]), f(W["c_conv"][j]), f(W["c_a_log"][j]), f(W["c_dt_bias"][j]),
                               f(W["c_norm"][j]), f(W["d_fgate_bias"][j]), f(W["norm_mix"][layer]))
        else:
            d = mix_inputs_even(r, f(W["ab_w_in"][j]), f(W["ab_w_out"][j]), f(W["a_lam_q1"][j]), f(W["a_lam_k1"][j]), f(W["a_lam_q2"][j]),
                                f(W["a_lam_k2"][j]), f(W["a_subln"][j]), f(W["b_conv"][j]), f(W["b_igate_bias"][j]),
                                f(W["b_fgate_bias"][j]), f(W["b_norm"][j]), f(W["norm_mix"][layer]))
        d.update(mix_consts(S, odd))
        for kk, vv in d.items():
            m["L%dm_%s" % (layer, kk)] = vv
        Wr = np.concatenate([f(W["moe_w_group"][layer]), f(W["moe_w_router"][layer])], axis=1)
        fd = {"pT": np.ascontiguousarray(p[layer, b, ts].T), "gain": chunk_cols(W["norm_ffn"][layer]), "gfin": chunk_cols(W["norm_final"]),
              "wr": np.ascontiguousarray(Wr.reshape(8, 128, 20).transpose(1, 0, 2).reshape(128, 160)),
              "br": np.concatenate([f(W["moe_b_group"][layer]), f(W["moe_b_router"][layer])])[None, :],
              "wg": tile_w(W["moe_w_gate"][layer]), "wu": tile_w(W["moe_w_up"][layer]), "wd": tile_w(W["moe_w_down"][layer]),
              "plg": f(W["ple_w_gate"][layer]), "plp": f(W["ple_w_proj"][layer]), "ident": ident, "sel": sel}
        for kk, vv in fd.items():
            m["L%df_%s" % (layer, kk)] = vv
    return m


def kernel_cc(x, p, **W):
    x = np.asarray(x, np.float32)
    p = np.asarray(p, np.float32)
    B, S, _ = x.shape
    T = S // 2
    if S not in _PROGS:
        _PROGS[S] = build_fused(S)[0]
    nc = _PROGS[S]
    maps = [fused_inputs(c, S, x, p, W) for c in range(NCORES)]
    res = run_bass_kernel_spmd(nc, maps, core_ids=list(range(NCORES)))
    out = np.empty((B, S, D), np.float32)
    for c in range(NCORES):
        b, r = c // 2, c % 2
        out[b, r * T:(r + 1) * T, :] = res.results[c]["outT"].T
    return out
```

```python
import numpy as np
from contextlib import ExitStack
import concourse.bass as bass
import concourse.mybir as mybir
from concourse.bass_utils import run_bass_kernel_spmd

F32 = mybir.dt.float32
BF16 = mybir.dt.bfloat16
AF = mybir.ActivationFunctionType
ALU = mybir.AluOpType
AX = mybir.AxisListType

D = 1024
NCORES = 8
RMS_EPS = 1e-6


class Res:
    __slots__ = ("name", "lw", "rd", "dsem", "dcnt")

    def __init__(self, name):
        self.name = name
        self.lw = None
        self.rd = {}
        self.dsem = None
        self.dcnt = 0


class Sched:
    ENGS = ("pe", "act", "dve", "pool", "sp")

    def __init__(self, nc, es):
        self.nc = nc
        self.es = es
        self.prog = {e: [] for e in self.ENGS}
        self.cnt = {e: 0 for e in self.ENGS}
        self.sems = {}
        for e in self.ENGS:
            self.sems[e] = es.enter_context(nc.semaphore("s_" + e))
        self.waited = {e: {} for e in self.ENGS}
        self.epoch = {e: 0 for e in self.ENGS}
        self.nsem = 0
        self.n_inst = 0
        self.n_wait = 0
        self.dcount = {}

    def new_sem(self, name):
        k = "d_" + name
        if k not in self.sems:
            self.nsem += 1
            self.sems[k] = self.es.enter_context(self.nc.semaphore(k))
            self.dcount[k] = 0
        return k

    EPOCH = 10 ** 9

    def _ekey(self, e):
        ep = self.epoch[e]
        return e if ep == 0 else "%s#%d" % (e, ep)

    def barrier(self):
        for e in self.ENGS:
            for f in self.ENGS:
                if f != e:
                    self._wait(e, self._ekey(f), self.cnt[f])
            for key, c in self.dcount.items():
                self._wait(e, key, c)
        for e in self.ENGS:
            if e != "pe":
                self._wait(e, self._ekey(e), self.cnt[e])

    def _wait(self, eng, key, val):
        if val <= 0:
            return
        if eng == "pe" and key.split("#")[0] == "pe":
            return
        w = self.waited[eng]
        if w.get(key, 0) >= val:
            return
        w[key] = val
        self.prog[eng].append(("w", key, val))
        self.n_wait += 1

    def _deps(self, eng, reads, writes):
        for r in reads:
            if r.lw is not None:
                self._wait(eng, r.lw[0], r.lw[1])
        for w in writes:
            if w.lw is not None:
                self._wait(eng, w.lw[0], w.lw[1])
            for k, v in w.rd.items():
                self._wait(eng, k, v)

    def op(self, eng, fn, reads=(), writes=()):
        self._deps(eng, reads, writes)
        if self.cnt[eng] >= self.EPOCH:
            self.epoch[eng] += 1
            self.cnt[eng] = 0
            nk = self._ekey(eng)
            self.sems[nk] = self.es.enter_context(self.nc.semaphore("s_" + nk.replace("#", "_")))
        key = self._ekey(eng)
        self.cnt[eng] += 1
        c = self.cnt[eng]
        self.prog[eng].append(("i", fn, key, 1))
        for r in reads:
            if r.rd.get(key, 0) < c:
                r.rd[key] = c
        for w in writes:
            w.lw = (key, c)
            w.rd = {}
        self.n_inst += 1

    def dma(self, q, fn, reads=(), writes=(), sem_res=None, inc=16):
        self._deps(q, reads, writes)
        sr = sem_res if sem_res is not None else (writes[0] if writes else reads[0])
        if sr.dsem is None:
            sr.dsem = self.new_sem(sr.name)
        key = sr.dsem
        self.dcount[key] += inc
        c = self.dcount[key]
        sr.dcnt = c
        self.prog[q].append(("i", fn, key, inc))
        for r in reads:
            if r.rd.get(key, 0) < c:
                r.rd[key] = c
        for w in writes:
            w.lw = (key, c)
            w.rd = {}
        self.n_inst += 1

    def wait_all(self, eng, ress):
        for r in ress:
            if r.lw is not None:
                self._wait(eng, r.lw[0], r.lw[1])
            for k, v in r.rd.items():
                self._wait(eng, k, v)

    def emit(self):
        nc = self.nc
        sems = self.sems
        prog = self.prog

        def run(engh, lst):
            for it in lst:
                if it[0] == "w":
                    engh.wait_ge(sems[it[1]], it[2])
                else:
                    it[1](engh).then_inc(sems[it[2]], it[3])

        with nc.Block() as block:
            @block.tensor
            def _(e):
                run(e, prog["pe"])

            @block.scalar
            def _(e):
                run(e, prog["act"])

            @block.vector
            def _(e):
                run(e, prog["dve"])

            @block.gpsimd
            def _(e):
                run(e, prog["pool"])

            @block.sync
            def _(e):
                run(e, prog["sp"])


class Tile:
    def __init__(self, t, name):
        self.t = t
        self.r = Res(name)

    def __getitem__(self, k):
        return self.t[k]


class KB:
    def __init__(self):
        self.nc = bass.Bass("TRN2", target_bir_lowering=False)
        self.es = ExitStack()
        self.S = Sched(self.nc, self.es)
        self.tes = ExitStack()
        self.pfx = ""
        self.in_names = []

    def new_section(self, pfx):
        self.S.barrier()
        self.tes.close()
        self.tes = ExitStack()
        self.pfx = pfx

    def din(self, name, shape, dt=F32):
        self.in_names.append(self.pfx + name)
        return self.nc.dram_tensor(self.pfx + name, list(shape), dt, kind="ExternalInput").ap()

    def dout(self, name, shape, dt=F32):
        return self.nc.dram_tensor(self.pfx + name, list(shape), dt, kind="ExternalOutput").ap()

    def dint(self, name, shape, dt=F32):
        return self.nc.dram_tensor(name, list(shape), dt).ap()

    def sb(self, name, shape, dt=F32):
        return Tile(self.tes.enter_context(self.nc.sbuf_tensor(self.pfx + name, list(shape), dt)), self.pfx + name)

    def ps(self, name, shape, dt=F32):
        return Tile(self.tes.enter_context(self.nc.psum_tensor(self.pfx + name, list(shape), dt)), self.pfx + name)

    def op(self, eng, fn, reads, writes):
        self.S.op(eng, fn, [x.r for x in reads], [x.r for x in writes])

    def load(self, out_ap, in_ap, wt, q="sp", reads=()):
        self.S.dma(q, lambda e: e.dma_start(out=out_ap, in_=in_ap), [x if isinstance(x, Res) else x.r for x in reads], [wt.r])

    def store(self, out_ap, in_ap, rt, ores, q="sp"):
        for kk, vv in ores.rd.items():
            self.S._wait(q, kk, vv)
        self.S.dma(q, lambda e: e.dma_start(out=out_ap, in_=in_ap), [rt.r], [], sem_res=ores)
        ores.lw = (ores.dsem, self.S.dcount[ores.dsem])

    def collective(self, kind, op, in_ap, out_ap, in_res, out_res, groups):
        import os
        if os.environ.get("MK_NOCC"):
            return
        self.S.dma("pool", lambda e: e.collective_compute(kind, op, replica_groups=groups, ins=[in_ap], outs=[out_ap]),
                   [in_res], [out_res], inc=1)

    def mm(self, out_ap, lhsT, rhs, start, stop, reads, wt):
        self.op("pe", lambda e: e.matmul(out_ap, lhsT, rhs, start=start, stop=stop), reads, [wt])

    def tr(self, out_ap, in_ap, ident_ap, reads, wt):
        self.op("pe", lambda e: e.transpose(out_ap, in_ap, ident_ap), reads, [wt])

    def finish(self, out_res_list):
        self.S.wait_all("sp", out_res_list)
        self.S.emit()
        self.tes.close()
        self.es.close()
        return self.nc


def build_ffn(T, final, SBT=1024, k=None, io=None):
    NB = T // 512
    NSB = max(1, T // SBT)
    BPS = NB // NSB
    standalone = k is None
    c8 = lambda ap, tok: ap[:, tok].rearrange("(c p) t -> p c t", p=128)
    if standalone:
        k = KB()
        hT = k.din("hT", [D, T])
        p0T = k.din("p0T", [D, T])
        p1T = k.din("p1T", [D, T])
        io = {"h": lambda tok: c8(hT, tok), "h_res": [], "pins": [lambda tok: c8(p0T, tok), lambda tok: c8(p1T, tok)], "pin_res": []}
    pT = k.din("pT", [256, T])
    gain_d = k.din("gain", [128, 8])
    gfin_d = k.din("gfin", [128, 8])
    wr_d = k.din("wr", [128, 8 * 20])
    br_d = k.din("br", [1, 20])
    wg_d = k.din("wg", [16, 128, 4096])
    wu_d = k.din("wu", [16, 128, 4096])
    wd_d = k.din("wd", [16, 128, 4096])
    plg_d = k.din("plg", [D, D])
    plp_d = k.din("plp", [256, D])
    ident_d = k.din("ident", [128, 128])
    sel_d = k.din("sel", [16, 16 * 128])
    if standalone:
        outT = k.dout("outT", [D, T])
        ores = Res("out")
        io["out"] = lambda tok: c8(outT, tok)
        io["out_res"] = ores
    ores = io["out_res"]

    acc = k.sb("acc", [128, BPS, 8, 512])
    hn16 = k.sb("hn16", [128, BPS, 8, 512], BF16)
    big32 = k.sb("big32", [128, 8, 512])
    combT = k.sb("combT", [16, BPS * 512])
    wbuf = [k.sb("wbuf%d" % i, [128, 12288], BF16) for i in range(2)]
    stage = [k.sb("stage%d" % i, [128, 2048]) for i in range(3)]
    p16 = k.sb("p16", [128, 2, 512], BF16)
    h2b = k.sb("h2b", [128, 8, 512], BF16)
    sg = [k.sb("sg%d" % i, [128, 512]) for i in range(2)]
    tt = [k.sb("tt%d" % i, [128, 512]) for i in range(2)]
    he = [k.sb("he%d" % i, [128, 4, 512], BF16) for i in range(2)]
    sq = [k.sb("sq%d" % i, [128, 512], BF16) for i in range(2)]
    rstd = k.sb("rstd", [128, 512])
    gain = k.sb("gain_s", [128, 8])
    gfin = k.sb("gfin_s", [128, 8])
    wr = k.sb("wr_s", [128, 8 * 20])
    br = k.sb("br_s", [128, 20])
    ident = k.sb("ident_s", [128, 128])
    sel = k.sb("sel_s", [16, 16 * 128])
    ones16 = k.sb("ones16", [128, 128], BF16)
    rt = {n: k.sb("rt_" + n, [128, w]) for n, w in
          [("L", 20), ("gmax", 1), ("ngmax", 1), ("gm", 4), ("ex", 4), ("se", 1), ("ptop", 1), ("t44", 16), ("esel", 4),
           ("m1", 1), ("k1", 4), ("e2", 4), ("m2", 1), ("k2", 4), ("d", 1), ("ed", 1), ("w1", 1), ("w2", 1), ("t1", 4),
           ("cl", 4), ("comb", 16)]}
    ps_gu = [k.ps("ps_gu%d" % i, [128, 512]) for i in range(4)]
    ps_y = [k.ps("ps_y%d" % i, [128, 512]) for i in range(2)]
    ps_c = k.ps("ps_c", [128, 512])
    ps_m = k.ps("ps_m", [128, 512])

    k.load(gain[:], gain_d[:, :], gain)
    k.load(gfin[:], gfin_d[:, :], gfin)
    k.load(wr[:], wr_d[:, :], wr)
    k.load(br[:], br_d.partition_broadcast(128), br)
    k.load(ident[:], ident_d[:, :], ident)
    k.load(sel[:], sel_d[:, :], sel)
    k.op("dve", lambda e: e.memset(ones16[:], 1.0), [], [ones16])
    epsb = k.sb("epsb", [128, 1])
    k.op("dve", lambda e: e.memset(epsb[:], float(D * RMS_EPS)), [], [epsb])
    k.op("dve", lambda e: e.tensor_scalar_mul(out=gain[:], in0=gain[:], scalar1=32.0), [gain], [gain])
    k.op("dve", lambda e: e.tensor_scalar_mul(out=gfin[:], in0=gfin[:], scalar1=32.0), [gfin], [gfin])

    stage_i = [0]

    def load_cast(dst_ap, src_ap, dst_tile, shape3=None):
        st = stage[stage_i[0] % 3]
        stage_i[0] += 1
        if shape3 is None:
            k.load(st[:], src_ap, st)
            k.op("act", lambda e: e.copy(out=dst_ap, in_=st[:]), [st], [dst_tile])
        else:
            a, b = shape3
            k.load(st[:].rearrange("p (a b) -> p a b", a=a), src_ap, st)
            k.op("act", lambda e: e.copy(out=dst_ap, in_=st[:]), [st], [dst_tile])

    def rmsnorm_stats(src_chunks, src_tile):
        for c in range(8):
            s = sq[c % 2]
            k.op("act", (lambda s, c: lambda e: e.activation(out=s[:], in_=src_chunks(c), func=AF.Square))(s, c),
                 [src_tile], [s])
            k.mm(ps_m[:], ones16[:], s[:], c == 0, c == 7, [ones16, s], ps_m)
        k.op("act", lambda e: e.activation(out=rstd[:], in_=ps_m[:], func=AF.Sqrt, bias=epsb[:, 0:1], scale=1.0),
             [ps_m, epsb], [rstd])
        k.op("dve", lambda e: e.reciprocal(out=rstd[:], in_=rstd[:]), [rstd], [rstd])

    for sb_i in range(NSB):
        for j in range(BPS):
            tok = slice((sb_i * BPS + j) * 512, (sb_i * BPS + j + 1) * 512)
            k.load(acc[:, j], io["h"](tok), acc, reads=io["h_res"])
            for src in io["pins"]:
                k.load(big32[:], src(tok), big32, reads=io["pin_res"])
                k.op("dve", (lambda j: lambda e: e.tensor_tensor(out=acc[:, j], in0=acc[:, j], in1=big32[:], op=ALU.add))(j),
                     [acc, big32], [acc])
            rmsnorm_stats(lambda c, j=j: acc[:, j, c], acc)
            for c in range(8):
                k.op("dve", (lambda j, c: lambda e: e.scalar_tensor_tensor(
                    out=big32[:, c], in0=acc[:, j, c], scalar=gain[:, c:c + 1], in1=rstd[:], op0=ALU.mult, op1=ALU.mult))(j, c),
                    [acc, gain, rstd], [big32])
            k.op("act", (lambda j: lambda e: e.copy(out=hn16[:, j], in_=big32[:]))(j), [big32], [hn16])
            for t4 in range(4):
                for c in range(8):
                    k.mm(ps_m[:, 0:20], big32[:, c, t4 * 128:(t4 + 1) * 128], wr[:, c * 20:(c + 1) * 20], c == 0, c == 7,
                         [big32, wr], ps_m)
                R = rt
                V = "dve"
                k.op(V, lambda e: e.tensor_tensor(out=R["L"][:], in0=ps_m[:, 0:20], in1=br[:], op=ALU.add), [ps_m, br], [R["L"]])
                k.op(V, lambda e: e.reduce_max(out=R["gmax"][:], in_=R["L"][:, 0:4], axis=AX.X), [R["L"]], [R["gmax"]])
                k.op(V, lambda e: e.tensor_tensor(out=R["gm"][:], in0=R["L"][:, 0:4], in1=R["gmax"][:, 0:1].to_broadcast([128, 4]),
                                                  op=ALU.is_ge), [R["L"], R["gmax"]], [R["gm"]])
                k.op(V, lambda e: e.tensor_scalar_mul(out=R["ngmax"][:], in0=R["gmax"][:], scalar1=-1.0), [R["gmax"]], [R["ngmax"]])
                k.op("act", lambda e: e.activation(out=R["ex"][:], in_=R["L"][:, 0:4], func=AF.Exp, bias=R["ngmax"][:, 0:1],
                                                   scale=1.0, accum_out=R["se"][:]), [R["L"], R["ngmax"]], [R["ex"], R["se"]])
                k.op(V, lambda e: e.reciprocal(out=R["ptop"][:], in_=R["se"][:]), [R["se"]], [R["ptop"]])
                k.op(V, lambda e: e.tensor_tensor(
                    out=R["t44"][:].rearrange("p (g x) -> p g x", g=4),
                    in0=R["L"][:, 4:20].rearrange("p (g x) -> p g x", g=4),
                    in1=R["gm"][:].unsqueeze(2).to_broadcast([128, 4, 4]), op=ALU.mult), [R["L"], R["gm"]], [R["t44"]])
                k.op(V, lambda e: e.tensor_reduce(out=R["esel"][:], in_=R["t44"][:].rearrange("p (g x) -> p x g", g=4),
                                                  axis=AX.X, op=ALU.add), [R["t44"]], [R["esel"]])
                k.op(V, lambda e: e.reduce_max(out=R["m1"][:], in_=R["esel"][:], axis=AX.X), [R["esel"]], [R["m1"]])
                k.op(V, lambda e: e.tensor_tensor(out=R["k1"][:], in0=R["esel"][:], in1=R["m1"][:, 0:1].to_broadcast([128, 4]),
                                                  op=ALU.is_ge), [R["esel"], R["m1"]], [R["k1"]])
                k.op(V, lambda e: e.scalar_tensor_tensor(out=R["e2"][:], in0=R["k1"][:], scalar=-1e30, in1=R["esel"][:],
                                                         op0=ALU.mult, op1=ALU.add), [R["k1"], R["esel"]], [R["e2"]])
                k.op(V, lambda e: e.reduce_max(out=R["m2"][:], in_=R["e2"][:], axis=AX.X), [R["e2"]], [R["m2"]])
                k.op(V, lambda e: e.tensor_tensor(out=R["k2"][:], in0=R["e2"][:], in1=R["m2"][:, 0:1].to_broadcast([128, 4]),
                                                  op=ALU.is_ge), [R["e2"], R["m2"]], [R["k2"]])
                k.op(V, lambda e: e.tensor_tensor(out=R["d"][:], in0=R["m2"][:], in1=R["m1"][:], op=ALU.subtract),
                     [R["m1"], R["m2"]], [R["d"]])
                k.op("act", lambda e: e.activation(out=R["ed"][:], in_=R["d"][:], func=AF.Exp), [R["d"]], [R["ed"]])
                k.op(V, lambda e: e.tensor_scalar_add(out=R["w1"][:], in0=R["ed"][:], scalar1=1.0), [R["ed"]], [R["w1"]])
                k.op(V, lambda e: e.reciprocal(out=R["w1"][:], in_=R["w1"][:]), [R["w1"]], [R["w1"]])
                k.op(V, lambda e: e.tensor_tensor(out=R["w1"][:], in0=R["w1"][:], in1=R["ptop"][:], op=ALU.mult),
                     [R["w1"], R["ptop"]], [R["w1"]])
                k.op(V, lambda e: e.tensor_tensor(out=R["w2"][:], in0=R["w1"][:], in1=R["ed"][:], op=ALU.mult),
                     [R["w1"], R["ed"]], [R["w2"]])
                k.op(V, lambda e: e.tensor_scalar(out=R["t1"][:], in0=R["k1"][:], scalar1=R["w1"][:, 0:1], scalar2=None,
                                                  op0=ALU.mult), [R["k1"], R["w1"]], [R["t1"]])
                k.op(V, lambda e: e.scalar_tensor_tensor(out=R["cl"][:], in0=R["k2"][:], scalar=R["w2"][:, 0:1], in1=R["t1"][:],
                                                         op0=ALU.mult, op1=ALU.add), [R["k2"], R["w2"], R["t1"]], [R["cl"]])
                k.op(V, lambda e: e.tensor_tensor(
                    out=R["comb"][:].rearrange("p (g x) -> p g x", g=4),
                    in0=R["gm"][:].unsqueeze(2).to_broadcast([128, 4, 4]),
                    in1=R["cl"][:].unsqueeze(1).to_broadcast([128, 4, 4]), op=ALU.mult), [R["gm"], R["cl"]], [R["comb"]])
                k.tr(ps_m[0:16, 128:256], R["comb"][:], ident[:], [R["comb"], ident], ps_m)
                k.op(V, (lambda j, t4: lambda e: e.tensor_copy(out=combT[:, j * 512 + t4 * 128: j * 512 + (t4 + 1) * 128],
                                                               in_=ps_m[0:16, 128:256]))(j, t4), [ps_m], [combT])

        def load_expert(e):
            wb = wbuf[e % 2]
            for mi, src in enumerate((wg_d, wu_d, wd_d)):
                for half in range(2):
                    load_cast(wb[:, mi * 4096 + half * 2048: mi * 4096 + (half + 1) * 2048], src[e, :, half * 2048:(half + 1) * 2048], wb)

        load_expert(0)
        gi = 0
        yi = 0
        pending = [None]

        def down_proj(j, hb, wb):
            nonlocal_yi = yi_box
            for c in range(8):
                py = ps_y[nonlocal_yi[0] % 2]
                nonlocal_yi[0] += 1
                for f in range(4):
                    k.mm(py[:], wb[:, 8192 + f * 1024 + c * 128: 8192 + f * 1024 + (c + 1) * 128], hb[:, f], f == 0, f == 3,
                         [wb, hb], py)
                k.op("dve", (lambda j, c, py: lambda e: e.tensor_tensor(out=acc[:, j, c], in0=acc[:, j, c], in1=py[:], op=ALU.add))(j, c, py),
                     [acc, py], [acc])

        yi_box = [0]
        for e in range(16):
            if pending[0] is not None:
                pending[0]()
                pending[0] = None
            if e + 1 < 16:
                load_expert(e + 1)
            wb = wbuf[e % 2]
            for j in range(BPS):
                k.mm(ps_c[:], sel[:, e * 128:(e + 1) * 128], combT[:, j * 512:(j + 1) * 512], True, True, [sel, combT], ps_c)
                hb = he[(e * BPS + j) % 2]
                for f in range(4):
                    pg = ps_gu[gi % 4]
                    pu = ps_gu[(gi + 1) % 4]
                    gi += 2
                    for c in range(8):
                        k.mm(pg[:], wb[:, c * 512 + f * 128: c * 512 + (f + 1) * 128], hn16[:, j, c], c == 0, c == 7, [wb, hn16], pg)
                    for c in range(8):
                        k.mm(pu[:], wb[:, 4096 + c * 512 + f * 128: 4096 + c * 512 + (f + 1) * 128], hn16[:, j, c], c == 0, c == 7,
                             [wb, hn16], pu)
                    s_ = sg[f % 2]
                    t_ = tt[f % 2]
                    k.op("act", (lambda s_, pg: lambda e: e.activation(out=s_[:], in_=pg[:], func=AF.Silu))(s_, pg), [pg], [s_])
                    k.op("dve", (lambda t_, s_, pu: lambda e: e.tensor_tensor(out=t_[:], in0=s_[:], in1=pu[:], op=ALU.mult))(t_, s_, pu),
                         [s_, pu], [t_])
                    k.op("dve", (lambda hb, f, t_: lambda e: e.tensor_tensor(out=hb[:, f], in0=t_[:], in1=ps_c[:], op=ALU.mult))(hb, f, t_),
                         [t_, ps_c], [hb])
                if pending[0] is not None:
                    pending[0]()
                pending[0] = (lambda j=j, hb=hb, wb=wb: down_proj(j, hb, wb))
        if pending[0] is not None:
            pending[0]()
            pending[0] = None

        wp = wbuf[0]
        for q4 in range(4):
            load_cast(wp[:, q4 * 2048:(q4 + 1) * 2048],
                      plg_d[q4 * 256:(q4 + 1) * 256, :].rearrange("(k p) n -> p k n", p=128), wp, (2, 1024))
        load_cast(wp[:, 8192:10240], plp_d[:, :].rearrange("(k p) n -> p k n", p=128), wp, (2, 1024))
        for j in range(BPS):
            tok = slice((sb_i * BPS + j) * 512, (sb_i * BPS + j + 1) * 512)
            st = stage[stage_i[0] % 3]
            stage_i[0] += 1
            k.load(st[:, 0:1024].rearrange("p (a b) -> p a b", a=2), pT[:, tok].rearrange("(c p) t -> p c t", p=128), st)
            k.op("act", (lambda st: lambda e: e.copy(out=p16[:], in_=st[:, 0:1024]))(st), [st], [p16])
            k.op("act", (lambda j: lambda e: e.copy(out=h2b[:], in_=acc[:, j]))(j), [acc], [h2b])
            for c in range(8):
                pg = ps_gu[gi % 4]
                pu = ps_gu[(gi + 1) % 4]
                gi += 2
                for kk in range(8):
                    k.mm(pg[:], wp[:, kk * 1024 + c * 128: kk * 1024 + (c + 1) * 128], h2b[:, kk], kk == 0, kk == 7, [wp, h2b], pg)
                for kk in range(2):
                    k.mm(pu[:], wp[:, 8192 + kk * 1024 + c * 128: 8192 + kk * 1024 + (c + 1) * 128], p16[:, kk], kk == 0, kk == 1,
                         [wp, p16], pu)
                s_ = sg[c % 2]
                t_ = tt[c % 2]
                k.op("act", (lambda s_, pg: lambda e: e.activation(out=s_[:], in_=pg[:], func=AF.Sigmoid))(s_, pg), [pg], [s_])
                k.op("dve", (lambda t_, s_, pu: lambda e: e.tensor_tensor(out=t_[:], in0=s_[:], in1=pu[:], op=ALU.mult))(t_, s_, pu),
                     [s_, pu], [t_])
                k.op("dve", (lambda j, c, t_: lambda e: e.tensor_tensor(out=acc[:, j, c], in0=acc[:, j, c], in1=t_[:], op=ALU.add))(j, c, t_),
                     [acc, t_], [acc])
            if final:
                rmsnorm_stats(lambda c, j=j: acc[:, j, c], acc)
                for c in range(8):
                    k.op("dve", (lambda j, c: lambda e: e.scalar_tensor_tensor(
                        out=big32[:, c], in0=acc[:, j, c], scalar=gfin[:, c:c + 1], in1=rstd[:], op0=ALU.mult, op1=ALU.mult))(j, c),
                        [acc, gfin, rstd], [big32])
                k.store(io["out"](tok), big32[:], big32, ores)
            else:
                k.store(io["out"](tok), acc[:, j], acc, ores)
    if not standalone:
        return None, k
    nc = k.finish([ores])
    return nc, k


def ffn_consts():
    ident = np.eye(128, dtype=np.float32)
    sel = np.zeros((16, 16 * 128), np.float32)
    for e in range(16):
        sel[e, e * 128:(e + 1) * 128] = 1.0
    return ident, sel


def tile_w(w):
    w = np.asarray(w, np.float32)
    E, K_, N_ = w.shape
    return np.ascontiguousarray(w.reshape(E, K_ // 128, 128, N_).transpose(0, 2, 1, 3).reshape(E, 128, (K_ // 128) * N_))


def chunk_cols(v):
    return np.ascontiguousarray(np.asarray(v, np.float32).reshape(8, 128).T)


class Sub:
    def __init__(self, ap, name, res=None):
        self.ap = ap
        self.r = res if res is not None else Res(name)

    def __getitem__(self, k):
        return self.ap[k]


def _v(k, eng, meth, reads, writes, **kw):
    k.S.op(eng, lambda e: getattr(e, meth)(**kw), [x.r for x in reads], [x.r for x in writes])


def build_mix(S, odd, lam_init=0.2, skip_attn=False, skip_rec=False, stop=99, k=None, io=None):
    NB = S // 512
    NKB = S // 128
    standalone = k is None
    if standalone:
        k = KB()
    V = lambda *a, **kw: _v(k, *a, **kw)
    NFG = 10 if odd else 12
    NTG = 4 if odd else 6
    NG = 6 if odd else 4
    NCV = 6 if odd else 4
    if standalone:
        hT = k.din("hT", [D, S])
        io = {"h": lambda blk: hT[:, blk * 512:(blk + 1) * 512].rearrange("(c p) t -> p c t", p=128), "h_res": []}
    gain_d = k.din("gain", [128, 8])
    wf_d = k.din("wf", [D, NFG * 128])
    wt_d = k.din("wt", [D, NTG * 128])
    wgt_d = k.din("wgt", [128, 8 * NG])
    gb_d = k.din("gbias", [1, NG])
    wout_d = k.din("wout", [512, D])
    convw_d = k.din("convw", [128, NCV * 4])
    nrm_d = k.din("nrm", [1, 128])
    ident_d = k.din("ident", [128, 128])
    U_d = k.din("U", [128, 128])
    BD_d = k.din("BD", [128, 128])
    MBu_d = k.din("MBu", [128, 128])
    MBl_d = k.din("MBl", [128, 128])
    mask_d = k.din("masks", [4, 128, 512])
    if odd:
        alog_d = k.din("alog", [1, 2])
        Uf_d = k.din("Uf", [128, 128])
    else:
        lamv_d = k.din("lamv", [1, 256])
        subln_d = k.din("subln", [128, 1])
        cos_d = k.din("cosT", [128, S])
        sin_d = k.din("sinT", [128, S])
    if standalone:
        partT = k.dout("partT", [D, S])
        io["out"] = lambda blk: partT[:, blk * 512:(blk + 1) * 512].rearrange("(c p) t -> p c t", p=128)
        io["out_res"] = Res("out")
    ores = io["out_res"]

    x32 = k.sb("x32", [128, 8, 512])
    hn16 = k.sb("hn16", [128, 8, 512], BF16)
    wf16 = k.sb("wf16", [128, 8, NFG * 128], BF16)
    wt16 = k.sb("wt16", [128, 8, NTG * 128], BF16)
    wout16 = k.sb("wout16", [128, 4, D], BF16)
    wg32 = k.sb("wg32", [128, 8 * NG])
    gbias = k.sb("gbias_s", [128, NG])
    gain = k.sb("gain_s", [128, 8])
    convw = k.sb("convw_s", [128, NCV * 4])
    nrmrep = k.sb("nrmrep", [128, 128])
    ident = k.sb("ident_s", [128, 128])
    Um = k.sb("U_s", [128, 128])
    BDm = k.sb("BD_s", [128, 128])
    MBu = k.sb("MBu_s", [128, 128])
    MBl = k.sb("MBl_s", [128, 128])
    masks = k.sb("masks_s", [128, 4, 512], BF16)
    ones16 = k.sb("ones16", [128, 128], BF16)
    ones32 = k.sb("ones32", [128, 128])
    epsb = k.sb("epsb", [128, 1])
    eps1 = k.sb("eps1", [128, 1])
    onec = k.sb("onec", [128, 1])
    sq = [k.sb("sq%d" % i, [128, 512], BF16) for i in range(2)]
    rstd = k.sb("rstd", [128, 512])
    Xt = rstd
    kcache = [k.sb("kc%d" % i, [128, S], BF16) for i in range(2)]
    vcache = [k.sb("vc%d" % i, [128, NKB, 128], BF16) for i in range(2)]
    qa = [k.sb("qa%d" % i, [128, 512], BF16) for i in range(2)]
    oT = k.sb("oT", [128, 4, 512], BF16)
    NET = 2 if odd else 3
    Et = [k.sb("E%d" % i, [128, 512], BF16) for i in range(NET)]
    Rt = [k.sb("R%d" % i, [128, 512]) for i in range(1 if odd else 2)]
    rz = k.sb("rz", [128, 512])
    cvin = [k.sb("cvin%d" % i, [128, 515]) for i in range(NCV)]
    cvo = [k.sb("cvo%d" % i, [128, 512]) for i in range(NCV)]
    vaug = [k.sb("vaug%d" % i, [128, 4, 130] if not odd else [128, 2]) for i in range(2)]
    gtok = [k.sb("gtok%d" % i, [128, 4, 128]) for i in range(2)]
    gts = k.sb("gts", [128, 4, NG])
    lf = k.sb("lf", [128, 4, NG])
    gt2 = k.sb("gt2", [128, 4, NG])
    Sst = [[k.sb("S%d_%d" % (i, j), [128, 130]) for j in range(2)] for i in range(2)]
    qhat = [k.sb("qhat%d" % i, [128, 2, 128]) for i in range(2)]
    smh = [{n: k.sb("sm%d_" % hh_ + n, [128, w]) for n, w in
          [("lfb", 128), ("crep", 128), ("bc", 8), ("rcol", 1), ("e1", 1), ("Dm", 128), ("G", 128), ("ecr", 128),
           ("k2", 128), ("dm", 1), ("hh", 128), ("junk", 128), ("ssq", 1), ("rs", 1)] +
          ([("Dl", 128), ("Gl", 128), ("N", 128), ("M", 128), ("P", 128), ("Mk", 128), ("Y", 128), ("vb", 128), ("kp", 128),
            ("u", 128), ("wT0", 128), ("wT1", 128), ("vn", 128), ("ktok", 128), ("bcol", 1), ("bebc", 1), ("ebd", 1), ("kd", 128),
            ("tmpc", 1)] if odd else [])} for hh_ in range(2)]
    for d_ in smh:
        d_["hn"] = d_["junk"]
        d_["ob"] = d_["hh"]
        d_["sgo"] = d_["G"]
        d_["AT"] = d_["Dm"]
    sm = smh[0]
    if odd:
        alog = k.sb("alog_s", [128, 2])
        Ufm = k.sb("Uf_s", [128, 128])
        carry = k.sb("carry", [128, 2])
        ncum = [k.sb("ncum%d" % i, [128, NKB]) for i in range(2)]
        Rq2 = k.sb("Rq2", [33, 512])
        Rq = [Sub(Rq2.t[32 * i_:32 * i_ + 1, :], "Rq%d" % i_, Rq2.r) for i_ in range(2)]
    else:
        lamv = k.sb("lamv_s", [128, 256])
        lamt = k.sb("lamt", [128, 8])
        subc = k.sb("subc", [128, 1])
        cost = k.sb("cost", [128, 512])
        sint = k.sb("sint", [128, 512])
        rt1 = Rt[0]
        rt2 = Rt[1]
    pj = [k.ps("pj%d" % i, [128, 512]) for i in range(2)]
    pst = [k.ps("pst%d" % i, [128, 512]) for i in range(2)]
    po = k.ps("po", [128, 512])
    pz = k.ps("pz", [128, 512])
    pr0 = k.ps("pr0", [128, 512])
    pr1 = k.ps("pr1", [128, 512])
    prb = [pr0, pr1]
    slots = [dict(A=Sub(prb[i_].t[:, 0:128], "pA%d" % i_, prb[i_].r), F=Sub(prb[i_].t[:, 128:258], "pF%d" % i_, prb[i_].r),
                  D=Sub(prb[i_].t[:, 384:512], "pD%d" % i_, prb[i_].r), E=Sub(pj[i_].t[:, 0:130], "pE%d" % i_, pj[i_].r),
                  C=Sub(pj[i_].t[:, 256:384], "pC%d" % i_, pj[i_].r), G=Sub(pj[i_].t[:, 384:512], "pG%d" % i_, pj[i_].r))
             for i_ in range(2)]
    pA = slots[0]["A"]
    pH = Sub(pst[0].t[:, 0:128], "pH", pst[0].r)

    for t_, d_ in ((gain, gain_d), (wg32, wgt_d), (convw, convw_d), (ident, ident_d), (Um, U_d), (BDm, BD_d), (MBu, MBu_d), (MBl, MBl_d)):
        k.load(t_[:], d_[:, :], t_)
    k.load(gbias[:], gb_d.partition_broadcast(128), gbias)
    k.load(nrmrep[:], nrm_d.partition_broadcast(128), nrmrep)
    V("dve", "memset", [], [ones16], ap=ones16[:], constant=1.0)
    V("dve", "memset", [], [ones32], ap=ones32[:], constant=1.0)
    V("dve", "memset", [], [epsb], ap=epsb[:], constant=float(D * RMS_EPS))
    V("dve", "memset", [], [eps1], ap=eps1[:], constant=float(RMS_EPS))
    V("dve", "memset", [], [onec], ap=onec[:], constant=1.0)
    V("dve", "tensor_scalar_mul", [gain], [gain], out=gain[:], in0=gain[:], scalar1=32.0)
    for i in range(2):
        V("dve", "memset", [], [qhat[i]], ap=qhat[i][:], constant=0.0)
        V("dve", "memset", [], [vaug[i]], ap=vaug[i][:], constant=1.0)
        for j in range(2):
            V("dve", "memset", [], [Sst[i][j]], ap=Sst[i][j][:], constant=0.0)
    for c_ in cvin:
        V("dve", "memset", [], [c_], ap=c_[:], constant=0.0)
    V("dve", "memset", [], [lf], ap=lf[:], constant=0.0)
    if odd:
        k.load(alog[:], alog_d.partition_broadcast(128), alog)
        k.load(Ufm[:], Uf_d[:, :], Ufm)
        V("act", "activation", [alog], [alog], out=alog[:], in_=alog[:], func=AF.Exp)
        V("dve", "tensor_scalar_mul", [alog], [alog], out=alog[:], in0=alog[:], scalar1=-1.0)
        V("dve", "memset", [], [carry], ap=carry[:], constant=0.0)
    else:
        k.load(lamv[:], lamv_d.partition_broadcast(128), lamv)
        k.load(subc[:], subln_d[:, :], subc)
        V("dve", "tensor_scalar_mul", [subc], [subc], out=subc[:], in0=subc[:], scalar1=float(1.0 - lam_init))
        V("dve", "tensor_tensor", [lamv], [lamv], out=lamv[:, 0:64], in0=lamv[:, 0:64], in1=lamv[:, 64:128], op=ALU.mult)
        V("dve", "tensor_tensor", [lamv], [lamv], out=lamv[:, 128:192], in0=lamv[:, 128:192], in1=lamv[:, 192:256], op=ALU.mult)
        V("dve", "reduce_sum", [lamv], [lamt], out=lamt[:, 0:1], in_=lamv[:, 0:64], axis=AX.X)
        V("dve", "reduce_sum", [lamv], [lamt], out=lamt[:, 1:2], in_=lamv[:, 128:192], axis=AX.X)
        V("act", "activation", [lamt], [lamt], out=lamt[:, 2:4], in_=lamt[:, 0:2], func=AF.Exp)
        V("dve", "tensor_tensor", [lamt], [lamt], out=lamt[:, 4:5], in0=lamt[:, 3:4], in1=lamt[:, 2:3], op=ALU.subtract)
        V("dve", "tensor_scalar_add", [lamt], [lamt], out=lamt[:, 4:5], in0=lamt[:, 4:5], scalar1=float(-lam_init))
    k.load(x32[:, 0:4, :], mask_d.rearrange("j p t -> p j t"), x32)
    if odd:
        V("dve", "tensor_scalar", [x32], [masks], out=masks[:], in0=x32[:, 0:4, :], scalar1=-1.0, scalar2=30000.0, op0=ALU.add, op1=ALU.mult)
    else:
        V("act", "copy", [x32], [masks], out=masks[:], in_=x32[:, 0:4, :])

    def load_w(dst, dcols, src, rows0, nrows, ncols, col0):
        nk = nrows // 128
        st = x32[:].rearrange("p a b -> p (a b)")[:, 0:nk * ncols].rearrange("p (a b) -> p a b", a=nk)
        k.load(st, src[rows0:rows0 + nrows, col0:col0 + ncols].rearrange("(a p) n -> p a n", p=128), x32)
        V("act", "copy", [x32], [dst], out=dcols, in_=st)

    for g in range(NFG):
        for h2 in range(2):
            load_w(wf16, wf16[:, h2 * 4:(h2 + 1) * 4, g * 128:(g + 1) * 128], wf_d, h2 * 512, 512, 128, g * 128)
    for g in range(NTG):
        for h2 in range(2):
            load_w(wt16, wt16[:, h2 * 4:(h2 + 1) * 4, g * 128:(g + 1) * 128], wt_d, h2 * 512, 512, 128, g * 128)
    for hh_ in range(4):
        load_w(wout16, wout16[:, hh_:hh_ + 1, :], wout_d, hh_ * 128, 128, D, 0)

    pji = [0]

    def proj_fm(g):
        p = pj[pji[0] % 2]
        pji[0] += 1
        for c in range(8):
            k.mm(p[:], wf16[:, c, g * 128:(g + 1) * 128], hn16[:, c], c == 0, c == 7, [wf16, hn16], p)
        return p

    def proj_tm(g):
        p = pj[pji[0] % 2]
        pji[0] += 1
        for t4 in range(4):
            for c in range(8):
                k.mm(p[:, t4 * 128:(t4 + 1) * 128], hn16[:, c, t4 * 128:(t4 + 1) * 128], wt16[:, c, g * 128:(g + 1) * 128],
                     c == 0, c == 7, [wt16, hn16], p)
        return p

    def conv_silu(p, ci, blk):
        xi = cvin[ci]
        V("act", "copy", [p], [xi], out=xi[:, 3:515], in_=p[:])
        o = cvo[ci]
        V("dve", "tensor_scalar_mul", [xi, convw], [o], out=o[:], in0=xi[:, 0:512], scalar1=convw[:, ci * 4:ci * 4 + 1])
        for j in range(1, 4):
            V("dve", "scalar_tensor_tensor", [xi, convw, o], [o], out=o[:], in0=xi[:, j:j + 512],
              scalar=convw[:, ci * 4 + j:ci * 4 + j + 1], in1=o[:], op0=ALU.mult, op1=ALU.add)
        V("act", "activation", [o], [o], out=o[:], in_=o[:], func=AF.Silu)
        V("dve", "tensor_copy", [xi], [xi], out=xi[:, 0:3], in_=xi[:, 512:515])
        return o

    def l2n(o, scale):
        V("act", "activation", [o], [sq[0]], out=sq[0][:], in_=o[:], func=AF.Square)
        k.mm(pz[:], ones16[:], sq[0][:], True, True, [ones16, sq[0]], pz)
        V("act", "activation", [pz, eps1], [rz], out=rz[:], in_=pz[:], func=AF.Sqrt, bias=eps1[:, 0:1], scale=1.0)
        V("dve", "reciprocal", [rz], [rz], out=rz[:], in_=rz[:])
        V("dve", "scalar_tensor_tensor", [o, rz], [o], out=o[:], in0=o[:], scalar=float(scale), in1=rz[:], op0=ALU.mult, op1=ALU.mult)

    sti = [0]
    ei = [0]

    def attn(i, blk, qparts, scale, bias_rows=None):
        outs = []
        nkb = 4 * blk + 4
        for ci, psl in enumerate(qparts):
            for kb in range(nkb):
                st = pst[sti[0] % 2]
                sti[0] += 1
                k.mm(st[:], kcache[i][psl, kb * 128:(kb + 1) * 128], qa[i][psl, :], True, bias_rows is None, [kcache[i], qa[i]], st)
                E = Et[ei[0] % NET]
                ei[0] += 1
                if bias_rows is not None:
                    nc_, rq_ = bias_rows
                    k.mm(st[:], ones32[32 * i:32 * i + 1, 0:128], rq_[:, :], False, True, [ones32, rq_], st)
                    if kb >= 4 * blk:
                        V("dve", "tensor_tensor", [st, masks], [rz], out=rz[:], in0=st[:], in1=masks[:, kb - 4 * blk, :], op=ALU.add)
                        V("act", "activation", [rz, nc_], [E], out=E[:], in_=rz[:], func=AF.Exp, bias=nc_[:, kb:kb + 1], scale=float(scale))
                    else:
                        V("act", "activation", [st, nc_], [E], out=E[:], in_=st[:], func=AF.Exp, bias=nc_[:, kb:kb + 1], scale=float(scale))
                else:
                    V("act", "activation", [st], [E], out=E[:], in_=st[:], func=AF.Exp, scale=float(scale))
                    if kb >= 4 * blk:
                        V("dve", "tensor_tensor", [E, masks], [E], out=E[:], in0=E[:], in1=masks[:, kb - 4 * blk, :], op=ALU.mult)
                k.mm(po[:], vcache[i][:, kb, :], E[:], kb == 0, kb == nkb - 1, [vcache[i], E], po)
                k.mm(pz[:], ones16[:], E[:], kb == 0, kb == nkb - 1, [ones16, E], pz)
            R = Rt[ci]
            V("dve", "reciprocal", [pz], [rz], out=rz[:], in_=pz[:])
            V("dve", "tensor_tensor", [po, rz], [R], out=R[:], in0=po[:], in1=rz[:], op=ALU.mult)
            outs.append(R)
        return outs

    def decay_prep(sm, pA, lfcol, lf2, i):
        V("dve", "tensor_scalar_mul", [ones32, lf, gts], [sm["lfb"]], out=sm["lfb"][:], in0=ones32[:], scalar1=lfcol)
        k.mm(pA[:], sm["lfb"][:], Um[:], True, True, [sm["lfb"], Um], pA)
        V("act", "copy", [pA], [sm["crep"]], out=sm["crep"][:], in_=pA[:])
        V("dve", "tensor_tensor", [sm["crep"], ident], [sm["junk"]], out=sm["junk"][:], in0=sm["crep"][:], in1=ident[:], op=ALU.mult)
        V("dve", "reduce_sum", [sm["junk"]], [sm["bc"]], out=sm["bc"][:, 0:1], in_=sm["junk"][:], axis=AX.X)
        V("dve", "tensor_copy", [sm["crep"]], [sm["bc"]], out=sm["bc"][0:64, 1:2], in_=sm["crep"][0:64, 63:64])
        V("dve", "tensor_copy", [sm["crep"]], [sm["bc"]], out=sm["bc"][64:128, 1:2], in_=sm["crep"][64:128, 127:128])
        V("act", "activation", [sm["crep"]], [sm["ecr"]], out=sm["ecr"][:], in_=sm["crep"][:], func=AF.Exp)

    def post_out(sm, pG, i, t4, src_ps, gate_func):
        V("act", "activation", [sm["hh"]], [sm["junk"], sm["ssq"]], out=sm["junk"][:], in_=sm["hh"][:], func=AF.Square,
          accum_out=sm["ssq"][:])
        V("act", "activation", [sm["ssq"], eps1], [sm["rs"]], out=sm["rs"][:], in_=sm["ssq"][:], func=AF.Sqrt, bias=eps1[:, 0:1],
          scale=1.0 / 128.0)
        V("dve", "reciprocal", [sm["rs"]], [sm["rs"]], out=sm["rs"][:], in_=sm["rs"][:])
        V("dve", "scalar_tensor_tensor", [sm["hh"], sm["rs"], nrmrep], [sm["hn"]], out=sm["hn"][:], in0=sm["hh"][:],
          scalar=sm["rs"][:, 0:1], in1=nrmrep[:], op0=ALU.mult, op1=ALU.mult)
        V("act", "activation", [gtok[i]], [sm["sgo"]], out=sm["sgo"][:], in_=gtok[i][:, t4, :], func=gate_func)
        V("dve", "tensor_tensor", [sm["hn"], sm["sgo"]], [sm["ob"]], out=sm["ob"][:], in0=sm["hn"][:], in1=sm["sgo"][:], op=ALU.mult)
        k.tr(pG[:], sm["ob"][:], ident[:], [sm["ob"], ident], pG)
        V("act", "copy", [pG], [oT], out=oT[:, 2 + i, t4 * 128:(t4 + 1) * 128], in_=pG[:])

    def record(fn):
        lst = []
        o_op, o_dma = k.S.op, k.S.dma
        k.S.op = lambda *a_, **kw_: lst.append((o_op, a_, kw_))
        k.S.dma = lambda *a_, **kw_: lst.append((o_dma, a_, kw_))
        try:
            fn()
        finally:
            k.S.op, k.S.dma = o_op, o_dma
        return lst

    def interleave(lists):
        lists = [l_ for l_ in lists if l_]
        if not lists:
            return
        import os
        if os.environ.get("MK_SEQ"):
            for l_ in lists:
                for f_, a_, kw_ in l_:
                    f_(*a_, **kw_)
            return
        n = max(len(l_) for l_ in lists)
        pos = [0] * len(lists)
        for tick in range(1, n + 1):
            for li, l_ in enumerate(lists):
                tgt = (tick * len(l_) + n - 1) // n
                while pos[li] < min(tgt, len(l_)):
                    f_, a_, kw_ = l_[pos[li]]
                    f_(*a_, **kw_)
                    pos[li] += 1

    V("dve", "memset", [], [oT], ap=oT[:], constant=0.0)
    for blk in range(NB):
        tok = slice(blk * 512, (blk + 1) * 512)
        if stop == 0:
            k.store(io["out"](blk), x32[:], x32, ores)
            continue
        k.load(x32[:], io["h"](blk), x32, reads=io["h_res"])
        for c in range(8):
            s_ = sq[c % 2]
            V("act", "activation", [x32], [s_], out=s_[:], in_=x32[:, c], func=AF.Square)
            k.mm(pz[:], ones16[:], s_[:], c == 0, c == 7, [ones16, s_], pz)
        V("act", "activation", [pz, epsb], [rstd], out=rstd[:], in_=pz[:], func=AF.Sqrt, bias=epsb[:, 0:1], scale=1.0)
        V("dve", "reciprocal", [rstd], [rstd], out=rstd[:], in_=rstd[:])
        for c in range(8):
            V("dve", "scalar_tensor_tensor", [x32, gain, rstd], [x32], out=x32[:, c], in0=x32[:, c], scalar=gain[:, c:c + 1],
              in1=rstd[:], op0=ALU.mult, op1=ALU.mult)
        V("act", "copy", [x32], [hn16], out=hn16[:], in_=x32[:])
        if stop == 1:
            k.store(io["out"](blk), x32[:], x32, ores)
            continue
        for t4 in range(4):
            for c in range(8):
                k.mm(pH[:, t4 * NG:(t4 + 1) * NG], x32[:, c, t4 * 128:(t4 + 1) * 128], wg32[:, c * NG:(c + 1) * NG], c == 0, c == 7,
                     [x32, wg32], pH)
        V("dve", "tensor_tensor", [pH, gbias], [gts], out=gts[:], in0=pH[:, 0:4 * NG].rearrange("p (a b) -> p a b", a=4),
          in1=gbias[:].unsqueeze(1).to_broadcast([128, 4, NG]), op=ALU.add)

        if stop == 2:
            k.store(io["out"](blk), x32[:], x32, ores)
            continue
        if not odd:
            k.load(cost[:], cos_d[:, tok], cost)
            k.load(sint[:], sin_d[:, tok], sint)
            for i in range(2):
                for which, dst in ((0, qa[i]), (2, None)):
                    p = proj_fm(i * 4 + which)
                    V("dve", "tensor_tensor", [p, cost], [rt1], out=rt1[:], in0=p[:], in1=cost[:], op=ALU.mult)
                    p2 = proj_fm(i * 4 + which + 1)
                    V("dve", "tensor_tensor", [p2, sint], [rt2], out=rt2[:], in0=p2[:], in1=sint[:], op=ALU.mult)
                    if dst is not None:
                        V("dve", "tensor_tensor", [rt1, rt2], [dst], out=dst[:], in0=rt1[:], in1=rt2[:], op=ALU.add)
                    else:
                        V("dve", "tensor_tensor", [rt1, rt2], [kcache[i]], out=kcache[i][:, tok], in0=rt1[:], in1=rt2[:], op=ALU.add)
                p = proj_tm(i)
                V("act", "copy", [p], [vcache[i]], out=vcache[i][:, blk * 4:(blk + 1) * 4, :], in_=p[:].rearrange("p (a b) -> p a b", a=4))
            V("act", "activation", [gts], [gt2], out=gt2[:, :, 2:4], in_=gts[:, :, 2:4], func=AF.Exp, scale=-1.0)
            V("act", "activation", [gt2, onec], [gt2], out=gt2[:, :, 2:4], in_=gt2[:, :, 2:4], func=AF.Ln, bias=onec[:, 0:1], scale=1.0)
            V("dve", "tensor_scalar_mul", [gt2], [lf], out=lf[:, :, 2:4], in0=gt2[:, :, 2:4], scalar1=-1.0)
            def att_task():
                for i in range(0 if skip_attn else 2):
                    R = attn(i, blk, [slice(0, 64), slice(64, 128)], 64 ** -0.5)
                    V("dve", "scalar_tensor_tensor", [R[0], R[1], lamt], [Xt], out=Xt[:], in0=R[1][:], scalar=lamt[:, 4:5], in1=R[0][:],
                      op0=ALU.mult, op1=ALU.add)
                    V("act", "activation", [Xt], [sq[0]], out=sq[0][:], in_=Xt[:], func=AF.Square)
                    k.mm(pz[:], ones16[:], sq[0][:], True, True, [ones16, sq[0]], pz)
                    V("act", "activation", [pz, eps1], [rz], out=rz[:], in_=pz[:], func=AF.Sqrt, bias=eps1[:, 0:1], scale=1.0 / 128.0)
                    V("dve", "reciprocal", [rz], [rz], out=rz[:], in_=rz[:])
                    V("dve", "scalar_tensor_tensor", [Xt, subc, rz], [oT], out=oT[:, i, :], in0=Xt[:], scalar=subc[:, 0:1], in1=rz[:],
                      op0=ALU.mult, op1=ALU.mult)

            preps = {}
            for i in range(0 if skip_rec else 2):
                qc = conv_silu(proj_fm(8 + 2 * i), 2 * i, blk)
                kc = conv_silu(proj_fm(8 + 2 * i + 1), 2 * i + 1, blk)
                p = proj_tm(2 + 2 * i)
                V("act", "copy", [p], [vaug[i]], out=vaug[i][:, :, 0:128], in_=p[:].rearrange("p (a b) -> p a b", a=4))
                p = proj_tm(2 + 2 * i + 1)
                V("act", "copy", [p], [gtok[i]], out=gtok[i][:], in_=p[:].rearrange("p (a b) -> p a b", a=4))
                preps[i] = (qc, kc)

            def rec_task(i):
                qc, kc = preps[i]
                sm = smh[i]
                sl_ = slots[i]
                pA, pF, pD, pE, pC, pG = sl_["A"], sl_["F"], sl_["D"], sl_["E"], sl_["C"], sl_["G"]
                for t4 in range(4 if stop > 10 else 0):
                    cs = slice(t4 * 128, (t4 + 1) * 128)
                    decay_prep(sm, pA, lf[:, t4, 2 + i:3 + i], lf[:, t4, 0:4], 2 + i)
                    if stop == 105:
                        continue
                    V("dve", "tensor_tensor", [sm["bc"], gts], [sm["rcol"]], out=sm["rcol"][:], in0=sm["bc"][:, 0:1], in1=gts[:, t4, i:i + 1],
                      op=ALU.subtract)
                    V("dve", "tensor_tensor", [sm["bc"], sm["rcol"]], [sm["e1"]], out=sm["e1"][:], in0=sm["bc"][:, 1:2], in1=sm["rcol"][:],
                      op=ALU.subtract)
                    V("act", "activation", [sm["e1"]], [sm["e1"]], out=sm["e1"][:], in_=sm["e1"][:], func=AF.Exp)
                    V("dve", "tensor_scalar_mul", [sm["e1"]], [sm["e1"]], out=sm["e1"][:], in0=sm["e1"][:], scalar1=float(128 ** -0.5))
                    if stop == 11:
                        continue
                    V("dve", "scalar_tensor_tensor", [sm["crep"], sm["rcol"], MBu], [sm["Dm"]], out=sm["Dm"][:], in0=sm["crep"][:],
                      scalar=sm["rcol"][:, 0:1], in1=MBu[:], op0=ALU.subtract, op1=ALU.add)
                    V("act", "activation", [sm["Dm"]], [sm["G"]], out=sm["G"][:], in_=sm["Dm"][:], func=AF.Exp)
                    V("dve", "tensor_tensor", [qc, sm["ecr"]], [qhat[i]], out=qhat[i][:, 0, 0:64], in0=qc[:, t4 * 128:t4 * 128 + 64],
                      in1=sm["ecr"][:, 0:64], op=ALU.mult)
                    V("dve", "tensor_tensor", [qc, sm["ecr"]], [qhat[i]], out=qhat[i][:, 1, 64:128], in0=qc[:, t4 * 128 + 64:(t4 + 1) * 128],
                      in1=sm["ecr"][:, 64:128], op=ALU.mult)
                    k.mm(pC[:], kc[:, cs], qc[:, cs], True, True, [kc, qc], pC)
                    V("dve", "scalar_tensor_tensor", [pC, sm["G"]], [sm["AT"]], out=sm["AT"][:], in0=pC[:], scalar=float(128 ** -0.5),
                      in1=sm["G"][:], op0=ALU.mult, op1=ALU.mult)
                    if stop == 12:
                        continue
                    k.tr(pD[:], kc[:, cs], ident[:], [kc, ident], pD)
                    V("dve", "tensor_scalar_mul", [pD, sm["e1"]], [sm["k2"]], out=sm["k2"][:], in0=pD[:], scalar1=sm["e1"][:, 0:1])
                    if stop == 13:
                        continue
                    S0, S1 = Sst[i]
                    k.mm(pE[:], sm["AT"][:], vaug[i][:, t4, :], True, False, [sm["AT"], vaug[i]], pE)
                    k.mm(pE[:], qhat[i][:, 0, :], S0[:], False, False, [qhat[i], S0], pE)
                    k.mm(pF[:], sm["k2"][0:64, :], vaug[i][0:64, t4, :], True, True, [sm["k2"], vaug[i]], pF)
                    V("dve", "scalar_tensor_tensor", [S0, sm["ecr"], pF], [S1], out=S1[:], in0=S0[:], scalar=sm["ecr"][:, 63:64], in1=pF[:],
                      op0=ALU.mult, op1=ALU.add)
                    k.mm(pE[:], qhat[i][:, 1, :], S1[:], False, True, [qhat[i], S1], pE)
                    k.mm(pF[:], sm["k2"][64:128, :], vaug[i][64:128, t4, :], True, True, [sm["k2"], vaug[i]], pF)
                    V("dve", "scalar_tensor_tensor", [S1, sm["ecr"], pF], [S0], out=S0[:], in0=S1[:], scalar=sm["ecr"][:, 127:128], in1=pF[:],
                      op0=ALU.mult, op1=ALU.add)
                    if stop == 14:
                        continue
                    V("act", "activation", [pE], [sm["dm"]], out=sm["dm"][:], in_=pE[:, 128:129], func=AF.Abs)
                    V("dve", "tensor_scalar_max", [sm["dm"]], [sm["dm"]], out=sm["dm"][:], in0=sm["dm"][:], scalar1=1.0)
                    V("dve", "reciprocal", [sm["dm"]], [sm["dm"]], out=sm["dm"][:], in_=sm["dm"][:])
                    V("dve", "tensor_scalar_mul", [pE, sm["dm"]], [sm["hh"]], out=sm["hh"][:], in0=pE[:, 0:128], scalar1=sm["dm"][:, 0:1])
                    if stop == 15:
                        continue
                    post_out(sm, pG, i, t4, None, AF.Sigmoid)

            tl = [record(att_task)] + [record(lambda i=i: rec_task(i)) for i in range(0 if skip_rec else 2)]
            interleave(tl)
        else:
            V("act", "activation", [gts], [gt2], out=gt2[:, :, 0:2], in_=gts[:, :, 0:2], func=AF.Exp)
            V("act", "activation", [gts], [gt2], out=gt2[:, :, 4:6], in_=gts[:, :, 4:6], func=AF.Exp, scale=-1.0)
            V("act", "activation", [gt2, onec], [gt2], out=gt2[:, :, 0:2], in_=gt2[:, :, 0:2], func=AF.Ln, bias=onec[:, 0:1], scale=1.0)
            V("act", "activation", [gt2, onec], [gt2], out=gt2[:, :, 4:6], in_=gt2[:, :, 4:6], func=AF.Ln, bias=onec[:, 0:1], scale=1.0)
            V("dve", "tensor_tensor", [gt2, alog], [lf], out=lf[:, :, 0:2], in0=gt2[:, :, 0:2],
              in1=alog[:].unsqueeze(1).to_broadcast([128, 4, 2]), op=ALU.mult)
            V("dve", "tensor_scalar_mul", [gt2], [lf], out=lf[:, :, 4:6], in0=gt2[:, :, 4:6], scalar1=-1.0)
            V("act", "activation", [gts], [lf], out=lf[:, :, 2:4], in_=gts[:, :, 2:4], func=AF.Sigmoid)
            for t4 in range(4):
                for i in range(2):
                    V("dve", "tensor_scalar_mul", [ones32, lf], [sm["lfb"]], out=sm["lfb"][:], in0=ones32[:], scalar1=lf[:, t4, 4 + i:5 + i])
                    k.mm(pA[:], sm["lfb"][:], Ufm[:], True, True, [sm["lfb"], Ufm], pA)
                    V("dve", "tensor_scalar", [pA, carry], [Rq[i]], out=Rq[i][:, t4 * 128:(t4 + 1) * 128], in0=pA[32 * i:32 * i + 1, :],
                      scalar1=carry[32 * i:32 * i + 1, i:i + 1], scalar2=None, op0=ALU.add)
                    V("dve", "tensor_tensor", [pA, ident], [sm["junk"]], out=sm["junk"][:], in0=pA[:], in1=ident[:], op=ALU.mult)
                    V("dve", "reduce_sum", [sm["junk"]], [sm["tmpc"]], out=sm["tmpc"][:], in_=sm["junk"][:], axis=AX.X)
                    V("dve", "tensor_scalar", [sm["tmpc"], carry], [ncum[i]], out=ncum[i][:, blk * 4 + t4:blk * 4 + t4 + 1], in0=sm["tmpc"][:],
                      scalar1=carry[:, i:i + 1], scalar2=-1.0, op0=ALU.add, op1=ALU.mult)
                    V("dve", "tensor_tensor", [pA, carry], [carry], out=carry[:, i:i + 1], in0=carry[:, i:i + 1], in1=pA[:, 127:128], op=ALU.add)
            for i in range(2):
                p = proj_fm(6 + 2 * i)
                V("act", "mul", [p], [qa[i]], out=qa[i][:], in_=p[:], mul=float(128 ** -0.5))
                p = proj_fm(6 + 2 * i + 1)
                V("act", "copy", [p], [kcache[i]], out=kcache[i][:, tok], in_=p[:])
                p = proj_tm(2 + i)
                V("act", "copy", [p], [vcache[i]], out=vcache[i][:, blk * 4:(blk + 1) * 4, :], in_=p[:].rearrange("p (a b) -> p a b", a=4))
            def att_task():
                for i in range(0 if skip_attn else 2):
                    R = attn(i, blk, [slice(0, 128)], 1.0, bias_rows=(ncum[i], Rq[i]))
                    V("act", "copy", [R[0]], [oT], out=oT[:, i, :], in_=R[0][:])

            preps = {}
            for i in range(0 if skip_rec else 2):
                qc = conv_silu(proj_fm(3 * i), 3 * i, blk)
                kc = conv_silu(proj_fm(3 * i + 1), 3 * i + 1, blk)
                vc = conv_silu(proj_fm(3 * i + 2), 3 * i + 2, blk)
                l2n(qc, 128 ** -0.5)
                l2n(kc, 1.0)
                p = proj_tm(i)
                V("act", "copy", [p], [gtok[i]], out=gtok[i][:], in_=p[:].rearrange("p (a b) -> p a b", a=4))
                preps[i] = (qc, kc, vc)

            def rec_task(i):
                qc, kc, vc = preps[i]
                sm = smh[i]
                sl_ = slots[i]
                pA, pF, pD, pE, pC, pG = sl_["A"], sl_["F"], sl_["D"], sl_["E"], sl_["C"], sl_["G"]
                for t4 in range(4):
                    cs = slice(t4 * 128, (t4 + 1) * 128)
                    decay_prep(sm, pA, lf[:, t4, i:i + 1], lf[:, t4, 0:4], i)
                    bcol = sm["bc"][:, 0:1]
                    beta = lf[:, t4, 2 + i:3 + i]
                    V("dve", "scalar_tensor_tensor", [sm["crep"], sm["bc"], MBu], [sm["Dm"]], out=sm["Dm"][:], in0=sm["crep"][:],
                      scalar=bcol, in1=MBu[:], op0=ALU.subtract, op1=ALU.add)
                    V("act", "activation", [sm["Dm"]], [sm["G"]], out=sm["G"][:], in_=sm["Dm"][:], func=AF.Exp)
                    V("dve", "scalar_tensor_tensor", [sm["crep"], sm["bc"], MBl], [sm["Dl"]], out=sm["Dl"][:], in0=sm["crep"][:],
                      scalar=bcol, in1=MBl[:], op0=ALU.subtract, op1=ALU.add)
                    V("act", "activation", [sm["Dl"]], [sm["Gl"]], out=sm["Gl"][:], in_=sm["Dl"][:], func=AF.Exp, scale=-1.0)
                    k.tr(pD[:], kc[:, cs], ident[:], [kc, ident], pD)
                    V("act", "copy", [pD], [sm["ktok"]], out=sm["ktok"][:], in_=pD[:])
                    k.tr(pD[:], vc[:, cs], ident[:], [vc, ident], pD)
                    V("dve", "tensor_scalar_mul", [pD, lf], [sm["vb"]], out=sm["vb"][:], in0=pD[:], scalar1=beta)
                    V("act", "activation", [sm["bc"]], [sm["bcol"]], out=sm["bcol"][:], in_=sm["bc"][:, 0:1], func=AF.Exp)
                    V("dve", "tensor_tensor", [sm["bcol"], lf], [sm["bebc"]], out=sm["bebc"][:], in0=sm["bcol"][:], in1=beta, op=ALU.mult)
                    V("dve", "tensor_tensor", [sm["bc"]], [sm["ebd"]], out=sm["ebd"][:], in0=sm["bc"][:, 1:2], in1=sm["bc"][:, 0:1],
                      op=ALU.subtract)
                    V("act", "activation", [sm["ebd"]], [sm["ebd"]], out=sm["ebd"][:], in_=sm["ebd"][:], func=AF.Exp)
                    V("dve", "tensor_scalar_mul", [sm["ktok"], sm["bebc"]], [sm["kp"]], out=sm["kp"][:], in0=sm["ktok"][:],
                      scalar1=sm["bebc"][:, 0:1])
                    V("dve", "tensor_scalar_mul", [sm["ktok"], sm["ebd"]], [sm["kd"]], out=sm["kd"][:], in0=sm["ktok"][:],
                      scalar1=sm["ebd"][:, 0:1])
                    k.mm(pC[:], kc[:, cs], kc[:, cs], True, True, [kc], pC)
                    V("dve", "scalar_tensor_tensor", [pC, lf, sm["Gl"]], [sm["N"]], out=sm["N"][:], in0=pC[:], scalar=beta, in1=sm["Gl"][:],
                      op0=ALU.mult, op1=ALU.mult)
                    k.tr(pC[:], sm["N"][:], ident[:], [sm["N"], ident], pC)
                    V("act", "copy", [pC], [sm["M"]], out=sm["M"][:], in_=pC[:])
                    V("dve", "tensor_tensor", [ident, sm["M"]], [sm["Y"]], out=sm["Y"][:], in0=ident[:], in1=sm["M"][:], op=ALU.subtract)
                    Pc, Mc = sm["N"], sm["M"]
                    Pn, Mn = sm["P"], sm["Mk"]
                    for lev in range(5):
                        k.mm(pC[:], Mc[:], Pc[:], True, True, [Mc, Pc], pC)
                        if lev < 4:
                            k.mm(pD[:], Pc[:], Mc[:], True, True, [Mc, Pc], pD)
                        V("act", "copy", [pC], [Pn], out=Pn[:], in_=pC[:])
                        if lev < 4:
                            V("dve", "tensor_copy", [pD], [Mn], out=Mn[:], in_=pD[:])
                        k.mm(pA[:], Pn[:], sm["Y"][:], True, True, [Pn, sm["Y"]], pA)
                        V("dve", "tensor_tensor", [sm["Y"], pA], [sm["Y"]], out=sm["Y"][:], in0=sm["Y"][:], in1=pA[:], op=ALU.add)
                        Pc, Pn = Pn, Pc
                        Mc, Mn = Mn, Mc
                    k.mm(pC[:], sm["Y"][:], sm["vb"][:], True, True, [sm["Y"], sm["vb"]], pC)
                    V("act", "copy", [pC], [sm["u"]], out=sm["u"][:], in_=pC[:])
                    k.mm(pD[:], sm["kp"][:], sm["Y"][:], True, True, [sm["Y"], sm["kp"]], pD)
                    V("dve", "memset", [], [sm["wT0"]], ap=sm["wT0"][:], constant=0.0)
                    V("dve", "memset", [], [sm["wT1"]], ap=sm["wT1"][:], constant=0.0)
                    V("dve", "tensor_copy", [pD], [sm["wT0"]], out=sm["wT0"][:, 0:64], in_=pD[:, 0:64])
                    V("dve", "tensor_copy", [pD], [sm["wT1"]], out=sm["wT1"][:, 64:128], in_=pD[:, 64:128])
                    k.mm(pC[:], kc[:, cs], qc[:, cs], True, True, [kc, qc], pC)
                    V("dve", "tensor_tensor", [pC, sm["G"]], [sm["AT"]], out=sm["AT"][:], in0=pC[:], in1=sm["G"][:], op=ALU.mult)
                    V("dve", "tensor_tensor", [qc, sm["ecr"]], [qhat[i]], out=qhat[i][:, 0, 0:64], in0=qc[:, t4 * 128:t4 * 128 + 64],
                      in1=sm["ecr"][:, 0:64], op=ALU.mult)
                    V("dve", "tensor_tensor", [qc, sm["ecr"]], [qhat[i]], out=qhat[i][:, 1, 64:128], in0=qc[:, t4 * 128 + 64:(t4 + 1) * 128],
                      in1=sm["ecr"][:, 64:128], op=ALU.mult)
                    S0, S1 = Sst[i]
                    k.mm(pA[:], sm["wT0"][:], S0[:, 0:128], True, True, [sm["wT0"], S0], pA)
                    V("dve", "tensor_tensor", [sm["u"], pA], [sm["vn"]], out=sm["vn"][0:64, :], in0=sm["u"][0:64, :], in1=pA[0:64, :],
                      op=ALU.subtract)
                    k.mm(pE[:, 0:128], qhat[i][:, 0, :], S0[:, 0:128], True, False, [qhat[i], S0], pE)
                    k.mm(pF[:, 0:128], sm["kd"][0:64, :], sm["vn"][0:64, :], True, True, [sm["kd"], sm["vn"]], pF)
                    V("dve", "scalar_tensor_tensor", [S0, sm["ecr"], pF], [S1], out=S1[:, 0:128], in0=S0[:, 0:128], scalar=sm["ecr"][:, 63:64],
                      in1=pF[:, 0:128], op0=ALU.mult, op1=ALU.add)
                    k.mm(pA[:], sm["wT1"][:], S1[:, 0:128], True, True, [sm["wT1"], S1], pA)
                    V("dve", "tensor_tensor", [sm["u"], pA], [sm["vn"]], out=sm["vn"][64:128, :], in0=sm["u"][64:128, :], in1=pA[64:128, :],
                      op=ALU.subtract)
                    k.mm(pE[:, 0:128], qhat[i][:, 1, :], S1[:, 0:128], False, False, [qhat[i], S1], pE)
                    k.mm(pE[:, 0:128], sm["AT"][:], sm["vn"][:], False, True, [sm["AT"], sm["vn"]], pE)
                    k.mm(pF[:, 0:128], sm["kd"][64:128, :], sm["vn"][64:128, :], True, True, [sm["kd"], sm["vn"]], pF)
                    V("dve", "scalar_tensor_tensor", [S1, sm["ecr"], pF], [S0], out=S0[:, 0:128], in0=S1[:, 0:128], scalar=sm["ecr"][:, 127:128],
                      in1=pF[:, 0:128], op0=ALU.mult, op1=ALU.add)
                    V("act", "copy", [pE], [sm["hh"]], out=sm["hh"][:], in_=pE[:, 0:128])
                    post_out(sm, pG, i, t4, None, AF.Silu)


            tl = [record(att_task)] + [record(lambda i=i: rec_task(i)) for i in range(0 if skip_rec else 2)]
            interleave(tl)

        for c in range(8):
            p = pj[pji[0] % 2]
            pji[0] += 1
            for hs in range(4):
                k.mm(p[:], wout16[:, hs, c * 128:(c + 1) * 128], oT[:, hs, :], hs == 0, hs == 3, [wout16, oT], p)
            V("act", "copy", [p], [x32], out=x32[:, c], in_=p[:])
        k.store(io["out"](blk), x32[:], x32, ores)
    if not standalone:
        return None, k
    nc = k.finish([ores])
    return nc, k


def _kc(w):
    n = w.shape[1]
    return np.ascontiguousarray(w.reshape(8, 128, n).transpose(1, 0, 2).reshape(128, 8 * n))


def mix_consts(S, odd):
    idx = np.arange(128)
    same = (idx[:, None] // 64) == (idx[None, :] // 64)
    U = ((idx[:, None] <= idx[None, :]) & same).astype(np.float32)
    BD = same.astype(np.float32)
    MBu = np.where((idx[None, :] >= idx[:, None]) & same, 0.0, -30000.0).astype(np.float32)
    MBl = np.where((idx[:, None] > idx[None, :]) & same, 0.0, 30000.0).astype(np.float32)
    Uf = (idx[:, None] <= idx[None, :]).astype(np.float32)
    t = np.arange(512)
    masks = np.zeros((4, 128, 512), np.float32)
    for j in range(4):
        key = 128 * j + idx
        if odd:
            masks[j] = (key[:, None] <= t[None, :])
        else:
            masks[j] = ((key[:, None] // 64) <= (t[None, :] // 64))
    d = {"ident": np.eye(128, dtype=np.float32), "U": U, "BD": BD, "MBu": MBu, "MBl": MBl, "masks": masks}
    if odd:
        d["Uf"] = Uf
    else:
        inv = (10000.0 ** (-np.arange(0, 64, 2, dtype=np.float32) / np.float32(64))).astype(np.float32)
        ang = np.arange(S, dtype=np.float32)[None, :] * inv[:, None]
        cos, sin = np.cos(ang).astype(np.float32), np.sin(ang).astype(np.float32)
        p = np.arange(128)
        sign = np.where((p % 64) < 32, -1.0, 1.0).astype(np.float32)
        d["cosT"] = np.ascontiguousarray(cos[p % 32])
        d["sinT"] = np.ascontiguousarray(sin[p % 32] * sign[:, None])
    return d


def mix_inputs_even(hh, w_in, w_out, lq1, lk1, lq2, lk2, subln, conv_b, ig, fg, b_norm, gain):
    hs = [2 * hh, 2 * hh + 1]
    r = np.arange(128)
    swap = np.concatenate([r[32:64], r[0:32], r[96:128], r[64:96]])
    fcols = []
    for a in hs:
        fcols += [128 * a + r, 128 * a + swap, 512 + 128 * a + r, 512 + 128 * a + swap]
    for b in hs:
        fcols += [1536 + 128 * b + r, 2048 + 128 * b + r]
    tcols = [1024 + 128 * a + r for a in hs]
    for b in hs:
        tcols += [2560 + 128 * b + r, 3072 + 128 * b + r]
    gcols = [3584 + hs[0], 3584 + hs[1], 3588 + hs[0], 3588 + hs[1]]
    orow = np.concatenate([128 * hs[0] + r, 128 * hs[1] + r, 512 + 128 * hs[0] + r, 512 + 128 * hs[1] + r])
    cw = np.zeros((128, 16), np.float32)
    for i, b in enumerate(hs):
        cw[:, (2 * i) * 4:(2 * i) * 4 + 4] = conv_b[:, 128 * b + r].T
        cw[:, (2 * i + 1) * 4:(2 * i + 1) * 4 + 4] = conv_b[:, 512 + 128 * b + r].T
    return {"gain": chunk_cols(gain), "wf": np.ascontiguousarray(w_in[:, np.concatenate(fcols)]),
            "wt": np.ascontiguousarray(w_in[:, np.concatenate(tcols)]), "wgt": _kc(w_in[:, gcols]),
            "gbias": np.array([[ig[hs[0]], ig[hs[1]], fg[hs[0]], fg[hs[1]]]], np.float32),
            "wout": np.ascontiguousarray(w_out[orow]), "convw": cw, "nrm": np.ascontiguousarray(b_norm[None, :]),
            "lamv": np.concatenate([lq1, lk1, lq2, lk2])[None, :].astype(np.float32), "subln": np.ascontiguousarray(subln[:, None])}


def mix_inputs_odd(hh, w_in, w_out, conv_c, a_log, dt_bias, c_norm, fd_bias, gain):
    hs = [2 * hh, 2 * hh + 1]
    r = np.arange(128)
    fcols = []
    for c in hs:
        fcols += [128 * c + r, 512 + 128 * c + r, 1024 + 128 * c + r]
    for d in hs:
        fcols += [2056 + 128 * d + r, 2568 + 128 * d + r]
    tcols = [1536 + 128 * c + r for c in hs] + [3080 + 128 * d + r for d in hs]
    gcols = [2048 + hs[0], 2048 + hs[1], 2052 + hs[0], 2052 + hs[1], 3592 + hs[0], 3592 + hs[1]]
    orow = np.concatenate([512 + 128 * hs[0] + r, 512 + 128 * hs[1] + r, 128 * hs[0] + r, 128 * hs[1] + r])
    cw = np.zeros((128, 24), np.float32)
    for i, c in enumerate(hs):
        for j, off in enumerate((0, 512, 1024)):
            g = 3 * i + j
            cw[:, g * 4:g * 4 + 4] = conv_c[:, off + 128 * c + r].T
    return {"gain": chunk_cols(gain), "wf": np.ascontiguousarray(w_in[:, np.concatenate(fcols)]),
            "wt": np.ascontiguousarray(w_in[:, np.concatenate(tcols)]), "wgt": _kc(w_in[:, gcols]),
            "gbias": np.array([[dt_bias[hs[0]], dt_bias[hs[1]], 0.0, 0.0, fd_bias[hs[0]], fd_bias[hs[1]]]], np.float32),
            "wout": np.ascontiguousarray(w_out[orow]), "convw": cw, "nrm": np.ascontiguousarray(c_norm[None, :]),
            "alog": np.array([[a_log[hs[0]], a_log[hs[1]]]], np.float32)}


import math

_GROUPS = [[0, 1], [2, 3], [4, 5], [6, 7]]


def build_fused(S):
    T = S // 2
    NBH = T // 512
    k = KB()
    c8 = lambda ap, tok: ap[:, tok].rearrange("(c p) t -> p c t", p=128)
    xT = k.din("xT", [D, S])
    xh = k.din("xh", [D, T])
    outT = k.dout("outT", [D, T])
    part = [k.dint("part%d" % l, [2 * D, T]) for l in range(2)]
    psum = [k.dint("psumd%d" % l, [D, T]) for l in range(2)]
    h1 = k.dint("h1", [D, T])
    h1f = k.dint("h1f", [2 * D, T])
    part_r = [Res("part%d" % l) for l in range(2)]
    psum_r = [Res("psumd%d" % l) for l in range(2)]
    h1_r, h1f_r, out_r = Res("h1"), Res("h1f"), Res("out")

    def halfblk(ap):
        return lambda blk: ap[(blk // NBH) * D:(blk // NBH + 1) * D, (blk % NBH) * 512:(blk % NBH + 1) * 512].rearrange(
            "(c p) t -> p c t", p=128)

    k.pfx = "L0m_"
    build_mix(S, False, lam_init=0.8 - 0.6 * math.exp(-0.3 * 0), k=k,
              io={"h": lambda blk: c8(xT, slice(blk * 512, (blk + 1) * 512)), "h_res": [], "out": halfblk(part[0]), "out_res": part_r[0]})
    k.collective("ReduceScatter", ALU.add, part[0][:, :], psum[0][:, :], part_r[0], psum_r[0], _GROUPS)
    k.new_section("L0f_")
    build_ffn(T, False, SBT=min(1024, T), k=k,
              io={"h": lambda tok: c8(xh, tok), "h_res": [], "pins": [lambda tok: c8(psum[0], tok)], "pin_res": [psum_r[0]],
                  "out": lambda tok: c8(h1, tok), "out_res": h1_r})
    k.collective("AllGather", ALU.bypass, h1[:, :], h1f[:, :], h1_r, h1f_r, _GROUPS)
    k.new_section("L1m_")
    build_mix(S, True, k=k, io={"h": halfblk(h1f), "h_res": [h1f_r], "out": halfblk(part[1]), "out_res": part_r[1]})
    k.collective("ReduceScatter", ALU.add, part[1][:, :], psum[1][:, :], part_r[1], psum_r[1], _GROUPS)
    k.new_section("L1f_")
    build_ffn(T, True, SBT=min(1024, T), k=k,
              io={"h": lambda tok: c8(h1, tok), "h_res": [h1_r], "pins": [lambda tok: c8(psum[1], tok)], "pin_res": [psum_r[1]],
                  "out": lambda tok: c8(outT, tok), "out_res": out_r})
    nc = k.finish([out_r])
    return nc, k


_PROGS = {}


def build_fused4(S):
    k = KB()
    c8 = lambda ap, tok: ap[:, tok].rearrange("(c p) t -> p c t", p=128)
    blk8 = lambda ap: (lambda blk: c8(ap, slice(blk * 512, (blk + 1) * 512)))
    xT = k.din("xT", [D, S])
    outT = k.dout("outT", [D, S])
    pa = k.dint("partA", [D, S])
    pb = k.dint("partB", [D, S])
    h1 = k.dint("h1", [D, S])
    pa_r, pb_r, h1_r, out_r = Res("partA"), Res("partB"), Res("h1"), Res("out")
    first = True
    for layer in range(2):
        odd = layer % 2 == 1
        hsrc, hres = (xT, []) if layer == 0 else (h1, [h1_r])
        for tag, dst, dres in (("A", pa, pa_r), ("B", pb, pb_r)):
            if first:
                k.pfx = "L%dm%s_" % (layer, tag)
                first = False
            else:
                k.new_section("L%dm%s_" % (layer, tag))
            build_mix(S, odd, lam_init=0.8 - 0.6 * math.exp(-0.3 * layer), k=k,
                      io={"h": blk8(hsrc), "h_res": hres, "out": blk8(dst), "out_res": dres})
        k.new_section("L%df_" % layer)
        final = layer == 1
        odst, ores_ = (outT, out_r) if final else (h1, h1_r)
        build_ffn(S, final, SBT=min(1024, S), k=k,
                  io={"h": lambda tok, a=hsrc: c8(a, tok), "h_res": hres,
                      "pins": [lambda tok: c8(pa, tok), lambda tok: c8(pb, tok)], "pin_res": [pa_r, pb_r],
                      "out": lambda tok, a=odst: c8(a, tok), "out_res": ores_})
    nc = k.finish([out_r])
    return nc, k


def fused4_inputs(b, S, x, p, W):
    f = lambda a: np.asarray(a, np.float32)
    m = {"xT": np.ascontiguousarray(x[b].T)}
    ident, sel = ffn_consts()
    for layer in range(2):
        odd = layer % 2 == 1
        j = layer // 2
        cst = mix_consts(S, odd)
        for r, tag in ((0, "A"), (1, "B")):
            if odd:
                d = mix_inputs_odd(r, f(W["cd_w_in"][j]), f(W["cd_w_out"][j]), f(W["c_conv"][j]), f(W["c_a_log"][j]), f(W["c_dt_bias"][j]),
                                   f(W["c_norm"][j]), f(W["d_fgate_bias"][j]), f(W["norm_mix"][layer]))
            else:
                d = mix_inputs_even(r, f(W["ab_w_in"][j]), f(W["ab_w_out"][j]), f(W["a_lam_q1"][j]), f(W["a_lam_k1"][j]), f(W["a_lam_q2"][j]),
                                    f(W["a_lam_k2"][j]), f(W["a_subln"][j]), f(W["b_conv"][j]), f(W["b_igate_bias"][j]),
                                    f(W["b_fgate_bias"][j]), f(W["b_norm"][j]), f(W["norm_mix"][layer]))
            d.update(cst)
            for kk, vv in d.items():
                m["L%dm%s_%s" % (layer, tag, kk)] = vv
        Wr = np.concatenate([f(W["moe_w_group"][layer]), f(W["moe_w_router"][layer])], axis=1)
        fd = {"pT": np.ascontiguousarray(p[layer, b].T), "gain": chunk_cols(W["norm_ffn"][layer]), "gfin": chunk_cols(W["norm_final"]),
              "wr": np.ascontiguousarray(Wr.reshape(8, 128, 20).transpose(1, 0, 2).reshape(128, 160)),
              "br": np.concatenate([f(W["moe_b_group"][layer]), f(W["moe_b_router"][layer])])[None, :],
              "wg": tile_w(W["moe_w_gate"][layer]), "wu": tile_w(W["moe_w_up"][layer]), "wd": tile_w(W["moe_w_down"][layer]),
              "plg": f(W["ple_w_gate"][layer]), "plp": f(W["ple_w_proj"][layer]), "ident": ident, "sel": sel}
        for kk, vv in fd.items():
            m["L%df_%s" % (layer, kk)] = vv
    return m


def kernel(x, p, **W):
    x = np.asarray(x, np.float32)
    p = np.asarray(p, np.float32)
    B, S, _ = x.shape
    key = ("f4", S)
    if key not in _PROGS:
        _PROGS[key] = build_fused4(S)[0]
    nc = _PROGS[key]
    per_b = [fused4_inputs(b, S, x, p, W) for b in range(B)]
    maps = [per_b[c % B] for c in range(NCORES)]
    res = run_bass_kernel_spmd(nc, maps, core_ids=list(range(NCORES)))
    return np.stack([np.ascontiguousarray(res.results[b]["outT"].T) for b in range(B)]).astype(np.float32)


def fused_inputs(c, S, x, p, W):
    f = lambda a: np.asarray(a, np.float32)
    T = S // 2
    b, r = c // 2, c % 2
    ts = slice(r * T, (r + 1) * T)
    xTb = np.ascontiguousarray(x[b].T)
    m = {"xT": xTb, "xh": np.ascontiguousarray(xTb[:, ts])}
    ident, sel = ffn_consts()
    for layer in range(2):
        odd = layer % 2 == 1
        j = layer // 2
        if odd:
            d = mix_inputs_odd(r, f(W["cd_w_in"][j]), f(W["cd_w_out"][j]), f(W["c_conv"][j]), f(W["c_a_log"][j]), f(W["c_dt_bias"][j]),
                               f(W["c_norm"][j]), f(W["d_fgate_bias"][j]), f(W["norm_mix"][layer]))
        else:
            d = mix_inputs_even(r, f(W["ab_w_in"][j]), f(W["ab_w_out"][j]), f(W["a_lam_q1"][j]), f(W["a_lam_k1"][j]), f(W["a_lam_q2"][j]),
                                f(W["a_lam_k2"][j]), f(W["a_subln"][j]), f(W["b_conv"][j]), f(W["b_igate_bias"][j]),
                                f(W["b_fgate_bias"][j]), f(W["b_norm"][j]), f(W["norm_mix"][layer]))
        d.update(mix_consts(S, odd))
        for kk, vv in d.items():
            m["L%dm_%s" % (layer, kk)] = vv
        Wr = np.concatenate([f(W["moe_w_group"][layer]), f(W["moe_w_router"][layer])], axis=1)
        fd = {"pT": np.ascontiguousarray(p[layer, b, ts].T), "gain": chunk_cols(W["norm_ffn"][layer]), "gfin": chunk_cols(W["norm_final"]),
              "wr": np.ascontiguousarray(Wr.reshape(8, 128, 20).transpose(1, 0, 2).reshape(128, 160)),
              "br": np.concatenate([f(W["moe_b_group"][layer]), f(W["moe_b_router"][layer])])[None, :],
              "wg": tile_w(W["moe_w_gate"][layer]), "wu": tile_w(W["moe_w_up"][layer]), "wd": tile_w(W["moe_w_down"][layer]),
              "plg": f(W["ple_w_gate"][layer]), "plp": f(W["ple_w_proj"][layer]), "ident": ident, "sel": sel}
        for kk, vv in fd.items():
            m["L%df_%s" % (layer, kk)] = vv
    return m


def kernel_cc(x, p, **W):
    x = np.asarray(x, np.float32)
    p = np.asarray(p, np.float32)
    B, S, _ = x.shape
    T = S // 2
    if S not in _PROGS:
        _PROGS[S] = build_fused(S)[0]
    nc = _PROGS[S]
    maps = [fused_inputs(c, S, x, p, W) for c in range(NCORES)]
    res = run_bass_kernel_spmd(nc, maps, core_ids=list(range(NCORES)))
    out = np.empty((B, S, D), np.float32)
    for c in range(NCORES):
        b, r = c // 2, c % 2
        out[b, r * T:(r + 1) * T, :] = res.results[c]["outT"].T
    return out
```
